# Optimizing a Trainium2 kernel written in Bass

```python
import math
import jax, jax.numpy as jnp
from jax import lax
import numpy as np

D_MODEL = 2048
BATCH = 8
SEQ = 2048
DEPTH = 2

CTX_LEN = 256
GRID_W = 64
HEAD_DIM = 64
MIX_WIDTH = D_MODEL
GROUP_WIDTH = MIX_WIDTH // 4
RET_HEADS = GROUP_WIDTH // HEAD_DIM
RET_CHUNK = 128
CONV_CH = GROUP_WIDTH
SWA_HEADS = GROUP_WIDTH // HEAD_DIM
SWA_KV_HEADS = 2
SWA_WINDOW = 128
SWA_BLOCK = 128
NA_HEADS = GROUP_WIDTH // HEAD_DIM
NA_ROWS = 8
NA_COLS = 16
N_EXPERTS = 16
EXPERT_FF = D_MODEL // 2
EC_CAPACITY_FACTOR = 2
ROPE_BASE = 10000.0
EPS = 1e-6
NEG_INF = -1e30
F32 = jnp.float32

COL_SPLITS = (
    [GROUP_WIDTH] * 4
    + [CONV_CH] * 3
    + [SWA_HEADS * HEAD_DIM, SWA_KV_HEADS * HEAD_DIM, SWA_KV_HEADS * HEAD_DIM]
    + [NA_HEADS * HEAD_DIM] * 3
)
IN_COLS = 4 * GROUP_WIDTH + 3 * CONV_CH + (SWA_HEADS + 2 * SWA_KV_HEADS) * HEAD_DIM + 3 * NA_HEADS * HEAD_DIM

kernel_name = "hybrid_parallel_heads_diffusion_block"


def rmsnorm(x, g):
    xf = x.astype(F32)
    y = xf * lax.rsqrt(jnp.mean(xf * xf, axis=-1, keepdims=True) + EPS)
    return (y * g.astype(F32)).astype(x.dtype)


def head_groupnorm(y):
    yf = y.astype(F32)
    mu = jnp.mean(yf, axis=-1, keepdims=True)
    var = jnp.mean(jnp.square(yf - mu), axis=-1, keepdims=True)
    return (yf - mu) * lax.rsqrt(var + EPS)


def axial_rope(x):
    L, hd = x.shape[1], x.shape[-1]
    half = hd // 2
    nf = half // 2
    t = jnp.arange(L)
    row = (t // GRID_W).astype(F32)
    col = (t % GRID_W).astype(F32)
    inv = ROPE_BASE ** (-jnp.arange(nf, dtype=F32) / nf)
    ang = jnp.concatenate([row[:, None] * inv, col[:, None] * inv], axis=-1)[:, None, :]
    cos, sin = jnp.cos(ang), jnp.sin(ang)
    xf = x.astype(F32)
    x1, x2 = xf[..., :half], xf[..., half:]
    return jnp.concatenate([x1 * cos - x2 * sin, x1 * sin + x2 * cos], axis=-1).astype(x.dtype)


def retention_scan(q, k, v, log_gamma, s0, include_diag):
    B, T, H, d = q.shape
    n = T // RET_CHUNK
    q, k, v = q.astype(F32), k.astype(F32), v.astype(F32)

    def chunks(a):
        return a.reshape(B, n, RET_CHUNK, H, d).transpose(1, 0, 3, 2, 4)

    pos = jnp.arange(RET_CHUNK, dtype=F32)
    diff = pos[:, None] - pos[None, :]
    tri = diff >= 0 if include_diag else diff > 0
    lg = log_gamma.astype(F32)
    dmat = jnp.where(tri, jnp.exp(lg[:, None, None] * jnp.maximum(diff, 0.0)), 0.0)
    xi = jnp.exp(lg[:, None] * (pos + 1.0))[..., None]
    zeta = jnp.exp(lg[:, None] * (RET_CHUNK - 1.0 - pos))[..., None]
    cdecay = jnp.exp(lg * RET_CHUNK)[:, None, None]

    def step(state, inp):
        qi, ki, vi = inp
        inner = jnp.einsum('bhqd,bhkd->bhqk', qi, ki) * dmat
        y = jnp.einsum('bhqk,bhkv->bhqv', inner, vi) + jnp.einsum('bhqd,bhdv->bhqv', qi, state) * xi
        state = state * cdecay + jnp.einsum('bhkd,bhkv->bhdv', ki * zeta, vi)
        return state, y

    state, ys = lax.scan(step, s0, (chunks(q), chunks(k), chunks(v)))
    return ys.transpose(1, 0, 3, 2, 4).reshape(B, T, H, d), state


def retention_mixer(q, k, v, g, qc, kc, vc, gc, logit_f, logit_b, need_ctx):
    B, L, _ = q.shape
    kscale = HEAD_DIM ** -0.5

    def heads(a):
        return a.reshape(a.shape[0], a.shape[1], RET_HEADS, HEAD_DIM)

    q, k, v = axial_rope(heads(q)), axial_rope(heads(k)) * kscale, heads(v)
    qc, kc, vc = heads(qc), heads(kc) * kscale, heads(vc)
    lg_f = jax.nn.log_sigmoid(logit_f.astype(F32))
    lg_b = jax.nn.log_sigmoid(logit_b.astype(F32))
    s0 = jnp.zeros((B, RET_HEADS, HEAD_DIM, HEAD_DIM), F32)

    def flip(a):
        return a[:, ::-1]

    yc_f, st_f = retention_scan(qc, kc, vc, lg_f, s0, True)
    yc_b, st_b = retention_scan(flip(qc), flip(kc), flip(vc), lg_b, s0, False)
    y_f, _ = retention_scan(q, k, v, lg_f, st_f, True)
    y_b, _ = retention_scan(flip(q), flip(k), flip(v), lg_b, st_b, False)
    y = (jax.nn.silu(g.astype(F32)) * head_groupnorm(y_f + flip(y_b)).reshape(B, L, -1)).astype(g.dtype)
    yc = None
    if need_ctx:
        T = qc.shape[1]
        yc = (jax.nn.silu(gc.astype(F32)) * head_groupnorm(yc_f + flip(yc_b)).reshape(B, T, -1)).astype(gc.dtype)
    return y, yc


def short_conv(bg, cg, hin, w):
    u = cg * hin
    up = jnp.pad(u, ((0, 0), (1, 1), (0, 0)))
    y = up[:, :-2] * w[0] + up[:, 1:-1] * w[1] + up[:, 2:] * w[2]
    return bg * y


def dense_ctx_attention(qc, kc, vc, sink):
    B, T = qc.shape[0], qc.shape[1]
    s = jnp.einsum('bqkgd,bckd->bkgqc', qc, kc).astype(F32) * (HEAD_DIM ** -0.5)
    if sink is not None:
        s = jnp.concatenate([s, jnp.broadcast_to(sink.astype(F32)[None, :, :, None, None], s.shape[:-1] + (1,))], axis=-1)
    p = jax.nn.softmax(s, axis=-1)[..., :kc.shape[1]].astype(vc.dtype)
    return jnp.einsum('bkgqc,bckd->bqkgd', p, vc).reshape(B, T, -1)


def swa_mixer(q, k, v, qc, kc, vc, sink, need_ctx):
    B, L, _ = q.shape
    T = qc.shape[1]
    KV, G = SWA_KV_HEADS, SWA_HEADS // SWA_KV_HEADS
    scale = HEAD_DIM ** -0.5
    q = axial_rope(q.reshape(B, L, SWA_HEADS, HEAD_DIM)).reshape(B, L, KV, G, HEAD_DIM)
    k = axial_rope(k.reshape(B, L, KV, HEAD_DIM))
    v = v.reshape(B, L, KV, HEAD_DIM)
    qc = qc.reshape(B, T, KV, G, HEAD_DIM)
    kc = kc.reshape(B, T, KV, HEAD_DIM)
    vc = vc.reshape(B, T, KV, HEAD_DIM)
    nb = L // SWA_BLOCK
    qb = q.reshape(B, nb, SWA_BLOCK, KV, G, HEAD_DIM)

    def band(a):
        ap = jnp.pad(a, ((0, 0), (SWA_BLOCK, SWA_BLOCK), (0, 0), (0, 0))).reshape(B, nb + 2, SWA_BLOCK, KV, HEAD_DIM)
        return jnp.concatenate([ap[:, :-2], ap[:, 1:-1], ap[:, 2:]], axis=2)

    kw, vw = band(k), band(v)
    W = 3 * SWA_BLOCK
    blk = jnp.arange(nb)[:, None, None] * SWA_BLOCK
    qpos = blk + jnp.arange(SWA_BLOCK)[None, :, None]
    kpos = blk - SWA_BLOCK + jnp.arange(W)[None, None, :]
    valid = (jnp.abs(kpos - qpos) <= SWA_WINDOW) & (kpos >= 0) & (kpos < L)
    s_win = jnp.einsum('bnqkgd,bnjkd->bnkgqj', qb, kw).astype(F32) * scale
    s_win = jnp.where(valid[None, :, None, None], s_win, NEG_INF)
    s_ctx = jnp.einsum('bnqkgd,bckd->bnkgqc', qb, kc).astype(F32) * scale
    s_sink = jnp.broadcast_to(sink.astype(F32).reshape(KV, G)[None, None, :, :, None, None], s_win.shape[:-1] + (1,))
    p = jax.nn.softmax(jnp.concatenate([s_win, s_ctx, s_sink], axis=-1), axis=-1).astype(v.dtype)
    o = (jnp.einsum('bnkgqj,bnjkd->bnqkgd', p[..., :W], vw)
         + jnp.einsum('bnkgqc,bckd->bnqkgd', p[..., W:W + T], vc))
    y = o.reshape(B, L, SWA_HEADS * HEAD_DIM)
    yc = dense_ctx_attention(qc, kc, vc, sink.reshape(KV, G)) if need_ctx else None
    return y, yc


def na_mixer(q, k, v, qc, kc, vc, rpb, need_ctx):
    B, L, _ = q.shape
    T = qc.shape[1]
    H = NA_HEADS
    scale = HEAD_DIM ** -0.5
    rows = L // GRID_W
    wr, wc = min(NA_ROWS, rows), NA_COLS
    qg = q.reshape(B, rows, GRID_W, H, HEAD_DIM)
    kg = k.reshape(B, rows, GRID_W, H, HEAD_DIM)
    vg = v.reshape(B, rows, GRID_W, H, HEAD_DIM)
    kc4 = kc.reshape(B, T, H, HEAD_DIM)
    vc4 = vc.reshape(B, T, H, HEAD_DIM)
    r = jnp.arange(rows)
    row_idx = jnp.clip(r - wr // 2, 0, rows - wr)[:, None] + jnp.arange(wr)[None, :]
    nk = wr * GRID_W
    kb = kg[:, row_idx].reshape(B, rows, nk, H, HEAD_DIM)
    vb = vg[:, row_idx].reshape(B, rows, nk, H, HEAD_DIM)
    cq = jnp.arange(GRID_W)
    col_start = jnp.clip(cq - wc // 2, 0, GRID_W - wc)
    col_ok = (cq[None, :] >= col_start[:, None]) & (cq[None, :] < col_start[:, None] + wc)
    dr = row_idx - r[:, None] + (NA_ROWS - 1)
    dc = jnp.clip(cq[None, :] - cq[:, None], -(wc - 1), wc - 1) + (NA_COLS - 1)
    bias = rpb.astype(F32)[:, dr[:, :, None, None], dc[None, None, :, :]]
    bias = jnp.where(col_ok[None, None, None], bias, NEG_INF)
    bias = bias.transpose(1, 0, 3, 2, 4).reshape(rows, H, GRID_W, nk)
    s_nb = jnp.einsum('brqhd,brkhd->brhqk', qg, kb).astype(F32) * scale + bias[None]
    s_ctx = jnp.einsum('brqhd,bchd->brhqc', qg, kc4).astype(F32) * scale
    p = jax.nn.softmax(jnp.concatenate([s_nb, s_ctx], axis=-1), axis=-1).astype(v.dtype)
    o = (jnp.einsum('brhqk,brkhd->brqhd', p[..., :nk], vb)
         + jnp.einsum('brhqc,bchd->brqhd', p[..., nk:], vc4))
    y = o.reshape(B, L, H * HEAD_DIM)
    yc = dense_ctx_attention(qc.reshape(B, T, H, 1, HEAD_DIM), kc4, vc4, None) if need_ctx else None
    return y, yc


def split_cols(z):
    idx = [int(i) for i in np.cumsum(COL_SPLITS)[:-1]]
    return jnp.split(z, idx, axis=-1)


def token_mix(h, hc, w_in, w_out, logit_f, logit_b, conv_w, sink, rpb, need_ctx):
    p = split_cols(h @ w_in)
    pc = split_cols(hc @ w_in)
    y_ret, yc_ret = retention_mixer(*p[0:4], *pc[0:4], logit_f, logit_b, need_ctx)
    y_conv = short_conv(*p[4:7], conv_w)
    y_swa, yc_swa = swa_mixer(*p[7:10], *pc[7:10], sink, need_ctx)
    y_na, yc_na = na_mixer(*p[10:13], *pc[10:13], rpb, need_ctx)
    y = jnp.concatenate([y_ret, y_conv, y_swa, y_na], axis=-1) @ w_out
    yc = None
    if need_ctx:
        yc_conv = short_conv(*pc[4:7], conv_w)
        yc = jnp.concatenate([yc_ret, yc_conv, yc_swa, yc_na], axis=-1) @ w_out
    return y, yc


def expert_choice_ffn(h, w_router, w_gate, w_up, w_down):
    B, T, D = h.shape
    cap = EC_CAPACITY_FACTOR * T // N_EXPERTS
    aff = jax.nn.softmax((h @ w_router).astype(F32), axis=-1)
    gate, idx = lax.top_k(jnp.swapaxes(aff, 1, 2), cap)
    xs = jax.vmap(lambda hb, ib: hb[ib])(h, idx)
    a = jnp.einsum('becd,edf->becf', xs, w_gate)
    u = jnp.einsum('becd,edf->becf', xs, w_up)
    ye = jnp.einsum('becf,efd->becd', jax.nn.silu(a) * u, w_down) * gate[..., None].astype(h.dtype)
    return jax.vmap(lambda yb, ib: jnp.zeros((T, D), yb.dtype).at[ib.reshape(-1)].add(yb.reshape(-1, D)))(ye, idx)


def setup_inputs(seed: int = 0) -> dict:
    key = jax.random.key(seed)
    ks = jax.random.split(key, 24)
    D = D_MODEL
    nrm = jax.random.normal
    gamma0 = 1.0 - 2.0 ** (-5.0 - jnp.arange(RET_HEADS, dtype=F32))
    logit0 = jnp.log(gamma0) - jnp.log1p(-gamma0)
    return {
        "x": nrm(ks[0], (BATCH, SEQ, D), F32),
        "c": nrm(ks[1], (BATCH, D), F32),
        "ctx": nrm(ks[2], (BATCH, CTX_LEN, D), F32),
        "c_ctx": nrm(ks[3], (D,), F32),
        "w_ada": nrm(ks[4], (DEPTH, D, 6 * D), F32) * (0.5 * D ** -0.5),
        "b_ada": nrm(ks[5], (DEPTH, 6 * D), F32) * 0.01,
        "norm_mix": 1.0 + 0.02 * nrm(ks[6], (DEPTH, D), F32),
        "norm_ffn": 1.0 + 0.02 * nrm(ks[7], (DEPTH, D), F32),
        "w_in": nrm(ks[8], (DEPTH, D, IN_COLS), F32) * D ** -0.5,
        "w_out": nrm(ks[9], (DEPTH, MIX_WIDTH, D), F32) * MIX_WIDTH ** -0.5,
        "ret_decay_fwd": logit0[None] + 0.1 * nrm(ks[10], (DEPTH, RET_HEADS), F32),
        "ret_decay_bwd": logit0[None] + 0.1 * nrm(ks[11], (DEPTH, RET_HEADS), F32),
        "conv_w": nrm(ks[12], (DEPTH, 3, CONV_CH), F32) * 3.0 ** -0.5,
        "swa_sink": 0.5 * nrm(ks[13], (DEPTH, SWA_HEADS), F32),
        "na_rpb": 0.1 * nrm(ks[14], (DEPTH, NA_HEADS, 2 * NA_ROWS - 1, 2 * NA_COLS - 1), F32),
        "w_router": nrm(ks[15], (DEPTH, D, N_EXPERTS), F32) * D ** -0.5,
        "w_gate": nrm(ks[16], (DEPTH, N_EXPERTS, D, EXPERT_FF), F32) * D ** -0.5,
        "w_up": nrm(ks[17], (DEPTH, N_EXPERTS, D, EXPERT_FF), F32) * D ** -0.5,
        "w_down": nrm(ks[18], (DEPTH, N_EXPERTS, EXPERT_FF, D), F32) * EXPERT_FF ** -0.5,
        "norm_final": 1.0 + 0.02 * nrm(ks[19], (D,), F32),
    }


def reference(x, c, ctx, c_ctx, w_ada, b_ada, norm_mix, norm_ffn, w_in, w_out, ret_decay_fwd, ret_decay_bwd,
              conv_w, swa_sink, na_rpb, w_router, w_gate, w_up, w_down, norm_final):
    xc = ctx
    for l in range(DEPTH):
        need_ctx = l < DEPTH - 1
        ada = jax.nn.silu(c) @ w_ada[l] + b_ada[l]
        ada_c = jax.nn.silu(c_ctx) @ w_ada[l] + b_ada[l]
        sh1, sc1, g1, sh2, sc2, g2 = [a[:, None, :] for a in jnp.split(ada, 6, axis=-1)]
        sh1c, sc1c, g1c, sh2c, sc2c, g2c = jnp.split(ada_c, 6, axis=-1)
        h = rmsnorm(x, norm_mix[l]) * (1.0 + sc1) + sh1
        hc = rmsnorm(xc, norm_mix[l]) * (1.0 + sc1c) + sh1c
        y, yc = token_mix(h, hc, w_in[l], w_out[l], ret_decay_fwd[l], ret_decay_bwd[l], conv_w[l],
                          swa_sink[l], na_rpb[l], need_ctx)
        x = x + g1 * y
        h = rmsnorm(x, norm_ffn[l]) * (1.0 + sc2) + sh2
        x = x + g2 * expert_choice_ffn(h, w_router[l], w_gate[l], w_up[l], w_down[l])
        if need_ctx:
            xc = xc + g1c * yc
            hc = rmsnorm(xc, norm_ffn[l]) * (1.0 + sc2c) + sh2c
            xc = xc + g2c * expert_choice_ffn(hc, w_router[l], w_gate[l], w_up[l], w_down[l])
    return rmsnorm(x, norm_final)
```

```python
from contextlib import ExitStack
import numpy as np
import concourse.bass as bass
import concourse.mybir as mybir
from concourse.bass_utils import run_bass_kernel_spmd

F32 = mybir.dt.float32
BF16 = mybir.dt.bfloat16
U32 = mybir.dt.uint32
ALU = mybir.AluOpType
AF = mybir.ActivationFunctionType
AX = mybir.AxisListType

D = 2048
L = 2048
T = 256
NT = L + T
DEPTH = 2
NE = 16
FF = 1024
CAP = 256
CAPC = 32
NSL = CAP + CAPC
IN_COLS = 5888
EPS = 1e-6
GROUPS = [(0, 512), (512, 512), (1024, 512), (1536, 512), (2048, 256)]
NEG = -30000.0

ZT_RQ, ZT_RK, ZT_RV, ZT_RG, ZT_SQ, ZT_SK, ZT_SV, ZT_NV = 0, 512, 1024, 1536, 2048, 2560, 2688, 2816
ZT_COLS = 3328
ZF_CB, ZF_CC, ZF_CH, ZF_NQ, ZF_NK = 0, 512, 1024, 1536, 2048
ZF_ROWS = 2560

N_DMA_SEMS = 24
N_SW_SEMS = 8


class Prog:
    def __init__(self, nc):
        self.nc = nc
        self.eng = {"pe": nc.tensor, "act": nc.scalar, "dve": nc.vector,
                    "pool": nc.gpsimd, "sp": nc.sync}
        self.sem = {k: nc.alloc_semaphore(name=f"s_{k}") for k in self.eng}
        self.cnt = {k: 0 for k in self.eng}
        self.dma_sems = [nc.alloc_semaphore(name=f"s_dma{i}") for i in range(N_DMA_SEMS)]
        self.dma_cnt = [0] * N_DMA_SEMS
        self.dma_rr = 0
        self.known = {k: {} for k in self.eng}
        self.res = {}
        self.nwaits = 0
        self.nops = 0
        self.sw_sems = [nc.alloc_semaphore(name=f"s_sw{i}") for i in range(N_SW_SEMS)]
        self.sw_cnt = [0] * N_SW_SEMS
        self.sw_rr = 0

    def _semh(self, key):
        if key[0] == "e":
            return self.sem[key[1]]
        if key[0] == "s":
            return self.sw_sems[key[1]]
        return self.dma_sems[key[1]]

    def dma_gather(self, out, in_, idx_ap, element_offset=0, r=(), w=()):
        q = "pool"
        for k, v in self._deps(q, r, w).items():
            self._wait(q, (k, v))
        i = self.sw_rr
        self.sw_rr = (self.sw_rr + 1) % N_SW_SEMS
        if self.sw_cnt[i] > 0:
            self._wait(q, (("s", i), 16 * self.sw_cnt[i]))
        ins = self.eng[q].indirect_dma_start(out, None, in_, bass.IndirectOffsetOnAxis(idx_ap, 0),
                                             element_offset=element_offset)
        self.sw_cnt[i] += 1
        ins.then_inc(self.sw_sems[i], 16)
        tok = (("s", i), 16 * self.sw_cnt[i])
        self._record(tok, r, w)
        self.nops += 1
        return tok

    def dma_scatter_add(self, out, idx_ap, in_, element_offset=0, r=(), w=()):
        q = "pool"
        for k, v in self._deps(q, r, w).items():
            self._wait(q, (k, v))
        i = self.sw_rr
        self.sw_rr = (self.sw_rr + 1) % N_SW_SEMS
        if self.sw_cnt[i] > 0:
            self._wait(q, (("s", i), 16 * self.sw_cnt[i]))
        ins = self.eng[q].indirect_dma_start(out, bass.IndirectOffsetOnAxis(idx_ap, 0), in_, None,
                                             element_offset=element_offset, compute_op=ALU.add)
        self.sw_cnt[i] += 1
        ins.then_inc(self.sw_sems[i], 16)
        tok = (("s", i), 16 * self.sw_cnt[i])
        self._record(tok, r, w)
        self.nops += 1
        return tok

    def dma_sw(self, slot, out, in_, r=(), w=(), **kw):
        q = "pool"
        for k, v in self._deps(q, r, w).items():
            self._wait(q, (k, v))
        i = self.sw_rr
        self.sw_rr = (self.sw_rr + 1) % N_SW_SEMS
        if self.sw_cnt[i] > 0:
            self._wait(q, (("s", i), 16 * self.sw_cnt[i]))
        ins = self.eng[q].dma_start(out=out, in_=in_, **kw)
        self.sw_cnt[i] += 1
        ins.then_inc(self.sw_sems[i], 16)
        tok = (("s", i), 16 * self.sw_cnt[i])
        self._record(tok, r, w)
        self.nops += 1
        return tok

    def _wait(self, e, tok):
        key, val = tok
        if self.known[e].get(key, 0) >= val:
            return
        self.eng[e].wait_ge(self._semh(key), val)
        self.known[e][key] = val
        self.nwaits += 1

    def _deps(self, e, r, w):
        deps = {}

        def add(tok):
            if tok is None:
                return
            k, v = tok
            if k == ("e", "pe") and e == "pe":
                return
            if deps.get(k, 0) < v:
                deps[k] = v
        for k in r:
            st = self.res.get(k)
            if st:
                add(st[0])
        for k in w:
            st = self.res.get(k)
            if st:
                add(st[0])
                for t in st[1]:
                    add(t)
        return deps

    def _record(self, tok, r, w):
        for k in w:
            self.res[k] = [tok, []]
        for k in r:
            st = self.res.setdefault(k, [None, []])
            lst = st[1]
            for i, (kk, vv) in enumerate(lst):
                if kk == tok[0]:
                    if vv < tok[1]:
                        lst[i] = tok
                    break
            else:
                lst.append(tok)

    def op(self, e, fn, r=(), w=()):
        psr = [k for k in r if k.startswith("ps")]
        if psr:
            r = [k for k in r if not k.startswith("ps")]
            w = list(w) + psr
        for k, v in self._deps(e, r, w).items():
            self._wait(e, (k, v))
        ins = fn(self.eng[e])
        self.cnt[e] += 1
        ins.then_inc(self.sem[e], 1)
        tok = (("e", e), self.cnt[e])
        self._record(tok, r, w)
        self.nops += 1
        return tok

    def dma(self, q, out, in_, r=(), w=(), **kw):
        for k, v in self._deps(q, r, w).items():
            self._wait(q, (k, v))
        i = self.dma_rr
        self.dma_rr = (self.dma_rr + 1) % N_DMA_SEMS
        if self.dma_cnt[i] > 0:
            self._wait(q, (("d", i), 16 * self.dma_cnt[i]))
        ins = self.eng[q].dma_start(out=out, in_=in_, **kw)
        self.dma_cnt[i] += 1
        ins.then_inc(self.dma_sems[i], 16)
        tok = (("d", i), 16 * self.dma_cnt[i])
        self._record(tok, r, w)
        self.nops += 1
        return tok

    def _bump(self, q):
        if self.cnt[q] > 0:
            self._wait(q, (("e", q), self.cnt[q]))
        ins = self.eng[q].nop()
        self.cnt[q] += 1
        ins.then_inc(self.sem[q], 1)

    def _all_wait_all(self):
        for e in self.eng:
            for e2 in self.eng:
                if self.cnt[e2] > 0:
                    self._wait(e, (("e", e2), self.cnt[e2]))

    def barrier(self):
        for i in range(N_DMA_SEMS):
            if self.dma_cnt[i] > 0:
                self._wait("sp", (("d", i), 16 * self.dma_cnt[i]))
        for i in range(N_SW_SEMS):
            if self.sw_cnt[i] > 0:
                self._wait("pool", (("s", i), 16 * self.sw_cnt[i]))
        self._bump("sp")
        self._bump("pool")
        self._all_wait_all()
        self.res = {}


def _host_consts():
    c = {}
    t = np.arange(L)
    row = (t // 64).astype(np.float32)
    col = (t % 64).astype(np.float32)
    inv = (10000.0 ** (-np.arange(16, dtype=np.float32) / 16)).astype(np.float32)
    ang = np.concatenate([row[:, None] * inv, col[:, None] * inv], axis=-1).astype(np.float32)
    cos, sin = np.cos(ang).astype(np.float32), np.sin(ang).astype(np.float32)
    CC = np.concatenate([cos, cos], -1)
    SS = np.concatenate([-sin, sin], -1)
    tab = np.stack([CC, SS, 0.125 * CC, 0.125 * SS], 1)
    c["cs_tab"] = np.ascontiguousarray(tab.reshape(16, 128, 4, 64).transpose(1, 0, 2, 3)).astype(np.float32)
    k = np.arange(128)[:, None].astype(np.float32)
    q = np.arange(128)[None, :].astype(np.float32)
    retc = np.stack([np.maximum(q - k, 0), (q >= k).astype(np.float32),
                     np.maximum(k - q, 0), (k > q).astype(np.float32)], 1)
    c["retc"] = np.ascontiguousarray(retc).astype(np.float32)
    p = np.arange(128, dtype=np.float32)
    c["posv"] = np.stack([p + 1, 127 - p, 128 - p, p], 1).astype(np.float32)
    qr = np.stack([np.arange(128) + 1.0, 128.0 - np.arange(128)], 0)
    c["qrow"] = np.ascontiguousarray(np.broadcast_to(qr[None], (128, 2, 128))).astype(np.float32)
    c["swamask"] = np.ascontiguousarray(np.stack([(k >= q), (k <= q)], 1)).astype(np.float32)
    return c


NA_CLS_TILES = {0: [0, 1, 2, 3], 1: [0, 1, 2, 3], 2: None, 3: [12, 13, 14, 15], 4: [12, 13, 14, 15]}
NA_CLS_REP = {0: 0, 1: 1, 2: 2, 3: 14, 4: 15}


def na_cls(m):
    if m <= 1:
        return m
    if m >= 14:
        return m - 11
    return 2


def na_tiles(m):
    cl = na_cls(m)
    if cl == 2:
        return [m - 2, m - 1, m, m + 1, m + 2]
    return NA_CLS_TILES[cl]


def _na_bias_layout(rpb):
    out = np.full((DEPTH, 8, 128, 5, 5, 128), NEG, np.float32)
    a = np.arange(128) // 64
    cc = np.arange(128) % 64
    for cl in range(5):
        m = NA_CLS_REP[cl]
        tiles = na_tiles(m)
        qr = 2 * m + a
        qc = cc
        bs = np.clip(qr - 4, 0, 24)
        cs = np.clip(qc - 8, 0, 48)
        for j, kt in enumerate(tiles):
            kr = 2 * kt + a
            kc = cc
            inband = (kr[:, None] >= bs[None, :]) & (kr[:, None] < bs[None, :] + 8)
            colok = (kc[:, None] >= cs[None, :]) & (kc[:, None] < cs[None, :] + 16)
            dr = np.clip(kr[:, None] - qr[None, :] + 7, 0, 14)
            dc = np.clip(kc[:, None] - qc[None, :], -15, 15) + 15
            g = rpb[:, :, dr, dc]
            out[:, :, :, cl, j, :] = np.where((inband & colok)[None, None], g, np.float32(NEG))
    return out.reshape(DEPTH, 8, 128, 25 * 128)


def build_program(cfg=None):
    cfg = cfg or {}
    _uid = [0]

    def U(n):
        _uid[0] += 1
        return f"{n}_{_uid[0]}"
    dbg = cfg.get("debug", False)
    stop_after = cfg.get("stop_after", None)
    nlayers = cfg.get("nlayers", DEPTH)
    nc = bass.Bass("TRN2", target_bir_lowering=False)
    P = Prog(nc)

    def din(name, shape, dt=F32):
        if name in cfg.get("shrink", ()):
            shape = [1] * len(shape)
        return nc.dram_tensor(name, list(shape), dt, kind="ExternalInput").ap()

    def dscr(name, shape, dt, out=False):
        return nc.dram_tensor(name, list(shape), dt,
                              kind="ExternalOutput" if (out and dbg) else "Internal").ap()

    x_in = din("x", [L, D]); ctx_in = din("ctx", [T, D]); c_t = din("c_t", [128, 16, 2])
    w_ada = din("w_ada", [DEPTH, D, 6 * D]); bada_t = din("bada_t", [DEPTH, 128, 96])
    nmix_t = din("nmix_t", [DEPTH, 128, 16]); nffn_t = din("nffn_t", [DEPTH, 128, 16]); nfin_t = din("nfin_t", [128, 16])
    w_in = din("w_in", [DEPTH, D, IN_COLS]); w_out = din("w_out", [DEPTH, D, D])
    decA = din("decA", [DEPTH, 16]); decB = din("decB", [DEPTH, 2, 8])
    convw_t = din("convw_t", [DEPTH, 128, 4, 3]); sink_in = din("sink", [DEPTH, 8])
    na_bias = din("na_bias", [DEPTH, 8, 128, 3200])
    w_router = din("w_router", [DEPTH, D, NE])
    w_gate = din("w_gate", [DEPTH, NE, D, FF]); w_up = din("w_up", [DEPTH, NE, D, FF]); w_down = din("w_down", [DEPTH, NE, FF, D])
    cs_tab = din("cs_tab", [128, 16, 4, 64]); retc_in = din("retc", [128, 4, 128]); posv_in = din("posv", [128, 4])
    qrow_in = din("qrow", [128, 2, 128]); swamask_in = din("swamask", [128, 2, 128])
    y_out = nc.dram_tensor("y", [L, D], F32, kind="ExternalOutput").ap()

    XT = dscr("XT", [D, NT], F32, out=True)
    ZT = dscr("ZT", [NT, ZT_COLS], BF16, out=True)
    ZF = dscr("ZF", [ZF_ROWS, NT], BF16, out=True)
    YT = dscr("YT", [D, NT], BF16, out=True)
    YE = dscr("YE", [NE, NSL, D], BF16, out=True)
    H2D = dscr("H2D", [NT, D], BF16)
    MOE = dscr("MOE", [NT, D], F32)
    ADAD = dscr("ADAD", [DEPTH, 128, 96, 2], F32, out=True)
    IDXC = dscr("IDXC", [CAPC, NE], F32)
    DBG1 = dscr("DBG1", [NE, 2 * CAP], F32, out=True)
    XTv = XT.rearrange("(c p) n -> p c n", p=128)
    YTv = YT.rearrange("(c p) n -> p c n", p=128)

    PS = nc.alloc_psum_tensor("ps", [128, 8, 512], F32).ap()

    def bank(i):
        return PS[:, i, :]

    def bkey(i):
        return f"ps{i}"

    identF = nc.alloc_sbuf_tensor("identF", [128, 128], F32).ap()
    identB = nc.alloc_sbuf_tensor("identB", [128, 128], BF16).ap()
    onesF = nc.alloc_sbuf_tensor("onesF", [128, 128], F32).ap()
    iotaF = nc.alloc_sbuf_tensor("iotaF", [128, 2048], F32).ap()
    piota = nc.alloc_sbuf_tensor("piota", [128, 16], F32).ap()
    ada_sb = nc.alloc_sbuf_tensor("ada_sb", [128, DEPTH, 96, 2], F32).ap()
    scl = nc.alloc_sbuf_tensor("scl", [128, DEPTH, 2, 16, 2], F32).ap()
    nrm_sb = nc.alloc_sbuf_tensor("nrm_sb", [128, DEPTH, 2, 16], F32).ap()
    nfin_sb = nc.alloc_sbuf_tensor("nfin_sb", [128, 16], F32).ap()
    tmpi = nc.alloc_sbuf_tensor("tmpi", [128, 128], F32).ap()

    P.op("pool", lambda e: e.iota(tmpi, pattern=[[1, 128]], base=0, channel_multiplier=-1,
                                  allow_small_or_imprecise_dtypes=True), w=["tmpi"])
    P.op("dve", lambda e: e.tensor_scalar(identF, tmpi, 0.0, None, ALU.is_equal), r=["tmpi"], w=["identF"])
    P.op("dve", lambda e: e.tensor_copy(identB, identF), r=["identF"], w=["identB"])
    P.op("dve", lambda e: e.memset(onesF, 1.0), w=["onesF"])
    P.op("pool", lambda e: e.iota(iotaF, pattern=[[1, 2048]], base=0, channel_multiplier=0,
                                  allow_small_or_imprecise_dtypes=True), w=["iotaF"])
    P.op("pool", lambda e: e.iota(piota, pattern=[[128, 16]], base=0, channel_multiplier=1,
                                  allow_small_or_imprecise_dtypes=True), w=["piota"])
    P.dma("sp", nrm_sb[:, :, 0, :], nmix_t.rearrange("l p c -> p l c"), w=["nrm_sb"])
    P.dma("sp", nrm_sb[:, :, 1, :], nffn_t.rearrange("l p c -> p l c"), w=["nrm_sb"])
    P.dma("sp", nfin_sb, nfin_t, w=["nfin_sb"])

    NW = 4
    evac_rr = [0]

    def evac_eng():
        evac_rr[0] ^= 1
        return "act" if evac_rr[0] else "dve"

    def copy_op(eng, out, in_, r, w, scale=None):
        if eng == "act":
            if scale is None:
                P.op("act", lambda e: e.activation(out, in_, AF.Copy), r=r, w=w)
            else:
                P.op("act", lambda e: e.activation(out, in_, AF.Copy, scale=scale), r=r, w=w)
        else:
            if scale is None:
                P.op(eng, lambda e: e.tensor_copy(out, in_), r=r, w=w)
            else:
                P.op(eng, lambda e: e.tensor_scalar(out, in_, scale, None, ALU.mult), r=r, w=w)

    class Ring:
        def __init__(self, es):
            self.t = es.enter_context(nc.sbuf_tensor(U("wring"), [128, NW, 8192], BF16)).ap()
            self.i = 0

        def load(self, src_ap, view):
            s = self.i
            self.i = (self.i + 1) % NW
            dst = view(self.t[:, s, :])
            P.dma_sw(s, dst, src_ap, w=[f"w{s}"])
            return dst, f"w{s}"

    def phase_ada():
        with ExitStack() as es:
            ring = Ring(es)
            sc = es.enter_context(nc.sbuf_tensor(U("sc"), [128, 16, 2], BF16)).ap()
            cin = es.enter_context(nc.sbuf_tensor(U("cin"), [128, 16, 2], F32)).ap()
            bada = es.enter_context(nc.sbuf_tensor(U("bada"), [128, DEPTH, 96], F32)).ap()
            P.dma("sp", cin, c_t, w=["cin"])
            P.dma("sp", bada, bada_t.rearrange("l p c -> p l c"), w=["bada"])
            P.op("act", lambda e: e.activation(sc, cin, AF.Silu), r=["cin"], w=["sc"])
            for l in range(nlayers):
                for fg in range(24):
                    wt, wk = ring.load(w_ada[l, :, fg * 512:(fg + 1) * 512].rearrange("(c p) n -> p c n", p=128),
                                       lambda t: t.rearrange("p (c n) -> p c n", c=16))
                    bi = fg % 2
                    for m in range(4):
                        for c in range(16):
                            P.op("pe", lambda e: e.matmul(bank(bi)[:, m * 2:m * 2 + 2], wt[:, c, m * 128:(m + 1) * 128],
                                                          sc[:, c, :], start=(c == 0), stop=(c == 15)),
                                 r=[wk, "sc"], w=[bkey(bi)])
                    P.op("dve", lambda e: e.tensor_tensor(
                        ada_sb[:, l, fg * 4:(fg + 1) * 4, :],
                        bank(bi)[:, 0:8].rearrange("p (m r) -> p m r", r=2),
                        bada[:, l, fg * 4:(fg + 1) * 4].unsqueeze(2).to_broadcast([128, 4, 2]), ALU.add),
                        r=[bkey(bi), "bada"], w=["ada_sb"])
                for wi, c0 in ((0, 16), (1, 64)):
                    P.op("dve", lambda e: e.scalar_tensor_tensor(
                        scl[:, l, wi, :, :], ada_sb[:, l, c0:c0 + 16, :], 1.0,
                        nrm_sb[:, l, wi, :].unsqueeze(2).to_broadcast([128, 16, 2]), ALU.add, ALU.mult),
                        r=["ada_sb", "nrm_sb"], w=["scl"])
            if dbg:
                P.dma("sp", ADAD.rearrange("l p c r -> p l c r")[:, 0:nlayers], ada_sb[:, 0:nlayers], r=["ada_sb"], w=["ADAD"])
            P.barrier()

    def ada_vec(l, which, r):
        return ada_sb[:, l, which * 16:(which + 1) * 16, r]

    def norm_group(bufs, xg, gsz, scale_ap, shift_ap, out_fn, ssb=7):
        sq, rstd, tcs = bufs["sq"], bufs["rstd"], bufs["tc"]
        for c in range(16):
            s = c % 3
            P.op("act", lambda e: e.activation(sq[:, s, 0:gsz], xg[:, c, 0:gsz], AF.Square), r=["xg"], w=[f"sq{s}"])
            P.op("pe", lambda e: e.matmul(bank(ssb)[:, 0:gsz], onesF, sq[:, s, 0:gsz], start=(c == 0), stop=(c == 15)),
                 r=[f"sq{s}", "onesF"], w=[bkey(ssb)])
        P.op("act", lambda e: e.activation(rstd[:, 0, 0:gsz], bank(ssb)[:, 0:gsz], AF.Sqrt, bias=bufs["eps"][:, 0:1], scale=1.0 / D),
             r=[bkey(ssb), "eps"], w=["rstd0"])
        P.op("dve", lambda e: e.reciprocal(rstd[:, 1, 0:gsz], rstd[:, 0, 0:gsz]), r=["rstd0"], w=["rstd1"])
        for c in range(16):
            s = c % 3
            P.op("dve", lambda e: e.scalar_tensor_tensor(tcs[:, s, 0:gsz], xg[:, c, 0:gsz], scale_ap[:, c:c + 1],
                                                         rstd[:, 1, 0:gsz], ALU.mult, ALU.mult),
                 r=["xg", "rstd1", "scl", "ada_sb", "nfin_sb"], w=[f"tc{s}"])
            out_fn(c, tcs[:, s, 0:gsz], f"tc{s}")

    def alloc_norm_bufs(es):
        b = {}
        b["sq"] = es.enter_context(nc.sbuf_tensor(U("sq"), [128, 3, 512], F32)).ap()
        b["rstd"] = es.enter_context(nc.sbuf_tensor(U("rstd"), [128, 2, 512], F32)).ap()
        b["tc"] = es.enter_context(nc.sbuf_tensor(U("tcs"), [128, 3, 512], F32)).ap()
        b["eps"] = es.enter_context(nc.sbuf_tensor(U("epsb"), [128, 1], F32)).ap()
        P.op("dve", lambda e: e.memset(b["eps"], EPS), w=["eps"])
        return b

    def phase_norm1(l, hT, es_outer):
        with ExitStack() as es:
            xg = es.enter_context(nc.sbuf_tensor(U("xg"), [128, 16, 512], F32)).ap()
            nb = alloc_norm_bufs(es)
            xin = None
            if l == 0:
                xin = es.enter_context(nc.sbuf_tensor(U("xin"), [128, 2, 2048], F32)).ap()
            for gi, (g0, gsz) in enumerate(GROUPS):
                r = 1 if g0 >= L else 0
                if l == 0:
                    ntile = gsz // 128
                    for tt in range(ntile):
                        n0 = g0 + tt * 128
                        s = tt % 2
                        src = x_in[n0:n0 + 128, :] if n0 < L else ctx_in[n0 - L:n0 - L + 128, :]
                        P.dma("sp", xin[:, s, :], src, w=[f"xin{s}"])
                        for cb in range(4):
                            for cc in range(4):
                                c = cb * 4 + cc
                                P.op("pe", lambda e: e.transpose(bank(cb)[:, cc * 128:(cc + 1) * 128],
                                                                 xin[:, s, c * 128:(c + 1) * 128], identF),
                                     r=[f"xin{s}", "identF"], w=[bkey(cb)])
                            copy_op(evac_eng(), xg[:, cb * 4:(cb + 1) * 4, tt * 128:(tt + 1) * 128],
                                    bank(cb).rearrange("p (c n) -> p c n", c=4), r=[bkey(cb)], w=["xg"])
                    P.dma("sp", XTv[:, :, g0:g0 + gsz], xg[:, :, 0:gsz], r=["xg"], w=["XT"])
                else:
                    P.dma("sp", xg[:, :, 0:gsz], XTv[:, :, g0:g0 + gsz], r=["XT"], w=["xg"])

                def out_fn(c, tc, key, r=r, g0=g0, gsz=gsz):
                    P.op("act", lambda e: e.activation(hT[:, c, g0:g0 + gsz], tc, AF.Identity,
                                                       bias=ada_vec(l, 0, r)[:, c:c + 1], scale=1.0),
                         r=[key, "ada_sb"], w=["hT"])
                norm_group(nb, xg, gsz, scl[:, l, 0, :, r], None, out_fn)
            P.barrier()

    def phase_inproj(l, hT):
        with ExitStack() as es:
            ring = Ring(es)
            cs = es.enter_context(nc.sbuf_tensor(U("cs"), [128, 16, 4, 64], F32)).ap()
            zst = es.enter_context(nc.sbuf_tensor(U("zst"), [128, 3, 512], BF16)).ap()
            rt = es.enter_context(nc.sbuf_tensor(U("rt"), [128, 2, 2, 512], F32)).ap()
            P.dma("sp", cs, cs_tab, w=["cs"])
            zi = [0]
            bi = [0]
            tiles = [
                (0, 512, "T", ZT_RQ, "rope"), (512, 512, "T", ZT_RK, "ropek"),
                (1024, 512, "T", ZT_RV, "copy"), (1536, 512, "T", ZT_RG, "silu"),
                (2048, 512, "F", ZF_CB, "copy"), (2560, 512, "F", ZF_CC, "copy"), (3072, 512, "F", ZF_CH, "copy"),
                (3584, 512, "T", ZT_SQ, "ropeq_swa"), (4096, 256, "T", ZT_SK, "swakv"),
                (4352, 512, "F", ZF_NQ, "copy"), (4864, 512, "F", ZF_NK, "copy"),
                (5376, 512, "T", ZT_NV, "copy"),
            ]
            only = cfg.get("inproj_tiles")
            for ti, (c0, ncol, orient, doff, kind) in enumerate(tiles):
                if only is not None and ti not in only:
                    continue
                wt, wk = ring.load(w_in[l, :, c0:c0 + ncol].rearrange("(c p) n -> p c n", p=128),
                                   lambda t: t[:, 0:16 * ncol].rearrange("p (c n) -> p c n", c=16))
                if orient == "T":
                    for tt in range(18):
                        b = bi[0] = (bi[0] + 1) % 6
                        for c in range(16):
                            P.op("pe", lambda e: e.matmul(bank(b)[:, 0:ncol], hT[:, c, tt * 128:(tt + 1) * 128], wt[:, c, :],
                                                          start=(c == 0), stop=(c == 15)),
                                 r=["hT", wk], w=[bkey(b)])
                        zs = zi[0] = (zi[0] + 1) % 3
                        zk = f"zst{zs}"
                        zo = zst[:, zs, 0:ncol]
                        ps = bank(b)[:, 0:ncol]
                        lat = tt < 16

                        def rope(ps_ap, out_ap, nh, tab, perm=False):
                            rs = tt % 2
                            x4 = ps_ap.rearrange("p (h t d) -> p h t d", t=2, d=32)
                            a = rt[:, rs, 0, 0:nh * 64]
                            bb = rt[:, rs, 1, 0:nh * 64]
                            a3 = a.rearrange("p (h d) -> p h d", d=64)
                            b4 = bb.rearrange("p (h t d) -> p h t d", t=2, d=32)
                            ccb = cs[:, tt, tab, :].unsqueeze(1).to_broadcast([128, nh, 64])
                            ssn = cs[:, tt, tab + 1, 0:32].unsqueeze(1).to_broadcast([128, nh, 32])
                            ssp = cs[:, tt, tab + 1, 32:64].unsqueeze(1).to_broadcast([128, nh, 32])
                            P.op("dve", lambda e: e.tensor_tensor(a3, ps_ap.rearrange("p (h d) -> p h d", d=64), ccb, ALU.mult),
                                 r=[bkey(b), "cs"], w=[f"rta{rs}"])
                            P.op("dve", lambda e: e.tensor_tensor(b4[:, :, 0, :], x4[:, :, 1, :], ssn, ALU.mult),
                                 r=[bkey(b), "cs"], w=[f"rtb{rs}"])
                            P.op("dve", lambda e: e.tensor_tensor(b4[:, :, 1, :], x4[:, :, 0, :], ssp, ALU.mult),
                                 r=[bkey(b), "cs"], w=[f"rtb{rs}"])
                            if perm:
                                o = out_ap.rearrange("p (i g d) -> p g i d", g=2, d=64)
                                P.op("pool", lambda e: e.tensor_tensor(o, a.rearrange("p (g i d) -> p g i d", g=2, d=64),
                                                                       bb.rearrange("p (g i d) -> p g i d", g=2, d=64), ALU.add),
                                     r=[f"rta{rs}", f"rtb{rs}"], w=[zk])
                            else:
                                P.op("pool", lambda e: e.tensor_tensor(out_ap, a, bb, ALU.add),
                                     r=[f"rta{rs}", f"rtb{rs}"], w=[zk])

                        if kind == "copy":
                            copy_op(evac_eng(), zo, ps, r=[bkey(b)], w=[zk])
                        elif kind == "silu":
                            P.op("act", lambda e: e.activation(zo, ps, AF.Silu), r=[bkey(b)], w=[zk])
                        elif kind == "rope":
                            if lat:
                                rope(ps, zo, 8, 0)
                            else:
                                copy_op("act", zo, ps, r=[bkey(b)], w=[zk])
                        elif kind == "ropek":
                            if lat:
                                rope(ps, zo, 8, 2)
                            else:
                                copy_op("act", zo, ps, r=[bkey(b)], w=[zk], scale=0.125)
                        elif kind == "ropeq_swa":
                            if lat:
                                rope(ps, zo, 8, 0, perm=True)
                            else:
                                P.op("act", lambda e: e.activation(zo.rearrange("p (i g d) -> p g i d", g=2, d=64),
                                                                   ps.rearrange("p (g i d) -> p g i d", g=2, d=64), AF.Copy),
                                     r=[bkey(b)], w=[zk])
                        elif kind == "swakv":
                            if lat:
                                rope(ps[:, 0:128], zo[:, 0:128], 2, 0)
                            else:
                                copy_op("act", zo[:, 0:128], ps[:, 0:128], r=[bkey(b)], w=[zk])
                            copy_op("dve", zo[:, 128:256], ps[:, 128:256], r=[bkey(b)], w=[zk])
                        P.dma("sp", ZT[tt * 128:(tt + 1) * 128, doff:doff + ncol], zo, r=[zk], w=["ZT"])
                else:
                    for m in range(ncol // 128):
                        for (g0, gsz) in GROUPS:
                            b = bi[0] = (bi[0] + 1) % 6
                            for c in range(16):
                                P.op("pe", lambda e: e.matmul(bank(b)[:, 0:gsz], wt[:, c, m * 128:(m + 1) * 128], hT[:, c, g0:g0 + gsz],
                                                              start=(c == 0), stop=(c == 15)),
                                     r=["hT", wk], w=[bkey(b)])
                            zs = zi[0] = (zi[0] + 1) % 3
                            zk = f"zst{zs}"
                            copy_op(evac_eng(), zst[:, zs, 0:gsz], bank(b)[:, 0:gsz], r=[bkey(b)], w=[zk])
                            P.dma("sp", ZF[doff + m * 128:doff + (m + 1) * 128, g0:g0 + gsz], zst[:, zs, 0:gsz], r=[zk], w=["ZF"])
            P.barrier()

    def phase_ret(l):
        need_ctx = l < DEPTH - 1
        with ExitStack() as es:
            def sb(name, shape, dt=F32):
                return es.enter_context(nc.sbuf_tensor(U(name), shape, dt)).ap()
            lgA = sb("lgA", [128, 16]); lgP = sb("lgP", [128, 8]); tA = sb("tA", [128, 16]); tP = sb("tP", [128, 8])
            retc = sb("retc", [128, 4, 128]); posv = sb("posv", [128, 4]); qrow = sb("qrow", [128, 2, 128])
            zeta = sb("zeta", [128, 2, 8]); cdP = sb("cdP", [128, 8]); xiT = sb("xiT", [128, 2, 4, 128])
            dm = sb("dm", [128, 8, 128]); dtmp = sb("dtmp", [128, 2, 128])
            epsb = sb("epsb", [128, 1])
            SfAll = sb("SfAll", [128, 18, 512], BF16)
            stf = sb("stf", [128, 512]); stb = sb("stb", [128, 512]); stb_bf = sb("stb_bf", [128, 512], BF16)
            kvin = sb("kvin", [128, 2, 1024], BF16); zin = sb("zin", [128, 2, 2048], BF16)
            kz = sb("kz", [128, 2, 512], BF16)
            qTe = sb("qTe", [128, 2, 512], BF16); qTo = sb("qTo", [128, 2, 512], BF16); kT = sb("kT", [128, 2, 512], BF16)
            bmask = sb("bmask", [128, 512]); tkv = sb("tkv", [128, 512])
            qxf = sb("qxf", [128, 2, 512], BF16); qxb = sb("qxb", [128, 2, 512], BF16)
            inn = sb("inn", [128, 2, 1024], BF16)
            sqv = sb("sqv", [128, 512]); t1 = sb("t1", [128, 512]); t2 = sb("t2", [128, 512])
            st8 = sb("st8", [128, 8, 8])
            yr = sb("yr", [128, 2, 512], BF16); yst = sb("yst", [128, 2, 4, 512], BF16)
            P.op("dve", lambda e: e.memset(epsb, EPS), w=["epsb"])
            P.dma("sp", retc, retc_in, w=["retc"]); P.dma("sp", posv, posv_in, w=["posv"]); P.dma("sp", qrow, qrow_in, w=["qrow"])
            P.dma("sp", tA, decA[l:l + 1, :].partition_broadcast(128), w=["tA"])
            P.dma("sp", tP[0:64, :], decB[l, 0:1, :].partition_broadcast(64), w=["tP"])
            P.dma("sp", tP[64:128, :], decB[l, 1:2, :].partition_broadcast(64), w=["tP"])
            for (src, dst, k1, k2) in ((tA, lgA, "tA", "lgA"), (tP, lgP, "tP", "lgP")):
                P.op("act", lambda e: e.activation(src, src, AF.Exp, scale=-1.0), r=[k1], w=[k1])
                P.op("dve", lambda e: e.tensor_scalar(src, src, 1.0, None, ALU.add), r=[k1], w=[k1])
                P.op("act", lambda e: e.activation(src, src, AF.Ln), r=[k1], w=[k1])
                P.op("dve", lambda e: e.tensor_scalar(dst, src, -1.0, None, ALU.mult), r=[k1], w=[k2])
            P.op("act", lambda e: e.activation(zeta[:, 0, :], lgA[:, 0:8], AF.Exp, scale=posv[:, 1:2]), r=["lgA", "posv"], w=["zeta"])
            P.op("act", lambda e: e.activation(zeta[:, 1, :], lgA[:, 8:16], AF.Exp, scale=posv[:, 3:4]), r=["lgA", "posv"], w=["zeta"])
            P.op("act", lambda e: e.activation(cdP, lgP, AF.Exp, scale=128.0), r=["lgP"], w=["cdP"])
            for dr in range(2):
                for j in range(4):
                    P.op("act", lambda e: e.activation(xiT[:, dr, j, :], qrow[:, dr, :], AF.Exp, scale=lgP[:, dr * 4 + j:dr * 4 + j + 1]),
                         r=["lgP", "qrow"], w=["xiT"])
            for h in range(8):
                P.op("act", lambda e: e.activation(dtmp[:, 0, :], retc[:, 0, :], AF.Exp, scale=lgA[:, h:h + 1]), r=["lgA", "retc"], w=["dtmp0"])
                P.op("act", lambda e: e.activation(dtmp[:, 1, :], retc[:, 2, :], AF.Exp, scale=lgA[:, 8 + h:9 + h]), r=["lgA", "retc"], w=["dtmp1"])
                P.op("dve", lambda e: e.tensor_tensor(dtmp[:, 0, :], dtmp[:, 0, :], retc[:, 1, :], ALU.mult), r=["dtmp0", "retc"], w=["dtmp0"])
                P.op("dve", lambda e: e.tensor_tensor(dtmp[:, 1, :], dtmp[:, 1, :], retc[:, 3, :], ALU.mult), r=["dtmp1", "retc"], w=["dtmp1"])
                P.op("dve", lambda e: e.tensor_tensor(dm[:, h, :], dtmp[:, 0, :], dtmp[:, 1, :], ALU.add), r=["dtmp0", "dtmp1"], w=["dm"])
            P.op("pool", lambda e: e.memset(qTe, 0.0), w=["qTe0", "qTe1"])
            P.op("pool", lambda e: e.memset(qTo, 0.0), w=["qTo0", "qTo1"])
            bm4 = bmask.rearrange("p (j t d) -> p j t d", t=2, d=64)
            P.op("pool", lambda e: e.memset(bmask, 0.0), w=["bmask"])
            P.op("pool", lambda e: e.memset(bm4[0:64, :, 0, :], 1.0), r=["bmask"], w=["bmask"])
            P.op("pool", lambda e: e.memset(bm4[64:128, :, 1, :], 1.0), r=["bmask"], w=["bmask"])
            P.op("dve", lambda e: e.memset(stf, 0.0), w=["stf"])
            P.op("dve", lambda e: e.memset(stb, 0.0), w=["stb"])
            P.op("dve", lambda e: e.memset(stb_bf, 0.0), w=["stb_bf"])

            def kv_update(i, dr, kin, kk, st, stk, s):
                P.op("dve", lambda e: e.tensor_tensor(kz[:, s, :].rearrange("p (h d) -> p h d", d=64),
                                                      kin[:, 0:512].rearrange("p (h d) -> p h d", d=64),
                                                      zeta[:, dr, :].unsqueeze(2).to_broadcast([128, 8, 64]), ALU.mult),
                     r=[kk, "zeta"], w=[f"kz{s}"])
                for j in range(4):
                    P.op("pe", lambda e: e.matmul(bank(4)[:, j * 128:(j + 1) * 128], kz[:, s, j * 128:(j + 1) * 128],
                                                  kin[:, 512 + j * 128:512 + (j + 1) * 128], start=True, stop=True),
                         r=[f"kz{s}", kk], w=[bkey(4)])
                P.op("dve", lambda e: e.tensor_tensor(st.rearrange("p (j v) -> p j v", v=128), st.rearrange("p (j v) -> p j v", v=128),
                                                      cdP[:, dr * 4:dr * 4 + 4].unsqueeze(2).to_broadcast([128, 4, 128]), ALU.mult),
                     r=[stk, "cdP"], w=[stk])
                P.op("dve", lambda e: e.tensor_tensor(tkv, bank(4), bmask, ALU.mult), r=[bkey(4), "bmask"], w=["tkv"])
                P.op("dve", lambda e: e.tensor_tensor(st, st, tkv, ALU.add), r=[stk, "tkv"], w=[stk])

            cut = cfg.get("ret_cut", 99)
            fwd_order = [16, 17] + list(range(16)) if cut >= 1 else []
            bwd_order = [17, 16] + list(range(15, -1, -1)) if cut >= 2 else []
            for n, i in enumerate(fwd_order):
                s = n % 2
                P.dma("sp", kvin[:, s, :], ZT[i * 128:(i + 1) * 128, ZT_RK:ZT_RK + 1024], r=["ZT"], w=[f"kvin{s}"])
                P.op("act", lambda e: e.activation(SfAll[:, i, :], stf, AF.Copy), r=["stf"], w=["SfAll"])
                if n < len(fwd_order) - 1:
                    kv_update(i, 0, kvin[:, s, :], f"kvin{s}", stf, "stf", s)
            for n, i in enumerate(bwd_order):
                s = n % 2
                zk = f"zin{s}"
                P.dma("sp", zin[:, s, :], ZT[i * 128:(i + 1) * 128, 0:2048], r=["ZT"], w=[zk])
                need_out = (i < 16) or need_ctx
                if cut < 2.05:
                    continue
                if need_out:
                    yb = 3 if s == 0 else 7
                    TPq, TPk = bank(0), bank(6)
                    for j in range(4):
                        P.op("pe", lambda e: e.matmul(TPq[:, j * 128:(j + 1) * 128], zin[:, s, j * 128:(j + 1) * 128], identB, start=True, stop=True),
                             r=[zk, "identB"], w=[bkey(0)])
                        P.op("pe", lambda e: e.matmul(TPk[:, j * 128:(j + 1) * 128], zin[:, s, 512 + j * 128:512 + (j + 1) * 128], identB, start=True, stop=True),
                             r=[zk, "identB"], w=[bkey(6)])
                    if cut < 2.15:
                        continue
                    P.op("act", lambda e: e.activation(qTe[0:64, s, :], TPq[0:64, :], AF.Copy), r=[bkey(0)], w=[f"qTe{s}"])
                    P.op("act", lambda e: e.activation(qTo[64:128, s, :], TPq[64:128, :], AF.Copy), r=[bkey(0)], w=[f"qTo{s}"])
                    P.op("act", lambda e: e.activation(kT[:, s, :], TPk, AF.Copy), r=[bkey(6)], w=[f"kT{s}"])
                    if cut < 2.25:
                        continue
                    rv = cfg.get("ret_var", 0)
                    if rv == 1:
                        P.op("dve", lambda e: e.tensor_tensor(qxf[:, s, :], TPq, bmask, ALU.mult),
                             r=[bkey(0), "bmask"], w=[f"qxf{s}"])
                    elif rv == 2:
                        P.op("dve", lambda e: e.tensor_copy(qxf[:, s, :], TPq), r=[bkey(0)], w=[f"qxf{s}"])
                    elif rv == 3:
                        P.op("dve", lambda e: e.tensor_tensor(qxf[:, s, :], qTe[:, s, :], xiT[:, 0, :, :].rearrange("p j q -> p (j q)"), ALU.mult),
                             r=[f"qTe{s}", "xiT"], w=[f"qxf{s}"])
                    else:
                        P.op("dve", lambda e: e.tensor_tensor(qxf[:, s, :], TPq, xiT[:, 0, :, :].rearrange("p j q -> p (j q)"), ALU.mult),
                             r=[bkey(0), "xiT"], w=[f"qxf{s}"])
                        P.op("dve", lambda e: e.tensor_tensor(qxb[:, s, :], TPq, xiT[:, 1, :, :].rearrange("p j q -> p (j q)"), ALU.mult),
                             r=[bkey(0), "xiT"], w=[f"qxb{s}"])
                    if cut < 3:
                        continue
                    for h in range(8):
                        j, hf = h // 2, h % 2
                        rows = slice(hf * 64, hf * 64 + 64)
                        sb_ = 1 + h // 4
                        qz = qTe if hf == 0 else qTo
                        P.op("pe", lambda e: e.matmul(bank(sb_)[:, (h % 4) * 128:(h % 4 + 1) * 128], kT[:, s, j * 128:(j + 1) * 128],
                                                      qz[:, s, j * 128:(j + 1) * 128], start=True, stop=True),
                             r=[f"kT{s}", f"qTe{s}", f"qTo{s}"], w=[bkey(sb_)])
                    for hb in range(2):
                        P.op("dve", lambda e: e.tensor_tensor(inn[:, s, hb * 512:(hb + 1) * 512], bank(1 + hb),
                                                              dm[:, hb * 4:(hb + 1) * 4, :].rearrange("p h q -> p (h q)"), ALU.mult),
                             r=[bkey(1 + hb), "dm"], w=[f"inn{s}"])
                    if cut < 4:
                        continue
                    for j in range(4):
                        pc = slice(j * 128, (j + 1) * 128)
                        P.op("pe", lambda e: e.matmul(bank(yb)[:, pc], qxf[:, s, pc], SfAll[:, i, pc], start=True, stop=False),
                             r=[f"qxf{s}", "SfAll"], w=[bkey(yb)])
                        P.op("pe", lambda e: e.matmul(bank(yb)[:, pc], qxb[:, s, pc], stb_bf[:, pc], start=False, stop=False),
                             r=[f"qxb{s}", "stb_bf"], w=[bkey(yb)])
                        for hf in range(2):
                            h = 2 * j + hf
                            P.op("pe", lambda e: e.matmul(bank(yb)[:, h * 64:(h + 1) * 64], inn[:, s, h * 128:(h + 1) * 128],
                                                          zin[:, s, 1024 + h * 64:1024 + (h + 1) * 64], start=False, stop=(hf == 1)),
                                 r=[f"inn{s}", zk], w=[bkey(yb)])
                    if cut < 5:
                        continue
                    Yv = bank(yb).rearrange("p (h d) -> p h d", d=64)
                    s1, s2, mean, msq, var, sd, rstd = [st8[:, k, :] for k in range(7)]
                    P.op("dve", lambda e: e.tensor_reduce(s1, Yv, AX.X, ALU.add), r=[bkey(yb)], w=["st_s1"])
                    P.op("act", lambda e: e.activation(sqv, bank(yb), AF.Square), r=[bkey(yb)], w=["sqv"])
                    P.op("dve", lambda e: e.tensor_reduce(s2, sqv.rearrange("p (h d) -> p h d", d=64), AX.X, ALU.add), r=["sqv"], w=["st_s2"])
                    P.op("dve", lambda e: e.tensor_scalar(mean, s1, 1.0 / 64, None, ALU.mult), r=["st_s1"], w=["st_mean"])
                    P.op("dve", lambda e: e.tensor_tensor(msq, mean, mean, ALU.mult), r=["st_mean"], w=["st_msq"])
                    P.op("dve", lambda e: e.scalar_tensor_tensor(var, s2, 1.0 / 64, msq, ALU.mult, ALU.subtract), r=["st_s2", "st_msq"], w=["st_var"])
                    P.op("act", lambda e: e.activation(sd, var, AF.Sqrt, bias=epsb[:, 0:1], scale=1.0), r=["st_var", "epsb"], w=["st_sd"])
                    P.op("dve", lambda e: e.reciprocal(rstd, sd), r=["st_sd"], w=["st_rstd"])
                    t1v = t1.rearrange("p (h d) -> p h d", d=64)
                    t2v = t2.rearrange("p (h d) -> p h d", d=64)
                    P.op("dve", lambda e: e.tensor_tensor(t1v, Yv, mean.unsqueeze(2).to_broadcast([128, 8, 64]), ALU.subtract),
                         r=[bkey(yb), "st_mean"], w=["t1"])
                    P.op("dve", lambda e: e.tensor_tensor(t2v, t1v, rstd.unsqueeze(2).to_broadcast([128, 8, 64]), ALU.mult),
                         r=["t1", "st_rstd"], w=["t2"])
                    P.op("pool", lambda e: e.tensor_tensor(yr[:, s, :], t2, zin[:, s, 1536:2048], ALU.mult), r=["t2", zk], w=[f"yr{s}"])
                    if cut < 6:
                        continue
                    TY = bank(5)
                    for j in range(4):
                        P.op("pe", lambda e: e.matmul(TY[:, j * 128:(j + 1) * 128], yr[:, s, j * 128:(j + 1) * 128], identB, start=True, stop=True),
                             r=[f"yr{s}", "identB"], w=[bkey(5)])
                    if i < 16:
                        grp, pos, gw = i // 4, i % 4, 512
                        g0 = grp * 512
                    else:
                        grp, pos, gw = 4, i - 16, 256
                        g0 = 2048
                    ys = grp % 2
                    P.op("act", lambda e: e.activation(yst[:, ys, :, pos * 128:(pos + 1) * 128], TY[:, 0:512].rearrange("p (j n) -> p j n", j=4), AF.Copy),
                         r=[bkey(5)], w=[f"yst{ys}"])
                    if pos == 0:
                        P.dma("sp", YTv[:, 0:4, g0:g0 + gw], yst[:, ys, :, 0:gw], r=[f"yst{ys}"], w=["YT"])
                if n < len(bwd_order) - 1:
                    kv_update(i, 1, zin[:, s, 512:1536], zk, stb, "stb", s)
                    P.op("act", lambda e: e.activation(stb_bf, stb, AF.Copy), r=["stb"], w=["stb_bf"])
            P.barrier()

    def phase_conv(l):
        need_ctx = l < DEPTH - 1
        with ExitStack() as es:
            def sb(name, shape, dt=F32):
                return es.enter_context(nc.sbuf_tensor(U(name), shape, dt)).ap()
            cw = sb("cw", [128, 4, 3])
            bch = sb("bch", [128, 2, 3, NT], BF16)
            u = sb("u", [128, 2, NT]); yv = sb("yv", [128, 2, NT]); ob = sb("ob", [128, 2, NT], BF16)
            P.dma("sp", cw, convw_t[l], w=["cw"])
            nend = NT if need_ctx else L
            seqs = [(0, L)] + ([(L, NT)] if need_ctx else [])
            for cc in range(4):
                s = cc % 2
                for k3, off in enumerate((ZF_CB, ZF_CC, ZF_CH)):
                    P.dma("sp", bch[:, s, k3, 0:nend], ZF[off + cc * 128:off + (cc + 1) * 128, 0:nend], r=["ZF"], w=[f"bch{s}"])
                P.op("dve", lambda e: e.tensor_tensor(u[:, s, 0:nend], bch[:, s, 1, 0:nend], bch[:, s, 2, 0:nend], ALU.mult), r=[f"bch{s}"], w=[f"u{s}"])
                P.op("act", lambda e: e.activation(yv[:, s, 0:nend], u[:, s, 0:nend], AF.Copy, scale=cw[:, cc, 1:2]), r=[f"u{s}", "cw"], w=[f"yv{s}"])
                for (s0, s1) in seqs:
                    P.op("dve", lambda e: e.scalar_tensor_tensor(yv[:, s, s0 + 1:s1], u[:, s, s0:s1 - 1], cw[:, cc, 0:1], yv[:, s, s0 + 1:s1], ALU.mult, ALU.add),
                         r=[f"u{s}", "cw", f"yv{s}"], w=[f"yv{s}"])
                    P.op("dve", lambda e: e.scalar_tensor_tensor(yv[:, s, s0:s1 - 1], u[:, s, s0 + 1:s1], cw[:, cc, 2:3], yv[:, s, s0:s1 - 1], ALU.mult, ALU.add),
                         r=[f"u{s}", "cw", f"yv{s}"], w=[f"yv{s}"])
                P.op("pool", lambda e: e.tensor_tensor(ob[:, s, 0:nend], yv[:, s, 0:nend], bch[:, s, 0, 0:nend], ALU.mult), r=[f"yv{s}", f"bch{s}"], w=[f"ob{s}"])
                P.dma("sp", YT[512 + cc * 128:512 + (cc + 1) * 128, 0:nend], ob[:, s, 0:nend], r=[f"ob{s}"], w=["YT"])
            P.barrier()

    def phase_swa(l):
        need_ctx = l < DEPTH - 1
        with ExitStack() as es:
            def sb(name, shape, dt=F32):
                return es.enter_context(nc.sbuf_tensor(U(name), shape, dt)).ap()
            esink = sb("esink", [128, 8]); mkf = sb("mkf", [128, 2, 128]); mk = sb("mk", [128, 2, 128], BF16)
            QT0 = sb("QT0", [128, 4, NT], BF16); QT1 = sb("QT1", [128, 4, NT], BF16)
            KT = sb("KT", [128, NT], BF16); Vp = sb("Vp", [128, 18, 2, 65], BF16)
            P.op("pool", lambda e: e.memset(QT0[64:128], 0.0), w=["QT"])
            P.op("pool", lambda e: e.memset(QT1[0:64], 0.0), w=["QT"])
            zin = sb("zin", [128, 2, 768], BF16)
            PT = sb("PT", [128, 2, 5, 512], BF16)
            den = sb("den", [128, 2, 4]); rec = sb("rec", [128, 2, 4])
            ysw = sb("ysw", [128, 2, 512], BF16); yst = sb("yst", [128, 2, 4, 512], BF16)
            P.dma("sp", esink, sink_in[l:l + 1, :].partition_broadcast(128), w=["esink"])
            P.op("act", lambda e: e.activation(esink, esink, AF.Exp), r=["esink"], w=["esink"])
            P.dma("sp", mkf, swamask_in, w=["mkf"])
            P.op("dve", lambda e: e.tensor_copy(mk, mkf), r=["mkf"], w=["mk"])
            P.op("pool", lambda e: e.memset(Vp, 1.0), w=["Vp"])
            for tt in range(18):
                s = tt % 2
                zk = f"zin{s}"
                P.dma("sp", zin[:, s, :], ZT[tt * 128:(tt + 1) * 128, ZT_SQ:ZT_SQ + 768], r=["ZT"], w=[zk])
                tb = s
                TP = bank(tb)
                TPk = bank(2 + s)
                for i4 in range(4):
                    P.op("pe", lambda e: e.matmul(TP[:, i4 * 128:(i4 + 1) * 128], zin[:, s, i4 * 128:(i4 + 1) * 128], identB, start=True, stop=True),
                         r=[zk, "identB"], w=[bkey(tb)])
                P.op("pe", lambda e: e.matmul(TPk[:, 0:128], zin[:, s, 512:640], identB, start=True, stop=True), r=[zk, "identB"], w=[bkey(2 + s)])
                P.op("act", lambda e: e.activation(QT0[0:64, :, tt * 128:(tt + 1) * 128], TP[0:64, 0:512].rearrange("p (i n) -> p i n", i=4), AF.Copy),
                     r=[bkey(tb), "QT"], w=["QT"])
                P.op("act", lambda e: e.activation(QT1[64:128, :, tt * 128:(tt + 1) * 128], TP[64:128, 0:512].rearrange("p (i n) -> p i n", i=4), AF.Copy),
                     r=[bkey(tb), "QT"], w=["QT"])
                P.op("dve", lambda e: e.tensor_copy(KT[:, tt * 128:(tt + 1) * 128], TPk[:, 0:128]), r=[bkey(2 + s)], w=["KT"])
                P.op("pool", lambda e: e.tensor_copy(Vp[:, tt, :, 0:64], zin[:, s, 640:768].rearrange("p (k d) -> p k d", d=64)), r=[zk, "Vp"], w=["Vp"])
            nblk = 18 if need_ctx else 16
            order = ([17, 16] if need_ctx else []) + list(range(15, -1, -1))
            it = 0
            for blk in order:
                if blk < 16:
                    tiles = [(16, None), (17, None)]
                    if blk > 0:
                        tiles.append((blk - 1, 0))
                    tiles.append((blk, None))
                    if blk < 15:
                        tiles.append((blk + 1, 1))
                else:
                    tiles = [(16, None), (17, None)]
                ysb = it % 2
                for kv in range(2):
                    ps = it % 2
                    it += 1
                    rows = slice(kv * 64, kv * 64 + 64)
                    for jj, (kt, mi) in enumerate(tiles):
                        b = 2 + (jj % 3)
                        QZ = QT0 if kv == 0 else QT1
                        P.op("pe", lambda e: e.matmul(bank(b), KT[:, kt * 128:(kt + 1) * 128], QZ[:, :, blk * 128:(blk + 1) * 128],
                                                      start=True, stop=True), r=["KT", "QT"], w=[bkey(b)])
                        P.op("act", lambda e: e.activation(PT[:, ps, jj, :], bank(b), AF.Exp, scale=0.125), r=[bkey(b)], w=[f"PT{ps}_{jj}"])
                        if mi is not None:
                            P.op("pool", lambda e: e.tensor_tensor(PT[:, ps, jj, :].rearrange("p (i q) -> p i q", i=4),
                                                                   PT[:, ps, jj, :].rearrange("p (i q) -> p i q", i=4),
                                                                   mk[:, mi, :].unsqueeze(1).to_broadcast([128, 4, 128]), ALU.mult),
                                 r=[f"PT{ps}_{jj}", "mk"], w=[f"PT{ps}_{jj}"])
                    ob_ = 5 + ps
                    for i4 in range(4):
                        for jj, (kt, mi) in enumerate(tiles):
                            P.op("pe", lambda e: e.matmul(bank(ob_)[:, i4 * 65:(i4 + 1) * 65], PT[:, ps, jj, i4 * 128:(i4 + 1) * 128], Vp[:, kt, kv, :],
                                                          start=(jj == 0), stop=(jj == len(tiles) - 1)),
                                 r=[f"PT{ps}_{jj}", "Vp"], w=[bkey(ob_)])
                    Ov = bank(ob_)[:, 0:260].rearrange("p (i d) -> p i d", d=65)
                    P.op("dve", lambda e: e.tensor_tensor(den[:, ps, :], Ov[:, :, 64], esink[:, kv * 4:(kv + 1) * 4], ALU.add),
                         r=[bkey(ob_), "esink"], w=[f"den{ps}"])
                    P.op("dve", lambda e: e.reciprocal(rec[:, ps, :], den[:, ps, :]), r=[f"den{ps}"], w=[f"rec{ps}"])
                    P.op("dve", lambda e: e.tensor_tensor(ysw[:, ysb, kv * 256:(kv + 1) * 256].rearrange("p (i d) -> p i d", d=64), Ov[:, :, 0:64],
                                                          rec[:, ps, :].unsqueeze(2).to_broadcast([128, 4, 64]), ALU.mult),
                         r=[bkey(ob_), f"rec{ps}"], w=[f"ysw{ysb}"])
                TY = bank(7)
                for j in range(4):
                    P.op("pe", lambda e: e.matmul(TY[:, j * 128:(j + 1) * 128], ysw[:, ysb, j * 128:(j + 1) * 128], identB, start=True, stop=True),
                         r=[f"ysw{ysb}", "identB"], w=[bkey(7)])
                if blk < 16:
                    grp, pos, gw, g0 = blk // 4, blk % 4, 512, (blk // 4) * 512
                else:
                    grp, pos, gw, g0 = 4, blk - 16, 256, 2048
                ys = grp % 2
                P.op("act", lambda e: e.activation(yst[:, ys, :, pos * 128:(pos + 1) * 128], TY[:, 0:512].rearrange("p (j n) -> p j n", j=4), AF.Copy),
                     r=[bkey(7)], w=[f"yst{ys}"])
                if pos == 0:
                    P.dma("sp", YTv[:, 8:12, g0:g0 + gw], yst[:, ys, :, 0:gw], r=[f"yst{ys}"], w=["YT"])
            P.barrier()

    def phase_na(l):
        need_ctx = l < DEPTH - 1
        with ExitStack() as es:
            def sb(name, shape, dt=F32):
                return es.enter_context(nc.sbuf_tensor(U(name), shape, dt)).ap()
            QTe = sb("QTne", [128, 4, NT], BF16); QTo = sb("QTno", [128, 4, NT], BF16); KT = sb("KTn", [128, 4, NT], BF16)
            Vp = sb("Vpn", [128, 18, 8, 65], BF16); vtmp = sb("vtmp", [128, 2, 512], BF16)
            bias = sb("biasn", [128, 2, 25, 128])
            PT = sb("PTn", [128, 2, 7, 128], BF16); tmp = sb("tmpn", [128, 2, 640])
            rec = sb("recn", [128, 2, 1]); yna = sb("yna", [128, 18, 512], BF16)
            yst = sb("ystn", [128, 2, 4, 512], BF16)
            P.op("pool", lambda e: e.memset(QTe[64:128], 0.0), w=["QTe"])
            P.op("pool", lambda e: e.memset(QTo[0:64], 0.0), w=["QTo"])
            zq = ZF[ZF_NQ:ZF_NQ + 512, :].rearrange("(j p) n -> p j n", p=128)
            P.dma("sp", QTe[0:64], zq[0:64], r=["ZF", "QTe"], w=["QTe"])
            P.dma("sp", QTo[64:128], zq[64:128], r=["ZF", "QTo"], w=["QTo"])
            P.dma("sp", KT, ZF[ZF_NK:ZF_NK + 512, :].rearrange("(j p) n -> p j n", p=128), r=["ZF"], w=["KT"])
            P.op("pool", lambda e: e.memset(Vp, 1.0), w=["Vp"])
            for tt in range(18):
                s = tt % 2
                P.dma("sp", vtmp[:, s, :], ZT[tt * 128:(tt + 1) * 128, ZT_NV:ZT_NV + 512], r=["ZT"], w=[f"vtmp{s}"])
                P.op("pool", lambda e: e.tensor_copy(Vp[:, tt, :, 0:64], vtmp[:, s, :].rearrange("p (h d) -> p h d", d=64)),
                     r=[f"vtmp{s}", "Vp"], w=["Vp"])
            nq = 18 if need_ctx else 16
            it = 0
            for h in range(8):
                j, hf = h // 2, h % 2
                rows = slice(hf * 64, hf * 64 + 64)
                bs = h % 2
                P.dma("sp", bias[:, bs, :, :].rearrange("p a q -> p (a q)"), na_bias[l, h], w=[f"bias{bs}"])
                for m in range(nq):
                    ps = it % 2
                    it += 1
                    lat = na_tiles(m) if m < 16 else []
                    tiles = [16, 17] + lat
                    nl = len(lat)
                    bA, bB = (0, 1) if ps == 0 else (2, 3)
                    for jj, kt in enumerate(tiles):
                        b = bA if jj < 4 else bB
                        QZ = QTe if hf == 0 else QTo
                        P.op("pe", lambda e: e.matmul(bank(b)[:, (jj % 4) * 128:(jj % 4 + 1) * 128], KT[:, j, kt * 128:(kt + 1) * 128],
                                                      QZ[:, j, m * 128:(m + 1) * 128], start=True, stop=True),
                             r=["KT", "QTe", "QTo"], w=[bkey(b)])
                    P.op("act", lambda e: e.activation(PT[:, ps, 0:2, :].rearrange("p a q -> p (a q)"), bank(bA)[:, 0:256], AF.Exp, scale=0.125),
                         r=[bkey(bA)], w=[f"PTa{ps}"])
                    if nl:
                        cl = na_cls(m)
                        P.op("dve", lambda e: e.scalar_tensor_tensor(tmp[:, ps, 0:256], bank(bA)[:, 256:512], 0.125,
                                                                     bias[:, bs, cl * 5:cl * 5 + 2, :].rearrange("p a q -> p (a q)"), ALU.mult, ALU.add),
                             r=[bkey(bA), f"bias{bs}"], w=[f"tmp{ps}"])
                        P.op("dve", lambda e: e.scalar_tensor_tensor(tmp[:, ps, 256:nl * 128], bank(bB)[:, 0:(nl - 2) * 128], 0.125,
                                                                     bias[:, bs, cl * 5 + 2:cl * 5 + nl, :].rearrange("p a q -> p (a q)"), ALU.mult, ALU.add),
                             r=[bkey(bB), f"bias{bs}"], w=[f"tmp{ps}"])
                        P.op("act", lambda e: e.activation(PT[:, ps, 2:2 + nl, :].rearrange("p a q -> p (a q)"), tmp[:, ps, 0:nl * 128], AF.Exp),
                             r=[f"tmp{ps}"], w=[f"PTb{ps}"])
                    ob_ = 4 + ps
                    for jj, kt in enumerate(tiles):
                        P.op("pe", lambda e: e.matmul(bank(ob_)[:, 0:65], PT[:, ps, jj, :], Vp[:, kt, h, :],
                                                      start=(jj == 0), stop=(jj == len(tiles) - 1)),
                             r=[f"PTa{ps}", f"PTb{ps}", "Vp"], w=[bkey(ob_)])
                    P.op("dve", lambda e: e.reciprocal(rec[:, ps, :], bank(ob_)[:, 64:65]), r=[bkey(ob_)], w=[f"rec{ps}"])
                    P.op("dve", lambda e: e.tensor_scalar(yna[:, m, h * 64:(h + 1) * 64], bank(ob_)[:, 0:64], rec[:, ps, 0:1], None, ALU.mult),
                         r=[bkey(ob_), f"rec{ps}"], w=["yna"])
            for m in range(nq):
                tb = 6 + (m % 2)
                TY = bank(tb)
                for jx in range(4):
                    P.op("pe", lambda e: e.matmul(TY[:, jx * 128:(jx + 1) * 128], yna[:, m, jx * 128:(jx + 1) * 128], identB, start=True, stop=True),
                         r=["yna", "identB"], w=[bkey(tb)])
                if m < 16:
                    grp, pos, gw, g0 = m // 4, m % 4, 512, (m // 4) * 512
                    last = pos == 3
                else:
                    grp, pos, gw, g0 = 4, m - 16, 256, 2048
                    last = pos == 1
                ys = grp % 2
                P.op("act", lambda e: e.activation(yst[:, ys, :, pos * 128:(pos + 1) * 128], TY[:, 0:512].rearrange("p (j n) -> p j n", j=4), AF.Copy),
                     r=[bkey(tb)], w=[f"yst{ys}"])
                if last:
                    P.dma("sp", YTv[:, 12:16, g0:g0 + gw], yst[:, ys, :, 0:gw], r=[f"yst{ys}"], w=["YT"])
            P.barrier()

    def phase_outproj(l):
        need_ctx = l < DEPTH - 1
        with ExitStack() as es:
            ring = Ring(es)
            ytsb = es.enter_context(nc.sbuf_tensor(U("ytsb"), [128, 16, NT], BF16)).ap()
            xt = es.enter_context(nc.sbuf_tensor(U("xt"), [128, 3, 512], F32)).ap()
            nend = NT if need_ctx else L
            for c4 in range(4):
                P.dma("sp", ytsb[:, c4 * 4:(c4 + 1) * 4, 0:nend], YTv[:, c4 * 4:(c4 + 1) * 4, 0:nend], r=["YT"], w=["ytsb"])
            groups = GROUPS if need_ctx else GROUPS[:4]
            xi = 0
            bi = 0
            for mg in range(4):
                wt, wk = ring.load(w_out[l, :, mg * 512:(mg + 1) * 512].rearrange("(c p) n -> p c n", p=128),
                                   lambda t: t.rearrange("p (c n) -> p c n", c=16))
                for mm in range(4):
                    m = mg * 4 + mm
                    for (g0, gsz) in groups:
                        r = 1 if g0 >= L else 0
                        xs = xi % 3
                        xi += 1
                        b = bi % 6
                        bi += 1
                        P.dma("sp", xt[:, xs, 0:gsz], XT[m * 128:(m + 1) * 128, g0:g0 + gsz], r=["XT"], w=[f"xt{xs}"])
                        for c in range(16):
                            P.op("pe", lambda e: e.matmul(bank(b)[:, 0:gsz], wt[:, c, mm * 128:(mm + 1) * 128], ytsb[:, c, g0:g0 + gsz],
                                                          start=(c == 0), stop=(c == 15)), r=[wk, "ytsb"], w=[bkey(b)])
                        P.op("dve", lambda e: e.scalar_tensor_tensor(xt[:, xs, 0:gsz], bank(b)[:, 0:gsz], ada_vec(l, 2, r)[:, m:m + 1],
                                                                     xt[:, xs, 0:gsz], ALU.mult, ALU.add),
                             r=[bkey(b), f"xt{xs}", "ada_sb"], w=[f"xt{xs}"])
                        P.dma("sp", XT[m * 128:(m + 1) * 128, g0:g0 + gsz], xt[:, xs, 0:gsz], r=[f"xt{xs}"], w=["XT"])
            P.barrier()

    def phase_ffn(l):
        need_ctx = l < DEPTH - 1
        grps = GROUPS if need_ctx else GROUPS[:4]
        nsl = NSL if need_ctx else CAP
        with ExitStack() as esf:
            def sbf(name, shape, dt=F32):
                return esf.enter_context(nc.sbuf_tensor(U(name), shape, dt)).ap()
            idxf = sbf("idxf", [16, CAP]); vals = sbf("vals", [16, CAP]); idxu = sbf("idxu", [16, CAP], U32)
            idxfc = sbf("idxfc", [16, CAPC]); valsc = sbf("valsc", [16, CAPC]); idxuc = sbf("idxuc", [16, CAPC], U32)
            idxT = sbf("idxT", [128, 16, 2]); gateT = sbf("gateT", [128, 16, 2])
            gateTc = sbf("gateTc", [32, 16]); idxTcu = sbf("idxTcu", [32, 16]); idxTcP = sbf("idxTcP", [128, 4])
            idxTu = sbf("idxTu", [128, 16, 2], U32); idxTcU = sbf("idxTcU", [32, 16], U32)
            with ExitStack() as esh:
                with ExitStack() as es:
                    def sb(name, shape, dt=F32):
                        return es.enter_context(nc.sbuf_tensor(U(name), shape, dt)).ap()
                    xg = sb("xg", [128, 16, 512]); nb = alloc_norm_bufs(es)
                    h32 = sb("h32", [128, 3, 512]); hg = sb("hg", [128, 16, 512], BF16)
                    wr = sb("wr", [128, 16, 16]); E_sb = sb("E_sb", [16, NT]); aff = sb("aff", [16, NT]); rs = sb("rs", [16, 512])
                    h2st = sb("h2st", [128, 2, 2048], BF16)
                    P.dma("sp", wr, w_router[l].rearrange("(c p) e -> p c e", p=128), w=["wr"])
                    zt = sb("zt", [128, 2048])
                    P.op("pool", lambda e: e.memset(zt, 0.0), w=["zt"])
                    for tz in range(18 if need_ctx else 16):
                        P.dma("sp", MOE[tz * 128:(tz + 1) * 128, :], zt, r=["zt"], w=["MOE"])
                    bi = 0
                    for (g0, gsz) in grps:
                        r = 1 if g0 >= L else 0
                        P.dma("sp", xg[:, :, 0:gsz], XTv[:, :, g0:g0 + gsz], r=["XT"], w=["xg"])

                        def out_fn(c, tc, key, r=r, gsz=gsz):
                            s = c % 3
                            P.op("act", lambda e: e.activation(h32[:, s, 0:gsz], tc, AF.Identity, bias=ada_vec(l, 3, r)[:, c:c + 1], scale=1.0),
                                 r=[key, "ada_sb"], w=[f"h32_{s}"])
                            P.op("pe", lambda e: e.matmul(bank(6)[0:16, 0:gsz], wr[:, c, :], h32[:, s, 0:gsz], start=(c == 0), stop=(c == 15)),
                                 r=["wr", f"h32_{s}"], w=[bkey(6)])
                            P.op("pool", lambda e: e.tensor_copy(hg[:, c, 0:gsz], h32[:, s, 0:gsz]), r=[f"h32_{s}"], w=["hg"])
                        norm_group(nb, xg, gsz, scl[:, l, 1, :, r], None, out_fn)
                        P.op("act", lambda e: e.activation(E_sb[:, g0:g0 + gsz], bank(6)[0:16, 0:gsz], AF.Exp), r=[bkey(6)], w=["E_sb"])
                        for tt in range(gsz // 128):
                            tile = (g0 // 128) + tt
                            for cb in range(4):
                                b = bi % 6
                                bi += 1
                                for cc in range(4):
                                    c = cb * 4 + cc
                                    P.op("pe", lambda e: e.matmul(bank(b)[:, cc * 128:(cc + 1) * 128], hg[:, c, tt * 128:(tt + 1) * 128], identB,
                                                                  start=True, stop=True), r=["hg", "identB"], w=[bkey(b)])
                                copy_op(evac_eng(), h2st[:, tile % 2, cb * 512:(cb + 1) * 512], bank(b), r=[bkey(b)], w=[f"h2st{tile % 2}"])
                            P.dma("sp", H2D[tile * 128:(tile + 1) * 128, :], h2st[:, tile % 2, :], r=[f"h2st{tile % 2}"], w=["H2D"])
                    for (g0, gsz) in grps:
                        P.op("pe", lambda e: e.matmul(bank(7)[0:16, 0:gsz], onesF[0:16, 0:16], E_sb[:, g0:g0 + gsz], start=True, stop=True),
                             r=["onesF", "E_sb"], w=[bkey(7)])
                        P.op("dve", lambda e: e.reciprocal(rs[:, 0:gsz], bank(7)[0:16, 0:gsz]), r=[bkey(7)], w=["rs"])
                        P.op("dve", lambda e: e.tensor_tensor(aff[:, g0:g0 + gsz], E_sb[:, g0:g0 + gsz], rs[:, 0:gsz], ALU.mult), r=["E_sb", "rs"], w=["aff"])
                    for (a0, n, k, vv, iu, ifl, kk) in ((0, L, CAP, vals, idxu, idxf, "l"),) + (((L, T, CAPC, valsc, idxuc, idxfc, "c"),) if need_ctx else ()):
                        aw = aff[:, a0:a0 + n]
                        for it in range(k // 8):
                            v8 = vv[:, it * 8:(it + 1) * 8]
                            P.op("dve", lambda e: e.max(v8, aw), r=["aff"], w=["v8" + kk])
                            P.op("dve", lambda e: e.max_index(iu[:, it * 8:(it + 1) * 8], v8, aw), r=["aff", "v8" + kk], w=["iu" + kk])
                            P.op("dve", lambda e: e.match_replace(aw, v8, aw, -1.0), r=["v8" + kk, "aff"], w=["aff"])
                        P.op("dve", lambda e: e.tensor_copy(ifl, iu), r=["iu" + kk], w=["idxf" + kk])
                    for t2 in range(2):
                        P.op("pe", lambda e: e.transpose(bank(0)[:, t2 * 16:(t2 + 1) * 16], idxf[0:16, t2 * 128:(t2 + 1) * 128], identF[0:16, 0:16]),
                             r=["idxfl", "identF"], w=[bkey(0)])
                        P.op("pe", lambda e: e.transpose(bank(1)[:, t2 * 16:(t2 + 1) * 16], vals[0:16, t2 * 128:(t2 + 1) * 128], identF[0:16, 0:16]),
                             r=["v8l", "identF"], w=[bkey(1)])
                    P.op("dve", lambda e: e.tensor_copy(idxT.rearrange("p e t -> p t e"), bank(0)[:, 0:32].rearrange("p (t e) -> p t e", t=2)), r=[bkey(0)], w=["idxT"])
                    P.op("dve", lambda e: e.tensor_copy(gateT.rearrange("p e t -> p t e"), bank(1)[:, 0:32].rearrange("p (t e) -> p t e", t=2)), r=[bkey(1)], w=["gateT"])
                    P.op("dve", lambda e: e.tensor_copy(idxTu, idxT), r=["idxT"], w=["idxTu"])
                    if need_ctx:
                        P.op("pe", lambda e: e.transpose(bank(2)[0:32, 0:16], idxfc[0:16, 0:32], identF[0:16, 0:16]), r=["idxfc", "identF"], w=[bkey(2)])
                        P.op("pe", lambda e: e.transpose(bank(3)[0:32, 0:16], valsc[0:16, 0:32], identF[0:16, 0:16]), r=["v8c", "identF"], w=[bkey(3)])
                        P.op("dve", lambda e: e.tensor_copy(idxTcu, bank(2)[0:32, 0:16]), r=[bkey(2)], w=["idxTcu"])
                        P.op("dve", lambda e: e.tensor_copy(gateTc, bank(3)[0:32, 0:16]), r=[bkey(3)], w=["gateTc"])
                        P.op("dve", lambda e: e.tensor_copy(idxTcU, idxTcu), r=["idxTcu"], w=["idxTcU"])
                        P.dma("sp", IDXC, idxTcu, r=["idxTcu"], w=["IDXC"])
                        for j4 in range(4):
                            P.dma("sp", idxTcP[j4 * 32:(j4 + 1) * 32, :], IDXC.rearrange("s (t j) -> s j t", j=4)[:, j4, :], r=["IDXC"], w=["idxTcP"],
                                  allow_slow_non_contiguous=True)
                    if dbg:
                        P.dma("sp", DBG1[:, 0:CAP], idxf, r=["idxfl"], w=["DBG1"])
                        P.dma("sp", DBG1[:, CAP:2 * CAP], vals, r=["v8l"], w=["DBG1"])
                    P.barrier()
                with ExitStack() as es:
                    def sb(name, shape, dt=F32):
                        return es.enter_context(nc.sbuf_tensor(U(name), shape, dt)).ap()
                    ring = Ring(es)
                    xs = sb("xs", [128, 2, 3, 2048], BF16)
                    xsT = sb("xsT", [128, 16, NSL], BF16); actT = sb("actT", [128, 8, NSL], BF16); sA = sb("sA", [128, 2, NSL])
                    yest = sb("yest", [128, 6, 2048], BF16)
                    gi = 0
                    yi = 0
                    ne = cfg.get("n_experts", NE)

                    def issue_scatter(ex):
                        for (s0, ssz, st) in stiles_all:
                            ys = (ex % 2) * 3 + st
                            if st < 2:
                                P.dma_scatter_add(MOE, idxTu[:, ex, st:st + 1], yest[:, ys, :], r=[f"yest{ys}", "idxTu"], w=["MOE"])
                            else:
                                P.dma_scatter_add(MOE, idxTcU[0:32, ex:ex + 1], yest[0:32, ys, :], element_offset=L * D,
                                                  r=[f"yest{ys}", "idxTcU"], w=["MOE"])
                    stiles_all = [(0, 128, 0), (128, 128, 1)] + ([(256, 32, 2)] if need_ctx else [])
                    gtiles = [(0, 128, 0), (128, 128, 1)] + ([(256, 32, 2)] if need_ctx else [])

                    def issue_gather(ex):
                        xb = ex % 2
                        for (s0, ssz, st) in gtiles:
                            if st < 2:
                                P.dma_gather(xs[:, xb, st, :], H2D, idxTu[:, ex, st:st + 1], r=["H2D", "idxTu"], w=[f"xs{xb}_{st}"])
                            else:
                                P.dma_gather(xs[0:32, xb, st, :], H2D, idxTcU[0:32, ex:ex + 1], element_offset=L * D,
                                             r=["H2D", "idxTcU"], w=[f"xs{xb}_{st}"])
                    if ne > 0:
                        issue_gather(0)
                    for ex in range(ne):
                        xb = ex % 2
                        if ex + 1 < ne:
                            issue_gather(ex + 1)
                        for (s0, ssz, st) in gtiles:
                            for cb in range(4):
                                b = 1 + gi % 4
                                gi += 1
                                for cc in range(4):
                                    c = cb * 4 + cc
                                    P.op("pe", lambda e: e.matmul(bank(b)[:, cc * 128:cc * 128 + ssz], xs[0:ssz, xb, st, c * 128:(c + 1) * 128],
                                                                  identB[0:ssz, 0:ssz], start=True, stop=True),
                                         r=[f"xs{xb}_{st}", "identB"], w=[bkey(b)])
                                copy_op(evac_eng(), xsT[:, cb * 4:(cb + 1) * 4, s0:s0 + ssz],
                                        bank(b).rearrange("p (c n) -> p c n", c=4)[:, :, 0:ssz], r=[bkey(b)], w=["xsT"])
                        for hf in range(2):
                            G, gk = ring.load(w_gate[l, ex, :, hf * 512:(hf + 1) * 512].rearrange("(c p) n -> p c n", p=128),
                                              lambda t: t.rearrange("p (c n) -> p c n", c=16))
                            Uw, uk = ring.load(w_up[l, ex, :, hf * 512:(hf + 1) * 512].rearrange("(c p) n -> p c n", p=128),
                                               lambda t: t.rearrange("p (c n) -> p c n", c=16))
                            for fcl in range(4):
                                fc = hf * 4 + fcl
                                bA, bU = (1, 2) if fcl % 2 == 0 else (3, 4)
                                for c in range(16):
                                    P.op("pe", lambda e: e.matmul(bank(bA)[:, 0:nsl], G[:, c, fcl * 128:(fcl + 1) * 128], xsT[:, c, 0:nsl], start=(c == 0), stop=(c == 15)),
                                         r=[gk, "xsT"], w=[bkey(bA)])
                                for c in range(16):
                                    P.op("pe", lambda e: e.matmul(bank(bU)[:, 0:nsl], Uw[:, c, fcl * 128:(fcl + 1) * 128], xsT[:, c, 0:nsl], start=(c == 0), stop=(c == 15)),
                                         r=[uk, "xsT"], w=[bkey(bU)])
                                ss = fcl % 2
                                P.op("act", lambda e: e.activation(sA[:, ss, 0:nsl], bank(bA)[:, 0:nsl], AF.Silu), r=[bkey(bA)], w=[f"sA{ss}"])
                                P.op("dve", lambda e: e.tensor_tensor(actT[:, fc, 0:nsl], sA[:, ss, 0:nsl], bank(bU)[:, 0:nsl], ALU.mult),
                                     r=[f"sA{ss}", bkey(bU)], w=["actT"])
                        if ex > 0:
                            issue_scatter(ex - 1)
                        Dt = []
                        for hf in range(2):
                            Dw, dk = ring.load(w_down[l, ex, hf * 512:(hf + 1) * 512, :].rearrange("(c p) n -> p c n", p=128),
                                               lambda t: t.rearrange("p (c n) -> p c n", c=4))
                            Dt.append((Dw, dk))
                        stiles = [(0, 128, 0), (128, 128, 1)] + ([(256, 32, 2)] if need_ctx else [])
                        for (s0, ssz, st) in stiles:
                            ys = (ex % 2) * 3 + st
                            for dg in range(4):
                                b = 5 + gi % 3
                                gi += 1
                                for fc in range(8):
                                    Dw, dk = Dt[fc // 4]
                                    P.op("pe", lambda e: e.matmul(bank(b)[0:ssz, :], actT[:, fc, s0:s0 + ssz], Dw[:, fc % 4, dg * 512:(dg + 1) * 512],
                                                                  start=(fc == 0), stop=(fc == 7)), r=["actT", dk], w=[bkey(b)])
                                gsc = gateT[:, ex, st:st + 1] if st < 2 else gateTc[:, ex:ex + 1]
                                eng = evac_eng()
                                if eng == "act":
                                    P.op("act", lambda e: e.activation(yest[0:ssz, ys, dg * 512:(dg + 1) * 512], bank(b)[0:ssz, :], AF.Copy, scale=gsc[0:ssz]),
                                         r=[bkey(b), "gateT", "gateTc"], w=[f"yest{ys}"])
                                else:
                                    P.op("dve", lambda e: e.tensor_scalar(yest[0:ssz, ys, dg * 512:(dg + 1) * 512], bank(b)[0:ssz, :], gsc[0:ssz], None, ALU.mult),
                                         r=[bkey(b), "gateT", "gateTc"], w=[f"yest{ys}"])
                    if ne > 0:
                        issue_scatter(ne - 1)
                    P.barrier()
            with ExitStack() as es:
                def sb(name, shape, dt=F32):
                    return es.enter_context(nc.sbuf_tensor(U(name), shape, dt)).ap()
                mt = sb("mt", [128, 4, 2048]); xg = sb("xg3", [128, 16, 512])
                bi = 0
                for (g0, gsz) in grps:
                    r = 1 if g0 >= L else 0
                    ntl = gsz // 128
                    P.dma("sp", xg[:, :, 0:gsz], XTv[:, :, g0:g0 + gsz], r=["XT"], w=["xg"])
                    for tt in range(ntl):
                        P.dma("sp", mt[:, tt, :], MOE[g0 + tt * 128:g0 + (tt + 1) * 128, :], r=["MOE"], w=[f"mt{tt}"])
                    for c in range(16):
                        b = bi % 6
                        bi += 1
                        for tt in range(ntl):
                            P.op("pe", lambda e: e.transpose(bank(b)[:, tt * 128:(tt + 1) * 128], mt[:, tt, c * 128:(c + 1) * 128], identF),
                                 r=[f"mt{tt}", "identF"], w=[bkey(b)])
                        P.op("dve", lambda e: e.scalar_tensor_tensor(xg[:, c, 0:gsz], bank(b)[:, 0:gsz], ada_vec(l, 5, r)[:, c:c + 1],
                                                                     xg[:, c, 0:gsz], ALU.mult, ALU.add),
                             r=[bkey(b), "xg", "ada_sb"], w=["xg"])
                    P.dma("sp", XTv[:, :, g0:g0 + gsz], xg[:, :, 0:gsz], r=["xg"], w=["XT"])
                P.barrier()

    def phase_final():
        with ExitStack() as es:
            def sb(name, shape, dt=F32):
                return es.enter_context(nc.sbuf_tensor(U(name), shape, dt)).ap()
            xg = sb("xg", [128, 16, 512]); xn = sb("xn", [128, 16, 512]); nb = alloc_norm_bufs(es)
            ost = sb("ost", [128, 2, 2048])
            oi = 0
            bi = 0
            for (g0, gsz) in GROUPS[:4]:
                P.dma("sp", xg, XTv[:, :, g0:g0 + gsz], r=["XT"], w=["xg"])

                def out_fn(c, tc, key):
                    copy_op("act" if c % 2 else "pool", xn[:, c, :], tc, r=[key], w=["xn"])
                norm_group(nb, xg, gsz, nfin_sb, None, out_fn)
                for tt in range(4):
                    os_ = oi % 2
                    oi += 1
                    for cb in range(4):
                        b = bi % 6
                        bi += 1
                        for cc in range(4):
                            c = cb * 4 + cc
                            P.op("pe", lambda e: e.transpose(bank(b)[:, cc * 128:(cc + 1) * 128], xn[:, c, tt * 128:(tt + 1) * 128], identF),
                                 r=["xn", "identF"], w=[bkey(b)])
                        copy_op(evac_eng(), ost[:, os_, cb * 512:(cb + 1) * 512], bank(b), r=[bkey(b)], w=[f"ost{os_}"])
                    P.dma("sp", y_out[g0 + tt * 128:g0 + (tt + 1) * 128, :], ost[:, os_, :], r=[f"ost{os_}"], w=["y"])
            P.barrier()

    if not cfg.get("skip_ada"):
        phase_ada()
    only_mix = cfg.get("only_mix")
    for l in range(nlayers):
        if not cfg.get("skip_inproj"):
            with ExitStack() as esl:
                hT = esl.enter_context(nc.sbuf_tensor(U("hT"), [128, 16, NT], BF16)).ap()
                phase_norm1(l, hT, esl)
                if stop_after == "norm1":
                    break
                phase_inproj(l, hT)
        if stop_after == "inproj":
            break
        if only_mix is None or "ret" in only_mix:
            phase_ret(l)
        if only_mix is None or "conv" in only_mix:
            phase_conv(l)
        if only_mix is None or "swa" in only_mix:
            phase_swa(l)
        if only_mix is None or "na" in only_mix:
            phase_na(l)
        if stop_after == "mix":
            break
        if not cfg.get("skip_outproj"):
            phase_outproj(l)
        if stop_after == "outproj":
            break
        phase_ffn(l)
        if stop_after == "ffn":
            break
    else:
        phase_final()

    P.barrier()
    print(f"[build] ops={P.nops} waits={P.nwaits}")
    return nc


def make_in_maps(inputs):
    hc = _host_consts()
    f = lambda a: np.ascontiguousarray(np.asarray(a, dtype=np.float32))
    x = f(inputs["x"]); c = f(inputs["c"]); ctx = f(inputs["ctx"]); c_ctx = f(inputs["c_ctx"])
    shared = {
        "w_ada": f(inputs["w_ada"]),
        "bada_t": f(np.asarray(inputs["b_ada"]).reshape(DEPTH, 96, 128).transpose(0, 2, 1)),
        "nmix_t": f(np.asarray(inputs["norm_mix"]).reshape(DEPTH, 16, 128).transpose(0, 2, 1)),
        "nffn_t": f(np.asarray(inputs["norm_ffn"]).reshape(DEPTH, 16, 128).transpose(0, 2, 1)),
        "nfin_t": f(np.asarray(inputs["norm_final"]).reshape(16, 128).T),
        "w_in": f(inputs["w_in"]), "w_out": f(inputs["w_out"]),
        "decA": f(np.concatenate([inputs["ret_decay_fwd"], inputs["ret_decay_bwd"]], axis=1)),
        "decB": f(np.stack([np.concatenate([np.asarray(inputs["ret_decay_fwd"])[:, hf::2],
                                            np.asarray(inputs["ret_decay_bwd"])[:, hf::2]], axis=1) for hf in range(2)], axis=1)),
        "convw_t": f(np.asarray(inputs["conv_w"]).reshape(DEPTH, 3, 4, 128).transpose(0, 3, 2, 1)),
        "sink": f(inputs["swa_sink"]),
        "na_bias": _na_bias_layout(np.asarray(inputs["na_rpb"], dtype=np.float32)),
        "w_router": f(inputs["w_router"]),
        "w_gate": f(inputs["w_gate"]), "w_up": f(inputs["w_up"]), "w_down": f(inputs["w_down"]),
    }
    shared.update(hc)
    maps = []
    for b in range(8):
        m = dict(shared)
        m["x"] = x[b]
        m["ctx"] = ctx[b]
        m["c_t"] = f(np.stack([c[b].reshape(16, 128).T, c_ctx.reshape(16, 128).T], axis=-1))
        maps.append(m)
    return maps


def kernel(**inputs):
    nc = build_program()
    maps = make_in_maps(inputs)
    res = run_bass_kernel_spmd(nc, maps, core_ids=list(range(8)))
    return np.stack([np.asarray(r["y"], dtype=np.float32) for r in res.results], axis=0)
```

```python
from contextlib import ExitStack
import numpy as np
import concourse.bass as bass
import concourse.mybir as mybir
from concourse.bass_utils import run_bass_kernel_spmd

F32 = mybir.dt.float32
BF16 = mybir.dt.bfloat16
U32 = mybir.dt.uint32
ALU = mybir.AluOpType
AF = mybir.ActivationFunctionType
AX = mybir.AxisListType

D = 2048
L = 2048
T = 256
NT = L + T
DEPTH = 2
NE = 16
FF = 1024
CAP = 256
CAPC = 32
NSL = CAP + CAPC
IN_COLS = 5888
EPS = 1e-6
GROUPS = [(0, 512), (512, 512), (1024, 512), (1536, 512), (2048, 256)]
NEG = -30000.0

ZT_RQ, ZT_RK, ZT_RV, ZT_RG, ZT_SQ, ZT_SK, ZT_SV, ZT_NV = 0, 512, 1024, 1536, 2048, 2560, 2688, 2816
ZT_COLS = 3328
ZF_CB, ZF_CC, ZF_CH, ZF_NQ, ZF_NK = 0, 512, 1024, 1536, 2048
ZF_ROWS = 2560

N_DMA_SEMS = 24
N_SW_SEMS = 8


class Prog:
    def __init__(self, nc):
        self.nc = nc
        self.eng = {"pe": nc.tensor, "act": nc.scalar, "dve": nc.vector,
                    "pool": nc.gpsimd, "sp": nc.sync}
        self.sem = {k: nc.alloc_semaphore(name=f"s_{k}") for k in self.eng}
        self.cnt = {k: 0 for k in self.eng}
        self.dma_sems = [nc.alloc_semaphore(name=f"s_dma{i}") for i in range(N_DMA_SEMS)]
        self.dma_cnt = [0] * N_DMA_SEMS
        self.dma_rr = 0
        self.known = {k: {} for k in self.eng}
        self.res = {}
        self.nwaits = 0
        self.nops = 0
        self.sw_sems = [nc.alloc_semaphore(name=f"s_sw{i}") for i in range(N_SW_SEMS)]
        self.sw_cnt = [0] * N_SW_SEMS
        self.sw_rr = 0

    def _semh(self, key):
        if key[0] == "e":
            return self.sem[key[1]]
        if key[0] == "s":
            return self.sw_sems[key[1]]
        return self.dma_sems[key[1]]

    def dma_gather(self, out, in_, idx_ap, element_offset=0, r=(), w=()):
        q = "pool"
        for k, v in self._deps(q, r, w).items():
            self._wait(q, (k, v))
        i = self.sw_rr
        self.sw_rr = (self.sw_rr + 1) % N_SW_SEMS
        if self.sw_cnt[i] > 0:
            self._wait(q, (("s", i), 16 * self.sw_cnt[i]))
        ins = self.eng[q].indirect_dma_start(out, None, in_, bass.IndirectOffsetOnAxis(idx_ap, 0),
                                             element_offset=element_offset)
        self.sw_cnt[i] += 1
        ins.then_inc(self.sw_sems[i], 16)
        tok = (("s", i), 16 * self.sw_cnt[i])
        self._record(tok, r, w)
        self.nops += 1
        return tok

    def dma_scatter_add(self, out, idx_ap, in_, element_offset=0, r=(), w=()):
        q = "pool"
        for k, v in self._deps(q, r, w).items():
            self._wait(q, (k, v))
        i = self.sw_rr
        self.sw_rr = (self.sw_rr + 1) % N_SW_SEMS
        if self.sw_cnt[i] > 0:
            self._wait(q, (("s", i), 16 * self.sw_cnt[i]))
        ins = self.eng[q].indirect_dma_start(out, bass.IndirectOffsetOnAxis(idx_ap, 0), in_, None,
                                             element_offset=element_offset, compute_op=ALU.add)
        self.sw_cnt[i] += 1
        ins.then_inc(self.sw_sems[i], 16)
        tok = (("s", i), 16 * self.sw_cnt[i])
        self._record(tok, r, w)
        self.nops += 1
        return tok

    def dma_sw(self, slot, out, in_, r=(), w=(), **kw):
        q = "pool"
        for k, v in self._deps(q, r, w).items():
            self._wait(q, (k, v))
        i = self.sw_rr
        self.sw_rr = (self.sw_rr + 1) % N_SW_SEMS
        if self.sw_cnt[i] > 0:
            self._wait(q, (("s", i), 16 * self.sw_cnt[i]))
        ins = self.eng[q].dma_start(out=out, in_=in_, **kw)
        self.sw_cnt[i] += 1
        ins.then_inc(self.sw_sems[i], 16)
        tok = (("s", i), 16 * self.sw_cnt[i])
        self._record(tok, r, w)
        self.nops += 1
        return tok

    def _wait(self, e, tok):
        key, val = tok
        if self.known[e].get(key, 0) >= val:
            return
        self.eng[e].wait_ge(self._semh(key), val)
        self.known[e][key] = val
        self.nwaits += 1

    def _deps(self, e, r, w):
        deps = {}

        def add(tok):
            if tok is None:
                return
            k, v = tok
            if k == ("e", "pe") and e == "pe":
                return
            if deps.get(k, 0) < v:
                deps[k] = v
        for k in r:
            st = self.res.get(k)
            if st:
                add(st[0])
        for k in w:
            st = self.res.get(k)
            if st:
                add(st[0])
                for t in st[1]:
                    add(t)
        return deps

    def _record(self, tok, r, w):
        for k in w:
            self.res[k] = [tok, []]
        for k in r:
            st = self.res.setdefault(k, [None, []])
            lst = st[1]
            for i, (kk, vv) in enumerate(lst):
                if kk == tok[0]:
                    if vv < tok[1]:
                        lst[i] = tok
                    break
            else:
                lst.append(tok)

    def op(self, e, fn, r=(), w=()):
        psr = [k for k in r if k.startswith("ps")]
        if psr:
            r = [k for k in r if not k.startswith("ps")]
            w = list(w) + psr
        for k, v in self._deps(e, r, w).items():
            self._wait(e, (k, v))
        ins = fn(self.eng[e])
        self.cnt[e] += 1
        ins.then_inc(self.sem[e], 1)
        tok = (("e", e), self.cnt[e])
        self._record(tok, r, w)
        self.nops += 1
        return tok

    def dma(self, q, out, in_, r=(), w=(), **kw):
        for k, v in self._deps(q, r, w).items():
            self._wait(q, (k, v))
        i = self.dma_rr
        self.dma_rr = (self.dma_rr + 1) % N_DMA_SEMS
        if self.dma_cnt[i] > 0:
            self._wait(q, (("d", i), 16 * self.dma_cnt[i]))
        ins = self.eng[q].dma_start(out=out, in_=in_, **kw)
        self.dma_cnt[i] += 1
        ins.then_inc(self.dma_sems[i], 16)
        tok = (("d", i), 16 * self.dma_cnt[i])
        self._record(tok, r, w)
        self.nops += 1
        return tok

    def _bump(self, q):
        if self.cnt[q] > 0:
            self._wait(q, (("e", q), self.cnt[q]))
        ins = self.eng[q].nop()
        self.cnt[q] += 1
        ins.then_inc(self.sem[q], 1)

    def _all_wait_all(self):
        for e in self.eng:
            for e2 in self.eng:
                if self.cnt[e2] > 0:
                    self._wait(e, (("e", e2), self.cnt[e2]))

    def barrier(self):
        for i in range(N_DMA_SEMS):
            if self.dma_cnt[i] > 0:
                self._wait("sp", (("d", i), 16 * self.dma_cnt[i]))
        for i in range(N_SW_SEMS):
            if self.sw_cnt[i] > 0:
                self._wait("pool", (("s", i), 16 * self.sw_cnt[i]))
        self._bump("sp")
        self._bump("pool")
        self._all_wait_all()
        self.res = {}


def _host_consts():
    c = {}
    t = np.arange(L)
    row = (t // 64).astype(np.float32)
    col = (t % 64).astype(np.float32)
    inv = (10000.0 ** (-np.arange(16, dtype=np.float32) / 16)).astype(np.float32)
    ang = np.concatenate([row[:, None] * inv, col[:, None] * inv], axis=-1).astype(np.float32)
    cos, sin = np.cos(ang).astype(np.float32), np.sin(ang).astype(np.float32)
    CC = np.concatenate([cos, cos], -1)
    SS = np.concatenate([-sin, sin], -1)
    tab = np.stack([CC, SS, 0.125 * CC, 0.125 * SS], 1)
    c["cs_tab"] = np.ascontiguousarray(tab.reshape(16, 128, 4, 64).transpose(1, 0, 2, 3)).astype(np.float32)
    k = np.arange(128)[:, None].astype(np.float32)
    q = np.arange(128)[None, :].astype(np.float32)
    retc = np.stack([np.maximum(q - k, 0), (q >= k).astype(np.float32),
                     np.maximum(k - q, 0), (k > q).astype(np.float32)], 1)
    c["retc"] = np.ascontiguousarray(retc).astype(np.float32)
    p = np.arange(128, dtype=np.float32)
    c["posv"] = np.stack([p + 1, 127 - p, 128 - p, p], 1).astype(np.float32)
    qr = np.stack([np.arange(128) + 1.0, 128.0 - np.arange(128)], 0)
    c["qrow"] = np.ascontiguousarray(np.broadcast_to(qr[None], (128, 2, 128))).astype(np.float32)
    c["swamask"] = np.ascontiguousarray(np.stack([(k >= q), (k <= q)], 1)).astype(np.float32)
    return c


NA_CLS_TILES = {0: [0, 1, 2, 3], 1: [0, 1, 2, 3], 2: None, 3: [12, 13, 14, 15], 4: [12, 13, 14, 15]}
NA_CLS_REP = {0: 0, 1: 1, 2: 2, 3: 14, 4: 15}


def na_cls(m):
    if m <= 1:
        return m
    if m >= 14:
        return m - 11
    return 2


def na_tiles(m):
    cl = na_cls(m)
    if cl == 2:
        return [m - 2, m - 1, m, m + 1, m + 2]
    return NA_CLS_TILES[cl]


def _na_bias_layout(rpb):
    out = np.full((DEPTH, 8, 128, 5, 5, 128), NEG, np.float32)
    a = np.arange(128) // 64
    cc = np.arange(128) % 64
    for cl in range(5):
        m = NA_CLS_REP[cl]
        tiles = na_tiles(m)
        qr = 2 * m + a
        qc = cc
        bs = np.clip(qr - 4, 0, 24)
        cs = np.clip(qc - 8, 0, 48)
        for j, kt in enumerate(tiles):
            kr = 2 * kt + a
            kc = cc
            inband = (kr[:, None] >= bs[None, :]) & (kr[:, None] < bs[None, :] + 8)
            colok = (kc[:, None] >= cs[None, :]) & (kc[:, None] < cs[None, :] + 16)
            dr = np.clip(kr[:, None] - qr[None, :] + 7, 0, 14)
            dc = np.clip(kc[:, None] - qc[None, :], -15, 15) + 15
            g = rpb[:, :, dr, dc]
            out[:, :, :, cl, j, :] = np.where((inband & colok)[None, None], g, np.float32(NEG))
    return out.reshape(DEPTH, 8, 128, 25 * 128)


def build_program(cfg=None):
    cfg = cfg or {}
    _uid = [0]

    def U(n):
        _uid[0] += 1
        return f"{n}_{_uid[0]}"
    dbg = cfg.get("debug", False)
    stop_after = cfg.get("stop_after", None)
    nlayers = cfg.get("nlayers", DEPTH)
    nc = bass.Bass("TRN2", target_bir_lowering=False)
    P = Prog(nc)

    def din(name, shape, dt=F32):
        if name in cfg.get("shrink", ()):
            shape = [1] * len(shape)
        return nc.dram_tensor(name, list(shape), dt, kind="ExternalInput").ap()

    def dscr(name, shape, dt, out=False):
        return nc.dram_tensor(name, list(shape), dt,
                              kind="ExternalOutput" if (out and dbg) else "Internal").ap()

    x_in = din("x", [L, D]); ctx_in = din("ctx", [T, D]); c_t = din("c_t", [128, 16, 2])
    w_ada = din("w_ada", [DEPTH, D, 6 * D]); bada_t = din("bada_t", [DEPTH, 128, 96])
    nmix_t = din("nmix_t", [DEPTH, 128, 16]); nffn_t = din("nffn_t", [DEPTH, 128, 16]); nfin_t = din("nfin_t", [128, 16])
    w_in = din("w_in", [DEPTH, D, IN_COLS]); w_out = din("w_out", [DEPTH, D, D])
    decA = din("decA", [DEPTH, 16]); decB = din("decB", [DEPTH, 2, 8])
    convw_t = din("convw_t", [DEPTH, 128, 4, 3]); sink_in = din("sink", [DEPTH, 8])
    na_bias = din("na_bias", [DEPTH, 8, 128, 3200])
    w_router = din("w_router", [DEPTH, D, NE])
    w_gate = din("w_gate", [DEPTH, NE, D, FF]); w_up = din("w_up", [DEPTH, NE, D, FF]); w_down = din("w_down", [DEPTH, NE, FF, D])
    cs_tab = din("cs_tab", [128, 16, 4, 64]); retc_in = din("retc", [128, 4, 128]); posv_in = din("posv", [128, 4])
    qrow_in = din("qrow", [128, 2, 128]); swamask_in = din("swamask", [128, 2, 128])
    y_out = nc.dram_tensor("y", [L, D], F32, kind="ExternalOutput").ap()

    XT = dscr("XT", [D, NT], F32, out=True)
    ZT = dscr("ZT", [NT, ZT_COLS], BF16, out=True)
    ZF = dscr("ZF", [ZF_ROWS, NT], BF16, out=True)
    YT = dscr("YT", [D, NT], BF16, out=True)
    YE = dscr("YE", [NE, NSL, D], BF16, out=True)
    H2D = dscr("H2D", [NT, D], BF16)
    MOE = dscr("MOE", [NT, D], F32)
    ADAD = dscr("ADAD", [DEPTH, 128, 96, 2], F32, out=True)
    IDXC = dscr("IDXC", [CAPC, NE], F32)
    DBG1 = dscr("DBG1", [NE, 2 * CAP], F32, out=True)
    XTv = XT.rearrange("(c p) n -> p c n", p=128)
    YTv = YT.rearrange("(c p) n -> p c n", p=128)

    PS = nc.alloc_psum_tensor("ps", [128, 8, 512], F32).ap()

    def bank(i):
        return PS[:, i, :]

    def bkey(i):
        return f"ps{i}"

    identF = nc.alloc_sbuf_tensor("identF", [128, 128], F32).ap()
    identB = nc.alloc_sbuf_tensor("identB", [128, 128], BF16).ap()
    onesF = nc.alloc_sbuf_tensor("onesF", [128, 128], F32).ap()
    iotaF = nc.alloc_sbuf_tensor("iotaF", [128, 2048], F32).ap()
    piota = nc.alloc_sbuf_tensor("piota", [128, 16], F32).ap()
    ada_sb = nc.alloc_sbuf_tensor("ada_sb", [128, DEPTH, 96, 2], F32).ap()
    scl = nc.alloc_sbuf_tensor("scl", [128, DEPTH, 2, 16, 2], F32).ap()
    nrm_sb = nc.alloc_sbuf_tensor("nrm_sb", [128, DEPTH, 2, 16], F32).ap()
    nfin_sb = nc.alloc_sbuf_tensor("nfin_sb", [128, 16], F32).ap()
    tmpi = nc.alloc_sbuf_tensor("tmpi", [128, 128], F32).ap()

    P.op("pool", lambda e: e.iota(tmpi, pattern=[[1, 128]], base=0, channel_multiplier=-1,
                                  allow_small_or_imprecise_dtypes=True), w=["tmpi"])
    P.op("dve", lambda e: e.tensor_scalar(identF, tmpi, 0.0, None, ALU.is_equal), r=["tmpi"], w=["identF"])
    P.op("dve", lambda e: e.tensor_copy(identB, identF), r=["identF"], w=["identB"])
    P.op("dve", lambda e: e.memset(onesF, 1.0), w=["onesF"])
    P.op("pool", lambda e: e.iota(iotaF, pattern=[[1, 2048]], base=0, channel_multiplier=0,
                                  allow_small_or_imprecise_dtypes=True), w=["iotaF"])
    P.op("pool", lambda e: e.iota(piota, pattern=[[128, 16]], base=0, channel_multiplier=1,
                                  allow_small_or_imprecise_dtypes=True), w=["piota"])
    P.dma("sp", nrm_sb[:, :, 0, :], nmix_t.rearrange("l p c -> p l c"), w=["nrm_sb"])
    P.dma("sp", nrm_sb[:, :, 1, :], nffn_t.rearrange("l p c -> p l c"), w=["nrm_sb"])
    P.dma("sp", nfin_sb, nfin_t, w=["nfin_sb"])

    NW = 4
    evac_rr = [0]

    def evac_eng():
        evac_rr[0] ^= 1
        return "act" if evac_rr[0] else "dve"

    def copy_op(eng, out, in_, r, w, scale=None):
        if eng == "act":
            if scale is None:
                P.op("act", lambda e: e.activation(out, in_, AF.Copy), r=r, w=w)
            else:
                P.op("act", lambda e: e.activation(out, in_, AF.Copy, scale=scale), r=r, w=w)
        else:
            if scale is None:
                P.op(eng, lambda e: e.tensor_copy(out, in_), r=r, w=w)
            else:
                P.op(eng, lambda e: e.tensor_scalar(out, in_, scale, None, ALU.mult), r=r, w=w)

    class Ring:
        def __init__(self, es):
            self.t = es.enter_context(nc.sbuf_tensor(U("wring"), [128, NW, 8192], BF16)).ap()
            self.i = 0

        def load(self, src_ap, view):
            s = self.i
            self.i = (self.i + 1) % NW
            dst = view(self.t[:, s, :])
            P.dma_sw(s, dst, src_ap, w=[f"w{s}"])
            return dst, f"w{s}"

    def phase_ada():
        with ExitStack() as es:
            ring = Ring(es)
            sc = es.enter_context(nc.sbuf_tensor(U("sc"), [128, 16, 2], BF16)).ap()
            cin = es.enter_context(nc.sbuf_tensor(U("cin"), [128, 16, 2], F32)).ap()
            bada = es.enter_context(nc.sbuf_tensor(U("bada"), [128, DEPTH, 96], F32)).ap()
            P.dma("sp", cin, c_t, w=["cin"])
            P.dma("sp", bada, bada_t.rearrange("l p c -> p l c"), w=["bada"])
            P.op("act", lambda e: e.activation(sc, cin, AF.Silu), r=["cin"], w=["sc"])
            for l in range(nlayers):
                for fg in range(24):
                    wt, wk = ring.load(w_ada[l, :, fg * 512:(fg + 1) * 512].rearrange("(c p) n -> p c n", p=128),
                                       lambda t: t.rearrange("p (c n) -> p c n", c=16))
                    bi = fg % 2
                    for m in range(4):
                        for c in range(16):
                            P.op("pe", lambda e: e.matmul(bank(bi)[:, m * 2:m * 2 + 2], wt[:, c, m * 128:(m + 1) * 128],
                                                          sc[:, c, :], start=(c == 0), stop=(c == 15)),
                                 r=[wk, "sc"], w=[bkey(bi)])
                    P.op("dve", lambda e: e.tensor_tensor(
                        ada_sb[:, l, fg * 4:(fg + 1) * 4, :],
                        bank(bi)[:, 0:8].rearrange("p (m r) -> p m r", r=2),
                        bada[:, l, fg * 4:(fg + 1) * 4].unsqueeze(2).to_broadcast([128, 4, 2]), ALU.add),
                        r=[bkey(bi), "bada"], w=["ada_sb"])
                for wi, c0 in ((0, 16), (1, 64)):
                    P.op("dve", lambda e: e.scalar_tensor_tensor(
                        scl[:, l, wi, :, :], ada_sb[:, l, c0:c0 + 16, :], 1.0,
                        nrm_sb[:, l, wi, :].unsqueeze(2).to_broadcast([128, 16, 2]), ALU.add, ALU.mult),
                        r=["ada_sb", "nrm_sb"], w=["scl"])
            if dbg:
                P.dma("sp", ADAD.rearrange("l p c r -> p l c r")[:, 0:nlayers], ada_sb[:, 0:nlayers], r=["ada_sb"], w=["ADAD"])
            P.barrier()

    def ada_vec(l, which, r):
        return ada_sb[:, l, which * 16:(which + 1) * 16, r]

    def norm_group(bufs, xg, gsz, scale_ap, shift_ap, out_fn, ssb=7):
        sq, rstd, tcs = bufs["sq"], bufs["rstd"], bufs["tc"]
        for c in range(16):
            s = c % 3
            P.op("act", lambda e: e.activation(sq[:, s, 0:gsz], xg[:, c, 0:gsz], AF.Square), r=["xg"], w=[f"sq{s}"])
            P.op("pe", lambda e: e.matmul(bank(ssb)[:, 0:gsz], onesF, sq[:, s, 0:gsz], start=(c == 0), stop=(c == 15)),
                 r=[f"sq{s}", "onesF"], w=[bkey(ssb)])
        P.op("act", lambda e: e.activation(rstd[:, 0, 0:gsz], bank(ssb)[:, 0:gsz], AF.Sqrt, bias=bufs["eps"][:, 0:1], scale=1.0 / D),
             r=[bkey(ssb), "eps"], w=["rstd0"])
        P.op("dve", lambda e: e.reciprocal(rstd[:, 1, 0:gsz], rstd[:, 0, 0:gsz]), r=["rstd0"], w=["rstd1"])
        for c in range(16):
            s = c % 3
            P.op("dve", lambda e: e.scalar_tensor_tensor(tcs[:, s, 0:gsz], xg[:, c, 0:gsz], scale_ap[:, c:c + 1],
                                                         rstd[:, 1, 0:gsz], ALU.mult, ALU.mult),
                 r=["xg", "rstd1", "scl", "ada_sb", "nfin_sb"], w=[f"tc{s}"])
            out_fn(c, tcs[:, s, 0:gsz], f"tc{s}")

    def alloc_norm_bufs(es):
        b = {}
        b["sq"] = es.enter_context(nc.sbuf_tensor(U("sq"), [128, 3, 512], F32)).ap()
        b["rstd"] = es.enter_context(nc.sbuf_tensor(U("rstd"), [128, 2, 512], F32)).ap()
        b["tc"] = es.enter_context(nc.sbuf_tensor(U("tcs"), [128, 3, 512], F32)).ap()
        b["eps"] = es.enter_context(nc.sbuf_tensor(U("epsb"), [128, 1], F32)).ap()
        P.op("dve", lambda e: e.memset(b["eps"], EPS), w=["eps"])
        return b

    def phase_norm1(l, hT, es_outer):
        with ExitStack() as es:
            xg = es.enter_context(nc.sbuf_tensor(U("xg"), [128, 16, 512], F32)).ap()
            nb = alloc_norm_bufs(es)
            xin = None
            if l == 0:
                xin = es.enter_context(nc.sbuf_tensor(U("xin"), [128, 2, 2048], F32)).ap()
            for gi, (g0, gsz) in enumerate(GROUPS):
                r = 1 if g0 >= L else 0
                if l == 0:
                    ntile = gsz // 128
                    for tt in range(ntile):
                        n0 = g0 + tt * 128
                        s = tt % 2
                        src = x_in[n0:n0 + 128, :] if n0 < L else ctx_in[n0 - L:n0 - L + 128, :]
                        P.dma("sp", xin[:, s, :], src, w=[f"xin{s}"])
                        for cb in range(4):
                            for cc in range(4):
                                c = cb * 4 + cc
                                P.op("pe", lambda e: e.transpose(bank(cb)[:, cc * 128:(cc + 1) * 128],
                                                                 xin[:, s, c * 128:(c + 1) * 128], identF),
                                     r=[f"xin{s}", "identF"], w=[bkey(cb)])
                            copy_op(evac_eng(), xg[:, cb * 4:(cb + 1) * 4, tt * 128:(tt + 1) * 128],
                                    bank(cb).rearrange("p (c n) -> p c n", c=4), r=[bkey(cb)], w=["xg"])
                    P.dma("sp", XTv[:, :, g0:g0 + gsz], xg[:, :, 0:gsz], r=["xg"], w=["XT"])
                else:
                    P.dma("sp", xg[:, :, 0:gsz], XTv[:, :, g0:g0 + gsz], r=["XT"], w=["xg"])

                def out_fn(c, tc, key, r=r, g0=g0, gsz=gsz):
                    P.op("act", lambda e: e.activation(hT[:, c, g0:g0 + gsz], tc, AF.Identity,
                                                       bias=ada_vec(l, 0, r)[:, c:c + 1], scale=1.0),
                         r=[key, "ada_sb"], w=["hT"])
                norm_group(nb, xg, gsz, scl[:, l, 0, :, r], None, out_fn)
            P.barrier()

    def phase_inproj(l, hT):
        with ExitStack() as es:
            ring = Ring(es)
            cs = es.enter_context(nc.sbuf_tensor(U("cs"), [128, 16, 4, 64], F32)).ap()
            zst = es.enter_context(nc.sbuf_tensor(U("zst"), [128, 3, 512], BF16)).ap()
            rt = es.enter_context(nc.sbuf_tensor(U("rt"), [128, 2, 2, 512], F32)).ap()
            P.dma("sp", cs, cs_tab, w=["cs"])
            zi = [0]
            bi = [0]
            tiles = [
                (0, 512, "T", ZT_RQ, "rope"), (512, 512, "T", ZT_RK, "ropek"),
                (1024, 512, "T", ZT_RV, "copy"), (1536, 512, "T", ZT_RG, "silu"),
                (2048, 512, "F", ZF_CB, "copy"), (2560, 512, "F", ZF_CC, "copy"), (3072, 512, "F", ZF_CH, "copy"),
                (3584, 512, "T", ZT_SQ, "ropeq_swa"), (4096, 256, "T", ZT_SK, "swakv"),
                (4352, 512, "F", ZF_NQ, "copy"), (4864, 512, "F", ZF_NK, "copy"),
                (5376, 512, "T", ZT_NV, "copy"),
            ]
            only = cfg.get("inproj_tiles")
            for ti, (c0, ncol, orient, doff, kind) in enumerate(tiles):
                if only is not None and ti not in only:
                    continue
                wt, wk = ring.load(w_in[l, :, c0:c0 + ncol].rearrange("(c p) n -> p c n", p=128),
                                   lambda t: t[:, 0:16 * ncol].rearrange("p (c n) -> p c n", c=16))
                if orient == "T":
                    for tt in range(18):
                        b = bi[0] = (bi[0] + 1) % 6
                        for c in range(16):
                            P.op("pe", lambda e: e.matmul(bank(b)[:, 0:ncol], hT[:, c, tt * 128:(tt + 1) * 128], wt[:, c, :],
                                                          start=(c == 0), stop=(c == 15)),
                                 r=["hT", wk], w=[bkey(b)])
                        zs = zi[0] = (zi[0] + 1) % 3
                        zk = f"zst{zs}"
                        zo = zst[:, zs, 0:ncol]
                        ps = bank(b)[:, 0:ncol]
                        lat = tt < 16

                        def rope(ps_ap, out_ap, nh, tab, perm=False):
                            rs = tt % 2
                            x4 = ps_ap.rearrange("p (h t d) -> p h t d", t=2, d=32)
                            a = rt[:, rs, 0, 0:nh * 64]
                            bb = rt[:, rs, 1, 0:nh * 64]
                            a3 = a.rearrange("p (h d) -> p h d", d=64)
                            b4 = bb.rearrange("p (h t d) -> p h t d", t=2, d=32)
                            ccb = cs[:, tt, tab, :].unsqueeze(1).to_broadcast([128, nh, 64])
                            ssn = cs[:, tt, tab + 1, 0:32].unsqueeze(1).to_broadcast([128, nh, 32])
                            ssp = cs[:, tt, tab + 1, 32:64].unsqueeze(1).to_broadcast([128, nh, 32])
                            P.op("dve", lambda e: e.tensor_tensor(a3, ps_ap.rearrange("p (h d) -> p h d", d=64), ccb, ALU.mult),
                                 r=[bkey(b), "cs"], w=[f"rta{rs}"])
                            P.op("dve", lambda e: e.tensor_tensor(b4[:, :, 0, :], x4[:, :, 1, :], ssn, ALU.mult),
                                 r=[bkey(b), "cs"], w=[f"rtb{rs}"])
                            P.op("dve", lambda e: e.tensor_tensor(b4[:, :, 1, :], x4[:, :, 0, :], ssp, ALU.mult),
                                 r=[bkey(b), "cs"], w=[f"rtb{rs}"])
                            if perm:
                                o = out_ap.rearrange("p (i g d) -> p g i d", g=2, d=64)
                                P.op("pool", lambda e: e.tensor_tensor(o, a.rearrange("p (g i d) -> p g i d", g=2, d=64),
                                                                       bb.rearrange("p (g i d) -> p g i d", g=2, d=64), ALU.add),
                                     r=[f"rta{rs}", f"rtb{rs}"], w=[zk])
                            else:
                                P.op("pool", lambda e: e.tensor_tensor(out_ap, a, bb, ALU.add),
                                     r=[f"rta{rs}", f"rtb{rs}"], w=[zk])

                        if kind == "copy":
                            copy_op(evac_eng(), zo, ps, r=[bkey(b)], w=[zk])
                        elif kind == "silu":
                            P.op("act", lambda e: e.activation(zo, ps, AF.Silu), r=[bkey(b)], w=[zk])
                        elif kind == "rope":
                            if lat:
                                rope(ps, zo, 8, 0)
                            else:
                                copy_op("act", zo, ps, r=[bkey(b)], w=[zk])
                        elif kind == "ropek":
                            if lat:
                                rope(ps, zo, 8, 2)
                            else:
                                copy_op("act", zo, ps, r=[bkey(b)], w=[zk], scale=0.125)
                        elif kind == "ropeq_swa":
                            if lat:
                                rope(ps, zo, 8, 0, perm=True)
                            else:
                                P.op("act", lambda e: e.activation(zo.rearrange("p (i g d) -> p g i d", g=2, d=64),
                                                                   ps.rearrange("p (g i d) -> p g i d", g=2, d=64), AF.Copy),
                                     r=[bkey(b)], w=[zk])
                        elif kind == "swakv":
                            if lat:
                                rope(ps[:, 0:128], zo[:, 0:128], 2, 0)
                            else:
                                copy_op("act", zo[:, 0:128], ps[:, 0:128], r=[bkey(b)], w=[zk])
                            copy_op("dve", zo[:, 128:256], ps[:, 128:256], r=[bkey(b)], w=[zk])
                        P.dma("sp", ZT[tt * 128:(tt + 1) * 128, doff:doff + ncol], zo, r=[zk], w=["ZT"])
                else:
                    for m in range(ncol // 128):
                        for (g0, gsz) in GROUPS:
                            b = bi[0] = (bi[0] + 1) % 6
                            for c in range(16):
                                P.op("pe", lambda e: e.matmul(bank(b)[:, 0:gsz], wt[:, c, m * 128:(m + 1) * 128], hT[:, c, g0:g0 + gsz],
                                                              start=(c == 0), stop=(c == 15)),
                                     r=["hT", wk], w=[bkey(b)])
                            zs = zi[0] = (zi[0] + 1) % 3
                            zk = f"zst{zs}"
                            copy_op(evac_eng(), zst[:, zs, 0:gsz], bank(b)[:, 0:gsz], r=[bkey(b)], w=[zk])
                            P.dma("sp", ZF[doff + m * 128:doff + (m + 1) * 128, g0:g0 + gsz], zst[:, zs, 0:gsz], r=[zk], w=["ZF"])
            P.barrier()

    def phase_ret(l):
        need_ctx = l < DEPTH - 1
        with ExitStack() as es:
            def sb(name, shape, dt=F32):
                return es.enter_context(nc.sbuf_tensor(U(name), shape, dt)).ap()
            lgA = sb("lgA", [128, 16]); lgP = sb("lgP", [128, 8]); tA = sb("tA", [128, 16]); tP = sb("tP", [128, 8])
            retc = sb("retc", [128, 4, 128]); posv = sb("posv", [128, 4]); qrow = sb("qrow", [128, 2, 128])
            zeta = sb("zeta", [128, 2, 8]); cdP = sb("cdP", [128, 8]); xiT = sb("xiT", [128, 2, 4, 128])
            dm = sb("dm", [128, 8, 128]); dtmp = sb("dtmp", [128, 2, 128])
            epsb = sb("epsb", [128, 1])
            SfAll = sb("SfAll", [128, 18, 512], BF16)
            stf = sb("stf", [128, 512]); stb = sb("stb", [128, 512]); stb_bf = sb("stb_bf", [128, 512], BF16)
            kvin = sb("kvin", [128, 2, 1024], BF16); zin = sb("zin", [128, 2, 2048], BF16)
            kz = sb("kz", [128, 2, 512], BF16)
            qTe = sb("qTe", [128, 2, 512], BF16); qTo = sb("qTo", [128, 2, 512], BF16); kT = sb("kT", [128, 2, 512], BF16)
            bmask = sb("bmask", [128, 512]); tkv = sb("tkv", [128, 512])
            qxf = sb("qxf", [128, 2, 512], BF16); qxb = sb("qxb", [128, 2, 512], BF16)
            inn = sb("inn", [128, 2, 1024], BF16)
            sqv = sb("sqv", [128, 512]); t1 = sb("t1", [128, 512]); t2 = sb("t2", [128, 512])
            st8 = sb("st8", [128, 8, 8])
            yr = sb("yr", [128, 2, 512], BF16); yst = sb("yst", [128, 2, 4, 512], BF16)
            P.op("dve", lambda e: e.memset(epsb, EPS), w=["epsb"])
            P.dma("sp", retc, retc_in, w=["retc"]); P.dma("sp", posv, posv_in, w=["posv"]); P.dma("sp", qrow, qrow_in, w=["qrow"])
            P.dma("sp", tA, decA[l:l + 1, :].partition_broadcast(128), w=["tA"])
            P.dma("sp", tP[0:64, :], decB[l, 0:1, :].partition_broadcast(64), w=["tP"])
            P.dma("sp", tP[64:128, :], decB[l, 1:2, :].partition_broadcast(64), w=["tP"])
            for (src, dst, k1, k2) in ((tA, lgA, "tA", "lgA"), (tP, lgP, "tP", "lgP")):
                P.op("act", lambda e: e.activation(src, src, AF.Exp, scale=-1.0), r=[k1], w=[k1])
                P.op("dve", lambda e: e.tensor_scalar(src, src, 1.0, None, ALU.add), r=[k1], w=[k1])
                P.op("act", lambda e: e.activation(src, src, AF.Ln), r=[k1], w=[k1])
                P.op("dve", lambda e: e.tensor_scalar(dst, src, -1.0, None, ALU.mult), r=[k1], w=[k2])
            P.op("act", lambda e: e.activation(zeta[:, 0, :], lgA[:, 0:8], AF.Exp, scale=posv[:, 1:2]), r=["lgA", "posv"], w=["zeta"])
            P.op("act", lambda e: e.activation(zeta[:, 1, :], lgA[:, 8:16], AF.Exp, scale=posv[:, 3:4]), r=["lgA", "posv"], w=["zeta"])
            P.op("act", lambda e: e.activation(cdP, lgP, AF.Exp, scale=128.0), r=["lgP"], w=["cdP"])
            for dr in range(2):
                for j in range(4):
                    P.op("act", lambda e: e.activation(xiT[:, dr, j, :], qrow[:, dr, :], AF.Exp, scale=lgP[:, dr * 4 + j:dr * 4 + j + 1]),
                         r=["lgP", "qrow"], w=["xiT"])
            for h in range(8):
                P.op("act", lambda e: e.activation(dtmp[:, 0, :], retc[:, 0, :], AF.Exp, scale=lgA[:, h:h + 1]), r=["lgA", "retc"], w=["dtmp0"])
                P.op("act", lambda e: e.activation(dtmp[:, 1, :], retc[:, 2, :], AF.Exp, scale=lgA[:, 8 + h:9 + h]), r=["lgA", "retc"], w=["dtmp1"])
                P.op("dve", lambda e: e.tensor_tensor(dtmp[:, 0, :], dtmp[:, 0, :], retc[:, 1, :], ALU.mult), r=["dtmp0", "retc"], w=["dtmp0"])
                P.op("dve", lambda e: e.tensor_tensor(dtmp[:, 1, :], dtmp[:, 1, :], retc[:, 3, :], ALU.mult), r=["dtmp1", "retc"], w=["dtmp1"])
                P.op("dve", lambda e: e.tensor_tensor(dm[:, h, :], dtmp[:, 0, :], dtmp[:, 1, :], ALU.add), r=["dtmp0", "dtmp1"], w=["dm"])
            P.op("pool", lambda e: e.memset(qTe, 0.0), w=["qTe0", "qTe1"])
            P.op("pool", lambda e: e.memset(qTo, 0.0), w=["qTo0", "qTo1"])
            bm4 = bmask.rearrange("p (j t d) -> p j t d", t=2, d=64)
            P.op("pool", lambda e: e.memset(bmask, 0.0), w=["bmask"])
            P.op("pool", lambda e: e.memset(bm4[0:64, :, 0, :], 1.0), r=["bmask"], w=["bmask"])
            P.op("pool", lambda e: e.memset(bm4[64:128, :, 1, :], 1.0), r=["bmask"], w=["bmask"])
            P.op("dve", lambda e: e.memset(stf, 0.0), w=["stf"])
            P.op("dve", lambda e: e.memset(stb, 0.0), w=["stb"])
            P.op("dve", lambda e: e.memset(stb_bf, 0.0), w=["stb_bf"])

            def kv_update(i, dr, kin, kk, st, stk, s):
                P.op("dve", lambda e: e.tensor_tensor(kz[:, s, :].rearrange("p (h d) -> p h d", d=64),
                                                      kin[:, 0:512].rearrange("p (h d) -> p h d", d=64),
                                                      zeta[:, dr, :].unsqueeze(2).to_broadcast([128, 8, 64]), ALU.mult),
                     r=[kk, "zeta"], w=[f"kz{s}"])
                for j in range(4):
                    P.op("pe", lambda e: e.matmul(bank(4)[:, j * 128:(j + 1) * 128], kz[:, s, j * 128:(j + 1) * 128],
                                                  kin[:, 512 + j * 128:512 + (j + 1) * 128], start=True, stop=True),
                         r=[f"kz{s}", kk], w=[bkey(4)])
                P.op("dve", lambda e: e.tensor_tensor(st.rearrange("p (j v) -> p j v", v=128), st.rearrange("p (j v) -> p j v", v=128),
                                                      cdP[:, dr * 4:dr * 4 + 4].unsqueeze(2).to_broadcast([128, 4, 128]), ALU.mult),
                     r=[stk, "cdP"], w=[stk])
                P.op("dve", lambda e: e.tensor_tensor(tkv, bank(4), bmask, ALU.mult), r=[bkey(4), "bmask"], w=["tkv"])
                P.op("dve", lambda e: e.tensor_tensor(st, st, tkv, ALU.add), r=[stk, "tkv"], w=[stk])

            cut = cfg.get("ret_cut", 99)
            fwd_order = [16, 17] + list(range(16)) if cut >= 1 else []
            bwd_order = [17, 16] + list(range(15, -1, -1)) if cut >= 2 else []
            for n, i in enumerate(fwd_order):
                s = n % 2
                P.dma("sp", kvin[:, s, :], ZT[i * 128:(i + 1) * 128, ZT_RK:ZT_RK + 1024], r=["ZT"], w=[f"kvin{s}"])
                P.op("act", lambda e: e.activation(SfAll[:, i, :], stf, AF.Copy), r=["stf"], w=["SfAll"])
                if n < len(fwd_order) - 1:
                    kv_update(i, 0, kvin[:, s, :], f"kvin{s}", stf, "stf", s)
            for n, i in enumerate(bwd_order):
                s = n % 2
                zk = f"zin{s}"
                P.dma("sp", zin[:, s, :], ZT[i * 128:(i + 1) * 128, 0:2048], r=["ZT"], w=[zk])
                need_out = (i < 16) or need_ctx
                if cut < 2.05:
                    continue
                if need_out:
                    yb = 3 if s == 0 else 7
                    TPq, TPk = bank(0), bank(6)
                    for j in range(4):
                        P.op("pe", lambda e: e.matmul(TPq[:, j * 128:(j + 1) * 128], zin[:, s, j * 128:(j + 1) * 128], identB, start=True, stop=True),
                             r=[zk, "identB"], w=[bkey(0)])
                        P.op("pe", lambda e: e.matmul(TPk[:, j * 128:(j + 1) * 128], zin[:, s, 512 + j * 128:512 + (j + 1) * 128], identB, start=True, stop=True),
                             r=[zk, "identB"], w=[bkey(6)])
                    if cut < 2.15:
                        continue
                    P.op("act", lambda e: e.activation(qTe[0:64, s, :], TPq[0:64, :], AF.Copy), r=[bkey(0)], w=[f"qTe{s}"])
                    P.op("act", lambda e: e.activation(qTo[64:128, s, :], TPq[64:128, :], AF.Copy), r=[bkey(0)], w=[f"qTo{s}"])
                    P.op("act", lambda e: e.activation(kT[:, s, :], TPk, AF.Copy), r=[bkey(6)], w=[f"kT{s}"])
                    if cut < 2.25:
                        continue
                    rv = cfg.get("ret_var", 0)
                    if rv == 1:
                        P.op("dve", lambda e: e.tensor_tensor(qxf[:, s, :], TPq, bmask, ALU.mult),
                             r=[bkey(0), "bmask"], w=[f"qxf{s}"])
                    elif rv == 2:
                        P.op("dve", lambda e: e.tensor_copy(qxf[:, s, :], TPq), r=[bkey(0)], w=[f"qxf{s}"])
                    elif rv == 3:
                        P.op("dve", lambda e: e.tensor_tensor(qxf[:, s, :], qTe[:, s, :], xiT[:, 0, :, :].rearrange("p j q -> p (j q)"), ALU.mult),
                             r=[f"qTe{s}", "xiT"], w=[f"qxf{s}"])
                    else:
                        P.op("dve", lambda e: e.tensor_tensor(qxf[:, s, :], TPq, xiT[:, 0, :, :].rearrange("p j q -> p (j q)"), ALU.mult),
                             r=[bkey(0), "xiT"], w=[f"qxf{s}"])
                        P.op("dve", lambda e: e.tensor_tensor(qxb[:, s, :], TPq, xiT[:, 1, :, :].rearrange("p j q -> p (j q)"), ALU.mult),
                             r=[bkey(0), "xiT"], w=[f"qxb{s}"])
                    if cut < 3:
                        continue
                    for h in range(8):
                        j, hf = h // 2, h % 2
                        rows = slice(hf * 64, hf * 64 + 64)
                        sb_ = 1 + h // 4
                        qz = qTe if hf == 0 else qTo
                        P.op("pe", lambda e: e.matmul(bank(sb_)[:, (h % 4) * 128:(h % 4 + 1) * 128], kT[:, s, j * 128:(j + 1) * 128],
                                                      qz[:, s, j * 128:(j + 1) * 128], start=True, stop=True),
                             r=[f"kT{s}", f"qTe{s}", f"qTo{s}"], w=[bkey(sb_)])
                    for hb in range(2):
                        P.op("dve", lambda e: e.tensor_tensor(inn[:, s, hb * 512:(hb + 1) * 512], bank(1 + hb),
                                                              dm[:, hb * 4:(hb + 1) * 4, :].rearrange("p h q -> p (h q)"), ALU.mult),
                             r=[bkey(1 + hb), "dm"], w=[f"inn{s}"])
                    if cut < 4:
                        continue
                    for j in range(4):
                        pc = slice(j * 128, (j + 1) * 128)
                        P.op("pe", lambda e: e.matmul(bank(yb)[:, pc], qxf[:, s, pc], SfAll[:, i, pc], start=True, stop=False),
                             r=[f"qxf{s}", "SfAll"], w=[bkey(yb)])
                        P.op("pe", lambda e: e.matmul(bank(yb)[:, pc], qxb[:, s, pc], stb_bf[:, pc], start=False, stop=False),
                             r=[f"qxb{s}", "stb_bf"], w=[bkey(yb)])
                        for hf in range(2):
                            h = 2 * j + hf
                            P.op("pe", lambda e: e.matmul(bank(yb)[:, h * 64:(h + 1) * 64], inn[:, s, h * 128:(h + 1) * 128],
                                                          zin[:, s, 1024 + h * 64:1024 + (h + 1) * 64], start=False, stop=(hf == 1)),
                                 r=[f"inn{s}", zk], w=[bkey(yb)])
                    if cut < 5:
                        continue
                    Yv = bank(yb).rearrange("p (h d) -> p h d", d=64)
                    s1, s2, mean, msq, var, sd, rstd = [st8[:, k, :] for k in range(7)]
                    P.op("dve", lambda e: e.tensor_reduce(s1, Yv, AX.X, ALU.add), r=[bkey(yb)], w=["st_s1"])
                    P.op("act", lambda e: e.activation(sqv, bank(yb), AF.Square), r=[bkey(yb)], w=["sqv"])
                    P.op("dve", lambda e: e.tensor_reduce(s2, sqv.rearrange("p (h d) -> p h d", d=64), AX.X, ALU.add), r=["sqv"], w=["st_s2"])
                    P.op("dve", lambda e: e.tensor_scalar(mean, s1, 1.0 / 64, None, ALU.mult), r=["st_s1"], w=["st_mean"])
                    P.op("dve", lambda e: e.tensor_tensor(msq, mean, mean, ALU.mult), r=["st_mean"], w=["st_msq"])
                    P.op("dve", lambda e: e.scalar_tensor_tensor(var, s2, 1.0 / 64, msq, ALU.mult, ALU.subtract), r=["st_s2", "st_msq"], w=["st_var"])
                    P.op("act", lambda e: e.activation(sd, var, AF.Sqrt, bias=epsb[:, 0:1], scale=1.0), r=["st_var", "epsb"], w=["st_sd"])
                    P.op("dve", lambda e: e.reciprocal(rstd, sd), r=["st_sd"], w=["st_rstd"])
                    t1v = t1.rearrange("p (h d) -> p h d", d=64)
                    t2v = t2.rearrange("p (h d) -> p h d", d=64)
                    P.op("dve", lambda e: e.tensor_tensor(t1v, Yv, mean.unsqueeze(2).to_broadcast([128, 8, 64]), ALU.subtract),
                         r=[bkey(yb), "st_mean"], w=["t1"])
                    P.op("dve", lambda e: e.tensor_tensor(t2v, t1v, rstd.unsqueeze(2).to_broadcast([128, 8, 64]), ALU.mult),
                         r=["t1", "st_rstd"], w=["t2"])
                    P.op("pool", lambda e: e.tensor_tensor(yr[:, s, :], t2, zin[:, s, 1536:2048], ALU.mult), r=["t2", zk], w=[f"yr{s}"])
                    if cut < 6:
                        continue
                    TY = bank(5)
                    for j in range(4):
                        P.op("pe", lambda e: e.matmul(TY[:, j * 128:(j + 1) * 128], yr[:, s, j * 128:(j + 1) * 128], identB, start=True, stop=True),
                             r=[f"yr{s}", "identB"], w=[bkey(5)])
                    if i < 16:
                        grp, pos, gw = i // 4, i % 4, 512
                        g0 = grp * 512
                    else:
                        grp, pos, gw = 4, i - 16, 256
                        g0 = 2048
                    ys = grp % 2
                    P.op("act", lambda e: e.activation(yst[:, ys, :, pos * 128:(pos + 1) * 128], TY[:, 0:512].rearrange("p (j n) -> p j n", j=4), AF.Copy),
                         r=[bkey(5)], w=[f"yst{ys}"])
                    if pos == 0:
                        P.dma("sp", YTv[:, 0:4, g0:g0 + gw], yst[:, ys, :, 0:gw], r=[f"yst{ys}"], w=["YT"])
                if n < len(bwd_order) - 1:
                    kv_update(i, 1, zin[:, s, 512:1536], zk, stb, "stb", s)
                    P.op("act", lambda e: e.activation(stb_bf, stb, AF.Copy), r=["stb"], w=["stb_bf"])
            P.barrier()

    def phase_conv(l):
        need_ctx = l < DEPTH - 1
        with ExitStack() as es:
            def sb(name, shape, dt=F32):
                return es.enter_context(nc.sbuf_tensor(U(name), shape, dt)).ap()
            cw = sb("cw", [128, 4, 3])
            bch = sb("bch", [128, 2, 3, NT], BF16)
            u = sb("u", [128, 2, NT]); yv = sb("yv", [128, 2, NT]); ob = sb("ob", [128, 2, NT], BF16)
            P.dma("sp", cw, convw_t[l], w=["cw"])
            nend = NT if need_ctx else L
            seqs = [(0, L)] + ([(L, NT)] if need_ctx else [])
            for cc in range(4):
                s = cc % 2
                for k3, off in enumerate((ZF_CB, ZF_CC, ZF_CH)):
                    P.dma("sp", bch[:, s, k3, 0:nend], ZF[off + cc * 128:off + (cc + 1) * 128, 0:nend], r=["ZF"], w=[f"bch{s}"])
                P.op("dve", lambda e: e.tensor_tensor(u[:, s, 0:nend], bch[:, s, 1, 0:nend], bch[:, s, 2, 0:nend], ALU.mult), r=[f"bch{s}"], w=[f"u{s}"])
                P.op("act", lambda e: e.activation(yv[:, s, 0:nend], u[:, s, 0:nend], AF.Copy, scale=cw[:, cc, 1:2]), r=[f"u{s}", "cw"], w=[f"yv{s}"])
                for (s0, s1) in seqs:
                    P.op("dve", lambda e: e.scalar_tensor_tensor(yv[:, s, s0 + 1:s1], u[:, s, s0:s1 - 1], cw[:, cc, 0:1], yv[:, s, s0 + 1:s1], ALU.mult, ALU.add),
                         r=[f"u{s}", "cw", f"yv{s}"], w=[f"yv{s}"])
                    P.op("dve", lambda e: e.scalar_tensor_tensor(yv[:, s, s0:s1 - 1], u[:, s, s0 + 1:s1], cw[:, cc, 2:3], yv[:, s, s0:s1 - 1], ALU.mult, ALU.add),
                         r=[f"u{s}", "cw", f"yv{s}"], w=[f"yv{s}"])
                P.op("pool", lambda e: e.tensor_tensor(ob[:, s, 0:nend], yv[:, s, 0:nend], bch[:, s, 0, 0:nend], ALU.mult), r=[f"yv{s}", f"bch{s}"], w=[f"ob{s}"])
                P.dma("sp", YT[512 + cc * 128:512 + (cc + 1) * 128, 0:nend], ob[:, s, 0:nend], r=[f"ob{s}"], w=["YT"])
            P.barrier()

    def phase_swa(l):
        need_ctx = l < DEPTH - 1
        with ExitStack() as es:
            def sb(name, shape, dt=F32):
                return es.enter_context(nc.sbuf_tensor(U(name), shape, dt)).ap()
            esink = sb("esink", [128, 8]); mkf = sb("mkf", [128, 2, 128]); mk = sb("mk", [128, 2, 128], BF16)
            QT0 = sb("QT0", [128, 4, NT], BF16); QT1 = sb("QT1", [128, 4, NT], BF16)
            KT = sb("KT", [128, NT], BF16); Vp = sb("Vp", [128, 18, 2, 65], BF16)
            P.op("pool", lambda e: e.memset(QT0[64:128], 0.0), w=["QT"])
            P.op("pool", lambda e: e.memset(QT1[0:64], 0.0), w=["QT"])
            zin = sb("zin", [128, 2, 768], BF16)
            PT = sb("PT", [128, 2, 5, 512], BF16)
            den = sb("den", [128, 2, 4]); rec = sb("rec", [128, 2, 4])
            ysw = sb("ysw", [128, 2, 512], BF16); yst = sb("yst", [128, 2, 4, 512], BF16)
            P.dma("sp", esink, sink_in[l:l + 1, :].partition_broadcast(128), w=["esink"])
            P.op("act", lambda e: e.activation(esink, esink, AF.Exp), r=["esink"], w=["esink"])
            P.dma("sp", mkf, swamask_in, w=["mkf"])
            P.op("dve", lambda e: e.tensor_copy(mk, mkf), r=["mkf"], w=["mk"])
            P.op("pool", lambda e: e.memset(Vp, 1.0), w=["Vp"])
            for tt in range(18):
                s = tt % 2
                zk = f"zin{s}"
                P.dma("sp", zin[:, s, :], ZT[tt * 128:(tt + 1) * 128, ZT_SQ:ZT_SQ + 768], r=["ZT"], w=[zk])
                tb = s
                TP = bank(tb)
                TPk = bank(2 + s)
                for i4 in range(4):
                    P.op("pe", lambda e: e.matmul(TP[:, i4 * 128:(i4 + 1) * 128], zin[:, s, i4 * 128:(i4 + 1) * 128], identB, start=True, stop=True),
                         r=[zk, "identB"], w=[bkey(tb)])
                P.op("pe", lambda e: e.matmul(TPk[:, 0:128], zin[:, s, 512:640], identB, start=True, stop=True), r=[zk, "identB"], w=[bkey(2 + s)])
                P.op("act", lambda e: e.activation(QT0[0:64, :, tt * 128:(tt + 1) * 128], TP[0:64, 0:512].rearrange("p (i n) -> p i n", i=4), AF.Copy),
                     r=[bkey(tb), "QT"], w=["QT"])
                P.op("act", lambda e: e.activation(QT1[64:128, :, tt * 128:(tt + 1) * 128], TP[64:128, 0:512].rearrange("p (i n) -> p i n", i=4), AF.Copy),
                     r=[bkey(tb), "QT"], w=["QT"])
                P.op("dve", lambda e: e.tensor_copy(KT[:, tt * 128:(tt + 1) * 128], TPk[:, 0:128]), r=[bkey(2 + s)], w=["KT"])
                P.op("pool", lambda e: e.tensor_copy(Vp[:, tt, :, 0:64], zin[:, s, 640:768].rearrange("p (k d) -> p k d", d=64)), r=[zk, "Vp"], w=["Vp"])
            nblk = 18 if need_ctx else 16
            order = ([17, 16] if need_ctx else []) + list(range(15, -1, -1))
            it = 0
            for blk in order:
                if blk < 16:
                    tiles = [(16, None), (17, None)]
                    if blk > 0:
                        tiles.append((blk - 1, 0))
                    tiles.append((blk, None))
                    if blk < 15:
                        tiles.append((blk + 1, 1))
                else:
                    tiles = [(16, None), (17, None)]
                ysb = it % 2
                for kv in range(2):
                    ps = it % 2
                    it += 1
                    rows = slice(kv * 64, kv * 64 + 64)
                    for jj, (kt, mi) in enumerate(tiles):
                        b = 2 + (jj % 3)
                        QZ = QT0 if kv == 0 else QT1
                        P.op("pe", lambda e: e.matmul(bank(b), KT[:, kt * 128:(kt + 1) * 128], QZ[:, :, blk * 128:(blk + 1) * 128],
                                                      start=True, stop=True), r=["KT", "QT"], w=[bkey(b)])
                        P.op("act", lambda e: e.activation(PT[:, ps, jj, :], bank(b), AF.Exp, scale=0.125), r=[bkey(b)], w=[f"PT{ps}_{jj}"])
                        if mi is not None:
                            P.op("pool", lambda e: e.tensor_tensor(PT[:, ps, jj, :].rearrange("p (i q) -> p i q", i=4),
                                                                   PT[:, ps, jj, :].rearrange("p (i q) -> p i q", i=4),
                                                                   mk[:, mi, :].unsqueeze(1).to_broadcast([128, 4, 128]), ALU.mult),
                                 r=[f"PT{ps}_{jj}", "mk"], w=[f"PT{ps}_{jj}"])
                    ob_ = 5 + ps
                    for i4 in range(4):
                        for jj, (kt, mi) in enumerate(tiles):
                            P.op("pe", lambda e: e.matmul(bank(ob_)[:, i4 * 65:(i4 + 1) * 65], PT[:, ps, jj, i4 * 128:(i4 + 1) * 128], Vp[:, kt, kv, :],
                                                          start=(jj == 0), stop=(jj == len(tiles) - 1)),
                                 r=[f"PT{ps}_{jj}", "Vp"], w=[bkey(ob_)])
                    Ov = bank(ob_)[:, 0:260].rearrange("p (i d) -> p i d", d=65)
                    P.op("dve", lambda e: e.tensor_tensor(den[:, ps, :], Ov[:, :, 64], esink[:, kv * 4:(kv + 1) * 4], ALU.add),
                         r=[bkey(ob_), "esink"], w=[f"den{ps}"])
                    P.op("dve", lambda e: e.reciprocal(rec[:, ps, :], den[:, ps, :]), r=[f"den{ps}"], w=[f"rec{ps}"])
                    P.op("dve", lambda e: e.tensor_tensor(ysw[:, ysb, kv * 256:(kv + 1) * 256].rearrange("p (i d) -> p i d", d=64), Ov[:, :, 0:64],
                                                          rec[:, ps, :].unsqueeze(2).to_broadcast([128, 4, 64]), ALU.mult),
                         r=[bkey(ob_), f"rec{ps}"], w=[f"ysw{ysb}"])
                TY = bank(7)
                for j in range(4):
                    P.op("pe", lambda e: e.matmul(TY[:, j * 128:(j + 1) * 128], ysw[:, ysb, j * 128:(j + 1) * 128], identB, start=True, stop=True),
                         r=[f"ysw{ysb}", "identB"], w=[bkey(7)])
                if blk < 16:
                    grp, pos, gw, g0 = blk // 4, blk % 4, 512, (blk // 4) * 512
                else:
                    grp, pos, gw, g0 = 4, blk - 16, 256, 2048
                ys = grp % 2
                P.op("act", lambda e: e.activation(yst[:, ys, :, pos * 128:(pos + 1) * 128], TY[:, 0:512].rearrange("p (j n) -> p j n", j=4), AF.Copy),
                     r=[bkey(7)], w=[f"yst{ys}"])
                if pos == 0:
                    P.dma("sp", YTv[:, 8:12, g0:g0 + gw], yst[:, ys, :, 0:gw], r=[f"yst{ys}"], w=["YT"])
            P.barrier()

    def phase_na(l):
        need_ctx = l < DEPTH - 1
        with ExitStack() as es:
            def sb(name, shape, dt=F32):
                return es.enter_context(nc.sbuf_tensor(U(name), shape, dt)).ap()
            QTe = sb("QTne", [128, 4, NT], BF16); QTo = sb("QTno", [128, 4, NT], BF16); KT = sb("KTn", [128, 4, NT], BF16)
            Vp = sb("Vpn", [128, 18, 8, 65], BF16); vtmp = sb("vtmp", [128, 2, 512], BF16)
            bias = sb("biasn", [128, 2, 25, 128])
            PT = sb("PTn", [128, 2, 7, 128], BF16); tmp = sb("tmpn", [128, 2, 640])
            rec = sb("recn", [128, 2, 1]); yna = sb("yna", [128, 18, 512], BF16)
            yst = sb("ystn", [128, 2, 4, 512], BF16)
            P.op("pool", lambda e: e.memset(QTe[64:128], 0.0), w=["QTe"])
            P.op("pool", lambda e: e.memset(QTo[0:64], 0.0), w=["QTo"])
            zq = ZF[ZF_NQ:ZF_NQ + 512, :].rearrange("(j p) n -> p j n", p=128)
            P.dma("sp", QTe[0:64], zq[0:64], r=["ZF", "QTe"], w=["QTe"])
            P.dma("sp", QTo[64:128], zq[64:128], r=["ZF", "QTo"], w=["QTo"])
            P.dma("sp", KT, ZF[ZF_NK:ZF_NK + 512, :].rearrange("(j p) n -> p j n", p=128), r=["ZF"], w=["KT"])
            P.op("pool", lambda e: e.memset(Vp, 1.0), w=["Vp"])
            for tt in range(18):
                s = tt % 2
                P.dma("sp", vtmp[:, s, :], ZT[tt * 128:(tt + 1) * 128, ZT_NV:ZT_NV + 512], r=["ZT"], w=[f"vtmp{s}"])
                P.op("pool", lambda e: e.tensor_copy(Vp[:, tt, :, 0:64], vtmp[:, s, :].rearrange("p (h d) -> p h d", d=64)),
                     r=[f"vtmp{s}", "Vp"], w=["Vp"])
            nq = 18 if need_ctx else 16
            it = 0
            for h in range(8):
                j, hf = h // 2, h % 2
                rows = slice(hf * 64, hf * 64 + 64)
                bs = h % 2
                P.dma("sp", bias[:, bs, :, :].rearrange("p a q -> p (a q)"), na_bias[l, h], w=[f"bias{bs}"])
                for m in range(nq):
                    ps = it % 2
                    it += 1
                    lat = na_tiles(m) if m < 16 else []
                    tiles = [16, 17] + lat
                    nl = len(lat)
                    bA, bB = (0, 1) if ps == 0 else (2, 3)
                    for jj, kt in enumerate(tiles):
                        b = bA if jj < 4 else bB
                        QZ = QTe if hf == 0 else QTo
                        P.op("pe", lambda e: e.matmul(bank(b)[:, (jj % 4) * 128:(jj % 4 + 1) * 128], KT[:, j, kt * 128:(kt + 1) * 128],
                                                      QZ[:, j, m * 128:(m + 1) * 128], start=True, stop=True),
                             r=["KT", "QTe", "QTo"], w=[bkey(b)])
                    P.op("act", lambda e: e.activation(PT[:, ps, 0:2, :].rearrange("p a q -> p (a q)"), bank(bA)[:, 0:256], AF.Exp, scale=0.125),
                         r=[bkey(bA)], w=[f"PTa{ps}"])
                    if nl:
                        cl = na_cls(m)
                        P.op("dve", lambda e: e.scalar_tensor_tensor(tmp[:, ps, 0:256], bank(bA)[:, 256:512], 0.125,
                                                                     bias[:, bs, cl * 5:cl * 5 + 2, :].rearrange("p a q -> p (a q)"), ALU.mult, ALU.add),
                             r=[bkey(bA), f"bias{bs}"], w=[f"tmp{ps}"])
                        P.op("dve", lambda e: e.scalar_tensor_tensor(tmp[:, ps, 256:nl * 128], bank(bB)[:, 0:(nl - 2) * 128], 0.125,
                                                                     bias[:, bs, cl * 5 + 2:cl * 5 + nl, :].rearrange("p a q -> p (a q)"), ALU.mult, ALU.add),
                             r=[bkey(bB), f"bias{bs}"], w=[f"tmp{ps}"])
                        P.op("act", lambda e: e.activation(PT[:, ps, 2:2 + nl, :].rearrange("p a q -> p (a q)"), tmp[:, ps, 0:nl * 128], AF.Exp),
                             r=[f"tmp{ps}"], w=[f"PTb{ps}"])
                    ob_ = 4 + ps
                    for jj, kt in enumerate(tiles):
                        P.op("pe", lambda e: e.matmul(bank(ob_)[:, 0:65], PT[:, ps, jj, :], Vp[:, kt, h, :],
                                                      start=(jj == 0), stop=(jj == len(tiles) - 1)),
                             r=[f"PTa{ps}", f"PTb{ps}", "Vp"], w=[bkey(ob_)])
                    P.op("dve", lambda e: e.reciprocal(rec[:, ps, :], bank(ob_)[:, 64:65]), r=[bkey(ob_)], w=[f"rec{ps}"])
                    P.op("dve", lambda e: e.tensor_scalar(yna[:, m, h * 64:(h + 1) * 64], bank(ob_)[:, 0:64], rec[:, ps, 0:1], None, ALU.mult),
                         r=[bkey(ob_), f"rec{ps}"], w=["yna"])
            for m in range(nq):
                tb = 6 + (m % 2)
                TY = bank(tb)
                for jx in range(4):
                    P.op("pe", lambda e: e.matmul(TY[:, jx * 128:(jx + 1) * 128], yna[:, m, jx * 128:(jx + 1) * 128], identB, start=True, stop=True),
                         r=["yna", "identB"], w=[bkey(tb)])
                if m < 16:
                    grp, pos, gw, g0 = m // 4, m % 4, 512, (m // 4) * 512
                    last = pos == 3
                else:
                    grp, pos, gw, g0 = 4, m - 16, 256, 2048
                    last = pos == 1
                ys = grp % 2
                P.op("act", lambda e: e.activation(yst[:, ys, :, pos * 128:(pos + 1) * 128], TY[:, 0:512].rearrange("p (j n) -> p j n", j=4), AF.Copy),
                     r=[bkey(tb)], w=[f"yst{ys}"])
                if last:
                    P.dma("sp", YTv[:, 12:16, g0:g0 + gw], yst[:, ys, :, 0:gw], r=[f"yst{ys}"], w=["YT"])
            P.barrier()

    def phase_outproj(l):
        need_ctx = l < DEPTH - 1
        with ExitStack() as es:
            ring = Ring(es)
            ytsb = es.enter_context(nc.sbuf_tensor(U("ytsb"), [128, 16, NT], BF16)).ap()
            xt = es.enter_context(nc.sbuf_tensor(U("xt"), [128, 3, 512], F32)).ap()
            nend = NT if need_ctx else L
            for c4 in range(4):
                P.dma("sp", ytsb[:, c4 * 4:(c4 + 1) * 4, 0:nend], YTv[:, c4 * 4:(c4 + 1) * 4, 0:nend], r=["YT"], w=["ytsb"])
            groups = GROUPS if need_ctx else GROUPS[:4]
            xi = 0
            bi = 0
            for mg in range(4):
                wt, wk = ring.load(w_out[l, :, mg * 512:(mg + 1) * 512].rearrange("(c p) n -> p c n", p=128),
                                   lambda t: t.rearrange("p (c n) -> p c n", c=16))
                for mm in range(4):
                    m = mg * 4 + mm
                    for (g0, gsz) in groups:
                        r = 1 if g0 >= L else 0
                        xs = xi % 3
                        xi += 1
                        b = bi % 6
                        bi += 1
                        P.dma("sp", xt[:, xs, 0:gsz], XT[m * 128:(m + 1) * 128, g0:g0 + gsz], r=["XT"], w=[f"xt{xs}"])
                        for c in range(16):
                            P.op("pe", lambda e: e.matmul(bank(b)[:, 0:gsz], wt[:, c, mm * 128:(mm + 1) * 128], ytsb[:, c, g0:g0 + gsz],
                                                          start=(c == 0), stop=(c == 15)), r=[wk, "ytsb"], w=[bkey(b)])
                        P.op("dve", lambda e: e.scalar_tensor_tensor(xt[:, xs, 0:gsz], bank(b)[:, 0:gsz], ada_vec(l, 2, r)[:, m:m + 1],
                                                                     xt[:, xs, 0:gsz], ALU.mult, ALU.add),
                             r=[bkey(b), f"xt{xs}", "ada_sb"], w=[f"xt{xs}"])
                        P.dma("sp", XT[m * 128:(m + 1) * 128, g0:g0 + gsz], xt[:, xs, 0:gsz], r=[f"xt{xs}"], w=["XT"])
            P.barrier()

    def phase_ffn(l):
        need_ctx = l < DEPTH - 1
        grps = GROUPS if need_ctx else GROUPS[:4]
        nsl = NSL if need_ctx else CAP
        with ExitStack() as esf:
            def sbf(name, shape, dt=F32):
                return esf.enter_context(nc.sbuf_tensor(U(name), shape, dt)).ap()
            idxf = sbf("idxf", [16, CAP]); vals = sbf("vals", [16, CAP]); idxu = sbf("idxu", [16, CAP], U32)
            idxfc = sbf("idxfc", [16, CAPC]); valsc = sbf("valsc", [16, CAPC]); idxuc = sbf("idxuc", [16, CAPC], U32)
            idxT = sbf("idxT", [128, 16, 2]); gateT = sbf("gateT", [128, 16, 2])
            gateTc = sbf("gateTc", [32, 16]); idxTcu = sbf("idxTcu", [32, 16]); idxTcP = sbf("idxTcP", [128, 4])
            idxTu = sbf("idxTu", [128, 16, 2], U32); idxTcU = sbf("idxTcU", [32, 16], U32)
            with ExitStack() as esh:
                with ExitStack() as es:
                    def sb(name, shape, dt=F32):
                        return es.enter_context(nc.sbuf_tensor(U(name), shape, dt)).ap()
                    xg = sb("xg", [128, 16, 512]); nb = alloc_norm_bufs(es)
                    h32 = sb("h32", [128, 3, 512]); hg = sb("hg", [128, 16, 512], BF16)
                    wr = sb("wr", [128, 16, 16]); E_sb = sb("E_sb", [16, NT]); aff = sb("aff", [16, NT]); rs = sb("rs", [16, 512])
                    h2st = sb("h2st", [128, 2, 2048], BF16)
                    P.dma("sp", wr, w_router[l].rearrange("(c p) e -> p c e", p=128), w=["wr"])
                    zt = sb("zt", [128, 2048])
                    P.op("pool", lambda e: e.memset(zt, 0.0), w=["zt"])
                    for tz in range(18 if need_ctx else 16):
                        P.dma("sp", MOE[tz * 128:(tz + 1) * 128, :], zt, r=["zt"], w=["MOE"])
                    bi = 0
                    for (g0, gsz) in grps:
                        r = 1 if g0 >= L else 0
                        P.dma("sp", xg[:, :, 0:gsz], XTv[:, :, g0:g0 + gsz], r=["XT"], w=["xg"])

                        def out_fn(c, tc, key, r=r, gsz=gsz):
                            s = c % 3
                            P.op("act", lambda e: e.activation(h32[:, s, 0:gsz], tc, AF.Identity, bias=ada_vec(l, 3, r)[:, c:c + 1], scale=1.0),
                                 r=[key, "ada_sb"], w=[f"h32_{s}"])
                            P.op("pe", lambda e: e.matmul(bank(6)[0:16, 0:gsz], wr[:, c, :], h32[:, s, 0:gsz], start=(c == 0), stop=(c == 15)),
                                 r=["wr", f"h32_{s}"], w=[bkey(6)])
                            P.op("pool", lambda e: e.tensor_copy(hg[:, c, 0:gsz], h32[:, s, 0:gsz]), r=[f"h32_{s}"], w=["hg"])
                        norm_group(nb, xg, gsz, scl[:, l, 1, :, r], None, out_fn)
                        P.op("act", lambda e: e.activation(E_sb[:, g0:g0 + gsz], bank(6)[0:16, 0:gsz], AF.Exp), r=[bkey(6)], w=["E_sb"])
                        for tt in range(gsz // 128):
                            tile = (g0 // 128) + tt
                            for cb in range(4):
                                b = bi % 6
                                bi += 1
                                for cc in range(4):
                                    c = cb * 4 + cc
                                    P.op("pe", lambda e: e.matmul(bank(b)[:, cc * 128:(cc + 1) * 128], hg[:, c, tt * 128:(tt + 1) * 128], identB,
                                                                  start=True, stop=True), r=["hg", "identB"], w=[bkey(b)])
                                copy_op(evac_eng(), h2st[:, tile % 2, cb * 512:(cb + 1) * 512], bank(b), r=[bkey(b)], w=[f"h2st{tile % 2}"])
                            P.dma("sp", H2D[tile * 128:(tile + 1) * 128, :], h2st[:, tile % 2, :], r=[f"h2st{tile % 2}"], w=["H2D"])
                    for (g0, gsz) in grps:
                        P.op("pe", lambda e: e.matmul(bank(7)[0:16, 0:gsz], onesF[0:16, 0:16], E_sb[:, g0:g0 + gsz], start=True, stop=True),
                             r=["onesF", "E_sb"], w=[bkey(7)])
                        P.op("dve", lambda e: e.reciprocal(rs[:, 0:gsz], bank(7)[0:16, 0:gsz]), r=[bkey(7)], w=["rs"])
                        P.op("dve", lambda e: e.tensor_tensor(aff[:, g0:g0 + gsz], E_sb[:, g0:g0 + gsz], rs[:, 0:gsz], ALU.mult), r=["E_sb", "rs"], w=["aff"])
                    for (a0, n, k, vv, iu, ifl, kk) in ((0, L, CAP, vals, idxu, idxf, "l"),) + (((L, T, CAPC, valsc, idxuc, idxfc, "c"),) if need_ctx else ()):
                        aw = aff[:, a0:a0 + n]
                        for it in range(k // 8):
                            v8 = vv[:, it * 8:(it + 1) * 8]
                            P.op("dve", lambda e: e.max(v8, aw), r=["aff"], w=["v8" + kk])
                            P.op("dve", lambda e: e.max_index(iu[:, it * 8:(it + 1) * 8], v8, aw), r=["aff", "v8" + kk], w=["iu" + kk])
                            P.op("dve", lambda e: e.match_replace(aw, v8, aw, -1.0), r=["v8" + kk, "aff"], w=["aff"])
                        P.op("dve", lambda e: e.tensor_copy(ifl, iu), r=["iu" + kk], w=["idxf" + kk])
                    for t2 in range(2):
                        P.op("pe", lambda e: e.transpose(bank(0)[:, t2 * 16:(t2 + 1) * 16], idxf[0:16, t2 * 128:(t2 + 1) * 128], identF[0:16, 0:16]),
                             r=["idxfl", "identF"], w=[bkey(0)])
                        P.op("pe", lambda e: e.transpose(bank(1)[:, t2 * 16:(t2 + 1) * 16], vals[0:16, t2 * 128:(t2 + 1) * 128], identF[0:16, 0:16]),
                             r=["v8l", "identF"], w=[bkey(1)])
                    P.op("dve", lambda e: e.tensor_copy(idxT.rearrange("p e t -> p t e"), bank(0)[:, 0:32].rearrange("p (t e) -> p t e", t=2)), r=[bkey(0)], w=["idxT"])
                    P.op("dve", lambda e: e.tensor_copy(gateT.rearrange("p e t -> p t e"), bank(1)[:, 0:32].rearrange("p (t e) -> p t e", t=2)), r=[bkey(1)], w=["gateT"])
                    P.op("dve", lambda e: e.tensor_copy(idxTu, idxT), r=["idxT"], w=["idxTu"])
                    if need_ctx:
                        P.op("pe", lambda e: e.transpose(bank(2)[0:32, 0:16], idxfc[0:16, 0:32], identF[0:16, 0:16]), r=["idxfc", "identF"], w=[bkey(2)])
                        P.op("pe", lambda e: e.transpose(bank(3)[0:32, 0:16], valsc[0:16, 0:32], identF[0:16, 0:16]), r=["v8c", "identF"], w=[bkey(3)])
                        P.op("dve", lambda e: e.tensor_copy(idxTcu, bank(2)[0:32, 0:16]), r=[bkey(2)], w=["idxTcu"])
                        P.op("dve", lambda e: e.tensor_copy(gateTc, bank(3)[0:32, 0:16]), r=[bkey(3)], w=["gateTc"])
                        P.op("dve", lambda e: e.tensor_copy(idxTcU, idxTcu), r=["idxTcu"], w=["idxTcU"])
                        P.dma("sp", IDXC, idxTcu, r=["idxTcu"], w=["IDXC"])
                        for j4 in range(4):
                            P.dma("sp", idxTcP[j4 * 32:(j4 + 1) * 32, :], IDXC.rearrange("s (t j) -> s j t", j=4)[:, j4, :], r=["IDXC"], w=["idxTcP"],
                                  allow_slow_non_contiguous=True)
                    if dbg:
                        P.dma("sp", DBG1[:, 0:CAP], idxf, r=["idxfl"], w=["DBG1"])
                        P.dma("sp", DBG1[:, CAP:2 * CAP], vals, r=["v8l"], w=["DBG1"])
                    P.barrier()
                with ExitStack() as es:
                    def sb(name, shape, dt=F32):
                        return es.enter_context(nc.sbuf_tensor(U(name), shape, dt)).ap()
                    ring = Ring(es)
                    xs = sb("xs", [128, 2, 3, 2048], BF16)
                    xsT = sb("xsT", [128, 16, NSL], BF16); actT = sb("actT", [128, 8, NSL], BF16); sA = sb("sA", [128, 2, NSL])
                    yest = sb("yest", [128, 6, 2048], BF16)
                    gi = 0
                    yi = 0
                    ne = cfg.get("n_experts", NE)

                    def issue_scatter(ex):
                        for (s0, ssz, st) in stiles_all:
                            ys = (ex % 2) * 3 + st
                            prev = [f"MOEx{ex - 1}_{k}" for k in range(3)]
                            if st < 2:
                                P.dma_scatter_add(MOE, idxTu[:, ex, st:st + 1], yest[:, ys, :], r=[f"yest{ys}", "idxTu"] + prev, w=[f"MOEx{ex}_{st}"])
                            else:
                                P.dma_scatter_add(MOE, idxTcU[0:32, ex:ex + 1], yest[0:32, ys, :], element_offset=L * D,
                                                  r=[f"yest{ys}", "idxTcU"] + prev, w=[f"MOEx{ex}_{st}"])
                    stiles_all = [(0, 128, 0), (128, 128, 1)] + ([(256, 32, 2)] if need_ctx else [])
                    gtiles = [(0, 128, 0), (128, 128, 1)] + ([(256, 32, 2)] if need_ctx else [])

                    def issue_gather(ex):
                        xb = ex % 2
                        for (s0, ssz, st) in gtiles:
                            if st < 2:
                                P.dma_gather(xs[:, xb, st, :], H2D, idxTu[:, ex, st:st + 1], r=["H2D", "idxTu"], w=[f"xs{xb}_{st}"])
                            else:
                                P.dma_gather(xs[0:32, xb, st, :], H2D, idxTcU[0:32, ex:ex + 1], element_offset=L * D,
                                             r=["H2D", "idxTcU"], w=[f"xs{xb}_{st}"])
                    if ne > 0:
                        issue_gather(0)
                    for ex in range(ne):
                        xb = ex % 2
                        if ex + 1 < ne:
                            issue_gather(ex + 1)
                        for (s0, ssz, st) in gtiles:
                            for cb in range(4):
                                b = 1 + gi % 4
                                gi += 1
                                for cc in range(4):
                                    c = cb * 4 + cc
                                    P.op("pe", lambda e: e.matmul(bank(b)[:, cc * 128:cc * 128 + ssz], xs[0:ssz, xb, st, c * 128:(c + 1) * 128],
                                                                  identB[0:ssz, 0:ssz], start=True, stop=True),
                                         r=[f"xs{xb}_{st}", "identB"], w=[bkey(b)])
                                copy_op(evac_eng(), xsT[:, cb * 4:(cb + 1) * 4, s0:s0 + ssz],
                                        bank(b).rearrange("p (c n) -> p c n", c=4)[:, :, 0:ssz], r=[bkey(b)], w=["xsT"])
                        for hf in range(2):
                            G, gk = ring.load(w_gate[l, ex, :, hf * 512:(hf + 1) * 512].rearrange("(c p) n -> p c n", p=128),
                                              lambda t: t.rearrange("p (c n) -> p c n", c=16))
                            Uw, uk = ring.load(w_up[l, ex, :, hf * 512:(hf + 1) * 512].rearrange("(c p) n -> p c n", p=128),
                                               lambda t: t.rearrange("p (c n) -> p c n", c=16))
                            for fcl in range(4):
                                fc = hf * 4 + fcl
                                bA, bU = (1, 2) if fcl % 2 == 0 else (3, 4)
                                for c in range(16):
                                    P.op("pe", lambda e: e.matmul(bank(bA)[:, 0:nsl], G[:, c, fcl * 128:(fcl + 1) * 128], xsT[:, c, 0:nsl], start=(c == 0), stop=(c == 15)),
                                         r=[gk, "xsT"], w=[bkey(bA)])
                                for c in range(16):
                                    P.op("pe", lambda e: e.matmul(bank(bU)[:, 0:nsl], Uw[:, c, fcl * 128:(fcl + 1) * 128], xsT[:, c, 0:nsl], start=(c == 0), stop=(c == 15)),
                                         r=[uk, "xsT"], w=[bkey(bU)])
                                ss = fcl % 2
                                P.op("act", lambda e: e.activation(sA[:, ss, 0:nsl], bank(bA)[:, 0:nsl], AF.Silu), r=[bkey(bA)], w=[f"sA{ss}"])
                                P.op("dve", lambda e: e.tensor_tensor(actT[:, fc, 0:nsl], sA[:, ss, 0:nsl], bank(bU)[:, 0:nsl], ALU.mult),
                                     r=[f"sA{ss}", bkey(bU)], w=["actT"])
                        if ex > 0:
                            issue_scatter(ex - 1)
                        Dt = []
                        for hf in range(2):
                            Dw, dk = ring.load(w_down[l, ex, hf * 512:(hf + 1) * 512, :].rearrange("(c p) n -> p c n", p=128),
                                               lambda t: t.rearrange("p (c n) -> p c n", c=4))
                            Dt.append((Dw, dk))
                        stiles = [(0, 128, 0), (128, 128, 1)] + ([(256, 32, 2)] if need_ctx else [])
                        for (s0, ssz, st) in stiles:
                            ys = (ex % 2) * 3 + st
                            for dg in range(4):
                                b = 5 + gi % 3
                                gi += 1
                                for fc in range(8):
                                    Dw, dk = Dt[fc // 4]
                                    P.op("pe", lambda e: e.matmul(bank(b)[0:ssz, :], actT[:, fc, s0:s0 + ssz], Dw[:, fc % 4, dg * 512:(dg + 1) * 512],
                                                                  start=(fc == 0), stop=(fc == 7)), r=["actT", dk], w=[bkey(b)])
                                gsc = gateT[:, ex, st:st + 1] if st < 2 else gateTc[:, ex:ex + 1]
                                eng = evac_eng()
                                if eng == "act":
                                    P.op("act", lambda e: e.activation(yest[0:ssz, ys, dg * 512:(dg + 1) * 512], bank(b)[0:ssz, :], AF.Copy, scale=gsc[0:ssz]),
                                         r=[bkey(b), "gateT", "gateTc"], w=[f"yest{ys}"])
                                else:
                                    P.op("dve", lambda e: e.tensor_scalar(yest[0:ssz, ys, dg * 512:(dg + 1) * 512], bank(b)[0:ssz, :], gsc[0:ssz], None, ALU.mult),
                                         r=[bkey(b), "gateT", "gateTc"], w=[f"yest{ys}"])
                    if ne > 0:
                        issue_scatter(ne - 1)
                    P.barrier()
            with ExitStack() as es:
                def sb(name, shape, dt=F32):
                    return es.enter_context(nc.sbuf_tensor(U(name), shape, dt)).ap()
                mt = sb("mt", [128, 4, 2048]); xg = sb("xg3", [128, 16, 512])
                bi = 0
                for (g0, gsz) in grps:
                    r = 1 if g0 >= L else 0
                    ntl = gsz // 128
                    P.dma("sp", xg[:, :, 0:gsz], XTv[:, :, g0:g0 + gsz], r=["XT"], w=["xg"])
                    for tt in range(ntl):
                        P.dma("sp", mt[:, tt, :], MOE[g0 + tt * 128:g0 + (tt + 1) * 128, :], r=["MOE"], w=[f"mt{tt}"])
                    for c in range(16):
                        b = bi % 6
                        bi += 1
                        for tt in range(ntl):
                            P.op("pe", lambda e: e.transpose(bank(b)[:, tt * 128:(tt + 1) * 128], mt[:, tt, c * 128:(c + 1) * 128], identF),
                                 r=[f"mt{tt}", "identF"], w=[bkey(b)])
                        P.op("dve", lambda e: e.scalar_tensor_tensor(xg[:, c, 0:gsz], bank(b)[:, 0:gsz], ada_vec(l, 5, r)[:, c:c + 1],
                                                                     xg[:, c, 0:gsz], ALU.mult, ALU.add),
                             r=[bkey(b), "xg", "ada_sb"], w=["xg"])
                    P.dma("sp", XTv[:, :, g0:g0 + gsz], xg[:, :, 0:gsz], r=["xg"], w=["XT"])
                P.barrier()

    def phase_final():
        with ExitStack() as es:
            def sb(name, shape, dt=F32):
                return es.enter_context(nc.sbuf_tensor(U(name), shape, dt)).ap()
            xg = sb("xg", [128, 16, 512]); xn = sb("xn", [128, 16, 512]); nb = alloc_norm_bufs(es)
            ost = sb("ost", [128, 2, 2048])
            oi = 0
            bi = 0
            for (g0, gsz) in GROUPS[:4]:
                P.dma("sp", xg, XTv[:, :, g0:g0 + gsz], r=["XT"], w=["xg"])

                def out_fn(c, tc, key):
                    copy_op("act" if c % 2 else "pool", xn[:, c, :], tc, r=[key], w=["xn"])
                norm_group(nb, xg, gsz, nfin_sb, None, out_fn)
                for tt in range(4):
                    os_ = oi % 2
                    oi += 1
                    for cb in range(4):
                        b = bi % 6
                        bi += 1
                        for cc in range(4):
                            c = cb * 4 + cc
                            P.op("pe", lambda e: e.transpose(bank(b)[:, cc * 128:(cc + 1) * 128], xn[:, c, tt * 128:(tt + 1) * 128], identF),
                                 r=["xn", "identF"], w=[bkey(b)])
                        copy_op(evac_eng(), ost[:, os_, cb * 512:(cb + 1) * 512], bank(b), r=[bkey(b)], w=[f"ost{os_}"])
                    P.dma("sp", y_out[g0 + tt * 128:g0 + (tt + 1) * 128, :], ost[:, os_, :], r=[f"ost{os_}"], w=["y"])
            P.barrier()

    if not cfg.get("skip_ada"):
        phase_ada()
    only_mix = cfg.get("only_mix")
    for l in range(nlayers):
        if not cfg.get("skip_inproj"):
            with ExitStack() as esl:
                hT = esl.enter_context(nc.sbuf_tensor(U("hT"), [128, 16, NT], BF16)).ap()
                phase_norm1(l, hT, esl)
                if stop_after == "norm1":
                    break
                phase_inproj(l, hT)
        if stop_after == "inproj":
            break
        if only_mix is None or "ret" in only_mix:
            phase_ret(l)
        if only_mix is None or "conv" in only_mix:
            phase_conv(l)
        if only_mix is None or "swa" in only_mix:
            phase_swa(l)
        if only_mix is None or "na" in only_mix:
            phase_na(l)
        if stop_after == "mix":
            break
        if not cfg.get("skip_outproj"):
            phase_outproj(l)
        if stop_after == "outproj":
            break
        phase_ffn(l)
        if stop_after == "ffn":
            break
    else:
        phase_final()

    P.barrier()
    print(f"[build] ops={P.nops} waits={P.nwaits}")
    return nc


def make_in_maps(inputs):
    hc = _host_consts()
    f = lambda a: np.ascontiguousarray(np.asarray(a, dtype=np.float32))
    x = f(inputs["x"]); c = f(inputs["c"]); ctx = f(inputs["ctx"]); c_ctx = f(inputs["c_ctx"])
    shared = {
        "w_ada": f(inputs["w_ada"]),
        "bada_t": f(np.asarray(inputs["b_ada"]).reshape(DEPTH, 96, 128).transpose(0, 2, 1)),
        "nmix_t": f(np.asarray(inputs["norm_mix"]).reshape(DEPTH, 16, 128).transpose(0, 2, 1)),
        "nffn_t": f(np.asarray(inputs["norm_ffn"]).reshape(DEPTH, 16, 128).transpose(0, 2, 1)),
        "nfin_t": f(np.asarray(inputs["norm_final"]).reshape(16, 128).T),
        "w_in": f(inputs["w_in"]), "w_out": f(inputs["w_out"]),
        "decA": f(np.concatenate([inputs["ret_decay_fwd"], inputs["ret_decay_bwd"]], axis=1)),
        "decB": f(np.stack([np.concatenate([np.asarray(inputs["ret_decay_fwd"])[:, hf::2],
                                            np.asarray(inputs["ret_decay_bwd"])[:, hf::2]], axis=1) for hf in range(2)], axis=1)),
        "convw_t": f(np.asarray(inputs["conv_w"]).reshape(DEPTH, 3, 4, 128).transpose(0, 3, 2, 1)),
        "sink": f(inputs["swa_sink"]),
        "na_bias": _na_bias_layout(np.asarray(inputs["na_rpb"], dtype=np.float32)),
        "w_router": f(inputs["w_router"]),
        "w_gate": f(inputs["w_gate"]), "w_up": f(inputs["w_up"]), "w_down": f(inputs["w_down"]),
    }
    shared.update(hc)
    maps = []
    for b in range(8):
        m = dict(shared)
        m["x"] = x[b]
        m["ctx"] = ctx[b]
        m["c_t"] = f(np.stack([c[b].reshape(16, 128).T, c_ctx.reshape(16, 128).T], axis=-1))
        maps.append(m)
    return maps


def kernel(**inputs):
    nc = build_program()
    maps = make_in_maps(inputs)
    res = run_bass_kernel_spmd(nc, maps, core_ids=list(range(8)))
    return np.stack([np.asarray(r["y"], dtype=np.float32) for r in res.results], axis=0)
```

```python
from contextlib import ExitStack
import numpy as np
import concourse.bass as bass
import concourse.mybir as mybir
from concourse.bass_utils import run_bass_kernel_spmd

F32 = mybir.dt.float32
BF16 = mybir.dt.bfloat16
U32 = mybir.dt.uint32
ALU = mybir.AluOpType
AF = mybir.ActivationFunctionType
AX = mybir.AxisListType

D = 2048
L = 2048
T = 256
NT = L + T
DEPTH = 2
NE = 16
FF = 1024
CAP = 256
CAPC = 32
NSL = CAP + CAPC
IN_COLS = 5888
EPS = 1e-6
GROUPS = [(0, 512), (512, 512), (1024, 512), (1536, 512), (2048, 256)]
NEG = -30000.0

ZT_RQ, ZT_RK, ZT_RV, ZT_RG, ZT_SQ, ZT_SK, ZT_SV, ZT_NV = 0, 512, 1024, 1536, 2048, 2560, 2688, 2816
ZT_COLS = 3328
ZF_CB, ZF_CC, ZF_CH, ZF_NQ, ZF_NK = 0, 512, 1024, 1536, 2048
ZF_ROWS = 2560

N_DMA_SEMS = 24
N_SW_SEMS = 8


class Prog:
    def __init__(self, nc):
        self.nc = nc
        self.eng = {"pe": nc.tensor, "act": nc.scalar, "dve": nc.vector,
                    "pool": nc.gpsimd, "sp": nc.sync}
        self.sem = {k: nc.alloc_semaphore(name=f"s_{k}") for k in self.eng}
        self.cnt = {k: 0 for k in self.eng}
        self.dma_sems = [nc.alloc_semaphore(name=f"s_dma{i}") for i in range(N_DMA_SEMS)]
        self.dma_cnt = [0] * N_DMA_SEMS
        self.dma_rr = 0
        self.known = {k: {} for k in self.eng}
        self.res = {}
        self.nwaits = 0
        self.nops = 0
        self.sw_sems = [nc.alloc_semaphore(name=f"s_sw{i}") for i in range(N_SW_SEMS)]
        self.sw_cnt = [0] * N_SW_SEMS
        self.sw_rr = 0

    def _semh(self, key):
        if key[0] == "e":
            return self.sem[key[1]]
        if key[0] == "s":
            return self.sw_sems[key[1]]
        return self.dma_sems[key[1]]

    def dma_gather(self, out, in_, idx_ap, element_offset=0, r=(), w=()):
        q = "pool"
        for k, v in self._deps(q, r, w).items():
            self._wait(q, (k, v))
        i = self.sw_rr
        self.sw_rr = (self.sw_rr + 1) % N_SW_SEMS
        if self.sw_cnt[i] > 0:
            self._wait(q, (("s", i), 16 * self.sw_cnt[i]))
        ins = self.eng[q].indirect_dma_start(out, None, in_, bass.IndirectOffsetOnAxis(idx_ap, 0),
                                             element_offset=element_offset)
        self.sw_cnt[i] += 1
        ins.then_inc(self.sw_sems[i], 16)
        tok = (("s", i), 16 * self.sw_cnt[i])
        self._record(tok, r, w)
        self.nops += 1
        return tok

    def dma_scatter_add(self, out, idx_ap, in_, element_offset=0, r=(), w=()):
        q = "pool"
        for k, v in self._deps(q, r, w).items():
            self._wait(q, (k, v))
        i = self.sw_rr
        self.sw_rr = (self.sw_rr + 1) % N_SW_SEMS
        if self.sw_cnt[i] > 0:
            self._wait(q, (("s", i), 16 * self.sw_cnt[i]))
        ins = self.eng[q].indirect_dma_start(out, bass.IndirectOffsetOnAxis(idx_ap, 0), in_, None,
                                             element_offset=element_offset, compute_op=ALU.add)
        self.sw_cnt[i] += 1
        ins.then_inc(self.sw_sems[i], 16)
        tok = (("s", i), 16 * self.sw_cnt[i])
        self._record(tok, r, w)
        self.nops += 1
        return tok

    def dma_sw(self, slot, out, in_, r=(), w=(), **kw):
        q = "pool"
        for k, v in self._deps(q, r, w).items():
            self._wait(q, (k, v))
        i = self.sw_rr
        self.sw_rr = (self.sw_rr + 1) % N_SW_SEMS
        if self.sw_cnt[i] > 0:
            self._wait(q, (("s", i), 16 * self.sw_cnt[i]))
        ins = self.eng[q].dma_start(out=out, in_=in_, **kw)
        self.sw_cnt[i] += 1
        ins.then_inc(self.sw_sems[i], 16)
        tok = (("s", i), 16 * self.sw_cnt[i])
        self._record(tok, r, w)
        self.nops += 1
        return tok

    def _wait(self, e, tok):
        key, val = tok
        if self.known[e].get(key, 0) >= val:
            return
        self.eng[e].wait_ge(self._semh(key), val)
        self.known[e][key] = val
        self.nwaits += 1

    def _deps(self, e, r, w):
        deps = {}

        def add(tok):
            if tok is None:
                return
            k, v = tok
            if k == ("e", "pe") and e == "pe":
                return
            if deps.get(k, 0) < v:
                deps[k] = v
        for k in r:
            st = self.res.get(k)
            if st:
                add(st[0])
        for k in w:
            st = self.res.get(k)
            if st:
                add(st[0])
                for t in st[1]:
                    add(t)
        return deps

    def _record(self, tok, r, w):
        for k in w:
            self.res[k] = [tok, []]
        for k in r:
            st = self.res.setdefault(k, [None, []])
            lst = st[1]
            for i, (kk, vv) in enumerate(lst):
                if kk == tok[0]:
                    if vv < tok[1]:
                        lst[i] = tok
                    break
            else:
                lst.append(tok)

    def op(self, e, fn, r=(), w=()):
        psr = [k for k in r if k.startswith("ps")]
        if psr:
            r = [k for k in r if not k.startswith("ps")]
            w = list(w) + psr
        for k, v in self._deps(e, r, w).items():
            self._wait(e, (k, v))
        ins = fn(self.eng[e])
        self.cnt[e] += 1
        ins.then_inc(self.sem[e], 1)
        tok = (("e", e), self.cnt[e])
        self._record(tok, r, w)
        self.nops += 1
        return tok

    def dma(self, q, out, in_, r=(), w=(), **kw):
        for k, v in self._deps(q, r, w).items():
            self._wait(q, (k, v))
        i = self.dma_rr
        self.dma_rr = (self.dma_rr + 1) % N_DMA_SEMS
        if self.dma_cnt[i] > 0:
            self._wait(q, (("d", i), 16 * self.dma_cnt[i]))
        ins = self.eng[q].dma_start(out=out, in_=in_, **kw)
        self.dma_cnt[i] += 1
        ins.then_inc(self.dma_sems[i], 16)
        tok = (("d", i), 16 * self.dma_cnt[i])
        self._record(tok, r, w)
        self.nops += 1
        return tok

    def _bump(self, q):
        if self.cnt[q] > 0:
            self._wait(q, (("e", q), self.cnt[q]))
        ins = self.eng[q].nop()
        self.cnt[q] += 1
        ins.then_inc(self.sem[q], 1)

    def _all_wait_all(self):
        for e in self.eng:
            for e2 in self.eng:
                if self.cnt[e2] > 0:
                    self._wait(e, (("e", e2), self.cnt[e2]))

    def barrier(self):
        for i in range(N_DMA_SEMS):
            if self.dma_cnt[i] > 0:
                self._wait("sp", (("d", i), 16 * self.dma_cnt[i]))
        for i in range(N_SW_SEMS):
            if self.sw_cnt[i] > 0:
                self._wait("pool", (("s", i), 16 * self.sw_cnt[i]))
        self._bump("sp")
        self._bump("pool")
        self._all_wait_all()
        self.res = {}


def _host_consts():
    c = {}
    t = np.arange(L)
    row = (t // 64).astype(np.float32)
    col = (t % 64).astype(np.float32)
    inv = (10000.0 ** (-np.arange(16, dtype=np.float32) / 16)).astype(np.float32)
    ang = np.concatenate([row[:, None] * inv, col[:, None] * inv], axis=-1).astype(np.float32)
    cos, sin = np.cos(ang).astype(np.float32), np.sin(ang).astype(np.float32)
    CC = np.concatenate([cos, cos], -1)
    SS = np.concatenate([-sin, sin], -1)
    tab = np.stack([CC, SS, 0.125 * CC, 0.125 * SS], 1)
    c["cs_tab"] = np.ascontiguousarray(tab.reshape(16, 128, 4, 64).transpose(1, 0, 2, 3)).astype(np.float32)
    k = np.arange(128)[:, None].astype(np.float32)
    q = np.arange(128)[None, :].astype(np.float32)
    retc = np.stack([np.maximum(q - k, 0), (q >= k).astype(np.float32),
                     np.maximum(k - q, 0), (k > q).astype(np.float32)], 1)
    c["retc"] = np.ascontiguousarray(retc).astype(np.float32)
    p = np.arange(128, dtype=np.float32)
    c["posv"] = np.stack([p + 1, 127 - p, 128 - p, p], 1).astype(np.float32)
    qr = np.stack([np.arange(128) + 1.0, 128.0 - np.arange(128)], 0)
    c["qrow"] = np.ascontiguousarray(np.broadcast_to(qr[None], (128, 2, 128))).astype(np.float32)
    c["swamask"] = np.ascontiguousarray(np.stack([(k >= q), (k <= q)], 1)).astype(np.float32)
    return c


NA_CLS_TILES = {0: [0, 1, 2, 3], 1: [0, 1, 2, 3], 2: None, 3: [12, 13, 14, 15], 4: [12, 13, 14, 15]}
NA_CLS_REP = {0: 0, 1: 1, 2: 2, 3: 14, 4: 15}


def na_cls(m):
    if m <= 1:
        return m
    if m >= 14:
        return m - 11
    return 2


def na_tiles(m):
    cl = na_cls(m)
    if cl == 2:
        return [m - 2, m - 1, m, m + 1, m + 2]
    return NA_CLS_TILES[cl]


def _na_bias_layout(rpb):
    out = np.full((DEPTH, 8, 128, 5, 5, 128), NEG, np.float32)
    a = np.arange(128) // 64
    cc = np.arange(128) % 64
    for cl in range(5):
        m = NA_CLS_REP[cl]
        tiles = na_tiles(m)
        qr = 2 * m + a
        qc = cc
        bs = np.clip(qr - 4, 0, 24)
        cs = np.clip(qc - 8, 0, 48)
        for j, kt in enumerate(tiles):
            kr = 2 * kt + a
            kc = cc
            inband = (kr[:, None] >= bs[None, :]) & (kr[:, None] < bs[None, :] + 8)
            colok = (kc[:, None] >= cs[None, :]) & (kc[:, None] < cs[None, :] + 16)
            dr = np.clip(kr[:, None] - qr[None, :] + 7, 0, 14)
            dc = np.clip(kc[:, None] - qc[None, :], -15, 15) + 15
            g = rpb[:, :, dr, dc]
            out[:, :, :, cl, j, :] = np.where((inband & colok)[None, None], g, np.float32(NEG))
    return out.reshape(DEPTH, 8, 128, 25 * 128)


def build_program(cfg=None):
    cfg = cfg or {}
    _uid = [0]

    def U(n):
        _uid[0] += 1
        return f"{n}_{_uid[0]}"
    dbg = cfg.get("debug", False)
    stop_after = cfg.get("stop_after", None)
    nlayers = cfg.get("nlayers", DEPTH)
    nc = bass.Bass("TRN2", target_bir_lowering=False)
    P = Prog(nc)

    def din(name, shape, dt=F32):
        if name in cfg.get("shrink", ()):
            shape = [1] * len(shape)
        return nc.dram_tensor(name, list(shape), dt, kind="ExternalInput").ap()

    def dscr(name, shape, dt, out=False):
        return nc.dram_tensor(name, list(shape), dt,
                              kind="ExternalOutput" if (out and dbg) else "Internal").ap()

    x_in = din("x", [L, D]); ctx_in = din("ctx", [T, D]); c_t = din("c_t", [128, 16, 2])
    w_ada = din("w_ada", [DEPTH, D, 6 * D]); bada_t = din("bada_t", [DEPTH, 128, 96])
    nmix_t = din("nmix_t", [DEPTH, 128, 16]); nffn_t = din("nffn_t", [DEPTH, 128, 16]); nfin_t = din("nfin_t", [128, 16])
    w_in = din("w_in", [DEPTH, D, IN_COLS]); w_out = din("w_out", [DEPTH, D, D])
    decA = din("decA", [DEPTH, 16]); decB = din("decB", [DEPTH, 2, 8])
    convw_t = din("convw_t", [DEPTH, 128, 4, 3]); sink_in = din("sink", [DEPTH, 8])
    na_bias = din("na_bias", [DEPTH, 8, 128, 3200])
    w_router = din("w_router", [DEPTH, D, NE])
    w_gate = din("w_gate", [DEPTH, NE, D, FF]); w_up = din("w_up", [DEPTH, NE, D, FF]); w_down = din("w_down", [DEPTH, NE, FF, D])
    cs_tab = din("cs_tab", [128, 16, 4, 64]); retc_in = din("retc", [128, 4, 128]); posv_in = din("posv", [128, 4])
    qrow_in = din("qrow", [128, 2, 128]); swamask_in = din("swamask", [128, 2, 128])
    y_out = nc.dram_tensor("y", [L, D], F32, kind="ExternalOutput").ap()

    XT = dscr("XT", [D, NT], F32, out=True)
    ZT = dscr("ZT", [NT, ZT_COLS], BF16, out=True)
    ZF = dscr("ZF", [ZF_ROWS, NT], BF16, out=True)
    YT = dscr("YT", [D, NT], BF16, out=True)
    YE = dscr("YE", [NE, NSL, D], BF16, out=True)
    NPC = cfg.get("npc", 5)
    WBG = dscr("WBG", [max(NPC, 1), D, FF], BF16)
    WBU = dscr("WBU", [max(NPC, 1), D, FF], BF16)
    WBD = dscr("WBD", [max(NPC, 1), FF, D], BF16)
    pc_jobs = []
    pc_n = [0]

    def precast_prepare(l):
        del pc_jobs[:]
        for ex in range(NPC):
            for q in range(4):
                pc_jobs.append((WBG[ex, q * 512:(q + 1) * 512, :], w_gate[l, ex, q * 512:(q + 1) * 512, :]))
                pc_jobs.append((WBU[ex, q * 512:(q + 1) * 512, :], w_up[l, ex, q * 512:(q + 1) * 512, :]))
                pc_jobs.append((WBD[ex, q * 256:(q + 1) * 256, :], w_down[l, ex, q * 256:(q + 1) * 256, :]))

    def precast_step():
        if pc_jobs:
            dst, src = pc_jobs.pop(0)
            pc_n[0] += 1
            P.dma_sw(0, dst, src, w=[f"WB{pc_n[0]}"])

    H2D = dscr("H2D", [NT, D], BF16)
    MOE = dscr("MOE", [NT, D], F32)
    ADAD = dscr("ADAD", [DEPTH, 128, 96, 2], F32, out=True)
    IDXC = dscr("IDXC", [CAPC, NE], F32)
    DBG1 = dscr("DBG1", [NE, 2 * CAP], F32, out=True)
    XTv = XT.rearrange("(c p) n -> p c n", p=128)
    YTv = YT.rearrange("(c p) n -> p c n", p=128)

    PS = nc.alloc_psum_tensor("ps", [128, 8, 512], F32).ap()

    def bank(i):
        return PS[:, i, :]

    def bkey(i):
        return f"ps{i}"

    identF = nc.alloc_sbuf_tensor("identF", [128, 128], F32).ap()
    identB = nc.alloc_sbuf_tensor("identB", [128, 128], BF16).ap()
    onesF = nc.alloc_sbuf_tensor("onesF", [128, 128], F32).ap()
    iotaF = nc.alloc_sbuf_tensor("iotaF", [128, 2048], F32).ap()
    piota = nc.alloc_sbuf_tensor("piota", [128, 16], F32).ap()
    ada_sb = nc.alloc_sbuf_tensor("ada_sb", [128, DEPTH, 96, 2], F32).ap()
    scl = nc.alloc_sbuf_tensor("scl", [128, DEPTH, 2, 16, 2], F32).ap()
    nrm_sb = nc.alloc_sbuf_tensor("nrm_sb", [128, DEPTH, 2, 16], F32).ap()
    nfin_sb = nc.alloc_sbuf_tensor("nfin_sb", [128, 16], F32).ap()
    tmpi = nc.alloc_sbuf_tensor("tmpi", [128, 128], F32).ap()

    P.op("pool", lambda e: e.iota(tmpi, pattern=[[1, 128]], base=0, channel_multiplier=-1,
                                  allow_small_or_imprecise_dtypes=True), w=["tmpi"])
    P.op("dve", lambda e: e.tensor_scalar(identF, tmpi, 0.0, None, ALU.is_equal), r=["tmpi"], w=["identF"])
    P.op("dve", lambda e: e.tensor_copy(identB, identF), r=["identF"], w=["identB"])
    P.op("dve", lambda e: e.memset(onesF, 1.0), w=["onesF"])
    P.op("pool", lambda e: e.iota(iotaF, pattern=[[1, 2048]], base=0, channel_multiplier=0,
                                  allow_small_or_imprecise_dtypes=True), w=["iotaF"])
    P.op("pool", lambda e: e.iota(piota, pattern=[[128, 16]], base=0, channel_multiplier=1,
                                  allow_small_or_imprecise_dtypes=True), w=["piota"])
    P.dma("sp", nrm_sb[:, :, 0, :], nmix_t.rearrange("l p c -> p l c"), w=["nrm_sb"])
    P.dma("sp", nrm_sb[:, :, 1, :], nffn_t.rearrange("l p c -> p l c"), w=["nrm_sb"])
    P.dma("sp", nfin_sb, nfin_t, w=["nfin_sb"])

    NW = 4
    evac_rr = [0]

    def evac_eng():
        evac_rr[0] ^= 1
        return "act" if evac_rr[0] else "dve"

    def copy_op(eng, out, in_, r, w, scale=None):
        if eng == "act":
            if scale is None:
                P.op("act", lambda e: e.activation(out, in_, AF.Copy), r=r, w=w)
            else:
                P.op("act", lambda e: e.activation(out, in_, AF.Copy, scale=scale), r=r, w=w)
        else:
            if scale is None:
                P.op(eng, lambda e: e.tensor_copy(out, in_), r=r, w=w)
            else:
                P.op(eng, lambda e: e.tensor_scalar(out, in_, scale, None, ALU.mult), r=r, w=w)

    class Ring:
        def __init__(self, es, nslots=NW):
            self.n = nslots
            self.t = es.enter_context(nc.sbuf_tensor(U("wring"), [128, nslots, 8192], BF16)).ap()
            self.i = 0
            self.tag = U("w")

        def load(self, src_ap, view):
            s = self.i
            self.i = (self.i + 1) % self.n
            dst = view(self.t[:, s, :])
            key = f"{self.tag}_{s}"
            P.dma_sw(s, dst, src_ap, w=[key])
            return dst, key

    sc_t = nc.alloc_sbuf_tensor("sc_t", [128, 16, 2], BF16).ap()
    cin_t = nc.alloc_sbuf_tensor("cin_t", [128, 16, 2], F32).ap()
    bada_sb = nc.alloc_sbuf_tensor("bada_sb", [128, DEPTH, 96], F32).ap()
    ada_bi = [0]

    def ada_setup():
        P.dma("sp", cin_t, c_t, w=["cin"])
        P.dma("sp", bada_sb, bada_t.rearrange("l p c -> p l c"), w=["bada"])
        P.op("act", lambda e: e.activation(sc_t, cin_t, AF.Silu), r=["cin"], w=["sc"])

    def ada_load(l, fg, ring):
        return ring.load(w_ada[l, :, fg * 512:(fg + 1) * 512].rearrange("(c p) n -> p c n", p=128),
                         lambda t: t.rearrange("p (c n) -> p c n", c=16))

    def ada_tile(l, fg, ring, banks, loaded=None):
        wt, wk = loaded if loaded is not None else ada_load(l, fg, ring)
        bi = banks[ada_bi[0] % len(banks)]
        ada_bi[0] += 1
        for m in range(4):
            for c in range(16):
                P.op("pe", lambda e: e.matmul(bank(bi)[:, m * 2:m * 2 + 2], wt[:, c, m * 128:(m + 1) * 128],
                                              sc_t[:, c, :], start=(c == 0), stop=(c == 15)),
                     r=[wk, "sc"], w=[bkey(bi)])
        P.op("dve", lambda e: e.tensor_tensor(
            ada_sb[:, l, fg * 4:(fg + 1) * 4, :],
            bank(bi)[:, 0:8].rearrange("p (m r) -> p m r", r=2),
            bada_sb[:, l, fg * 4:(fg + 1) * 4].unsqueeze(2).to_broadcast([128, 4, 2]), ALU.add),
            r=[bkey(bi), "bada"], w=["ada_sb"])

    def ada_scl(l, wi):
        c0 = 16 if wi == 0 else 64
        P.op("dve", lambda e: e.scalar_tensor_tensor(
            scl[:, l, wi, :, :], ada_sb[:, l, c0:c0 + 16, :], 1.0,
            nrm_sb[:, l, wi, :].unsqueeze(2).to_broadcast([128, 16, 2]), ALU.add, ALU.mult),
            r=["ada_sb", "nrm_sb"], w=["scl"])

    ada_pending = []

    def phase_ada():
        with ExitStack() as es:
            ring = Ring(es)
            ada_setup()
            bg = cfg.get("ada_bg", True)
            for l in range(nlayers):
                for fg in range(24):
                    if bg and not (l == 0 and fg < 8):
                        ada_pending.append((l, fg))
                    else:
                        ada_tile(l, fg, ring, (0, 1))
                if not bg:
                    ada_scl(l, 0); ada_scl(l, 1)
            if bg:
                ada_scl(0, 0)
            P.barrier()

    def ada_finish():
        for l in range(nlayers):
            if l > 0:
                ada_scl(l, 0)
            ada_scl(l, 1)
        if dbg:
            P.dma("sp", ADAD.rearrange("l p c r -> p l c r")[:, 0:nlayers], ada_sb[:, 0:nlayers], r=["ada_sb"], w=["ADAD"])

    def ada_vec(l, which, r):
        return ada_sb[:, l, which * 16:(which + 1) * 16, r]

    def norm_group(bufs, xg, gsz, scale_ap, shift_ap, out_fn, ssb=7):
        sq, rstd, tcs = bufs["sq"], bufs["rstd"], bufs["tc"]
        for c in range(16):
            s = c % 3
            P.op("act", lambda e: e.activation(sq[:, s, 0:gsz], xg[:, c, 0:gsz], AF.Square), r=["xg"], w=[f"sq{s}"])
            P.op("pe", lambda e: e.matmul(bank(ssb)[:, 0:gsz], onesF, sq[:, s, 0:gsz], start=(c == 0), stop=(c == 15)),
                 r=[f"sq{s}", "onesF"], w=[bkey(ssb)])
        P.op("act", lambda e: e.activation(rstd[:, 0, 0:gsz], bank(ssb)[:, 0:gsz], AF.Sqrt, bias=bufs["eps"][:, 0:1], scale=1.0 / D),
             r=[bkey(ssb), "eps"], w=["rstd0"])
        P.op("dve", lambda e: e.reciprocal(rstd[:, 1, 0:gsz], rstd[:, 0, 0:gsz]), r=["rstd0"], w=["rstd1"])
        for c in range(16):
            s = c % 3
            P.op("dve", lambda e: e.scalar_tensor_tensor(tcs[:, s, 0:gsz], xg[:, c, 0:gsz], scale_ap[:, c:c + 1],
                                                         rstd[:, 1, 0:gsz], ALU.mult, ALU.mult),
                 r=["xg", "rstd1", "scl", "ada_sb", "nfin_sb"], w=[f"tc{s}"])
            out_fn(c, tcs[:, s, 0:gsz], f"tc{s}")

    def alloc_norm_bufs(es):
        b = {}
        b["sq"] = es.enter_context(nc.sbuf_tensor(U("sq"), [128, 3, 512], F32)).ap()
        b["rstd"] = es.enter_context(nc.sbuf_tensor(U("rstd"), [128, 2, 512], F32)).ap()
        b["tc"] = es.enter_context(nc.sbuf_tensor(U("tcs"), [128, 3, 512], F32)).ap()
        b["eps"] = es.enter_context(nc.sbuf_tensor(U("epsb"), [128, 1], F32)).ap()
        P.op("dve", lambda e: e.memset(b["eps"], EPS), w=["eps"])
        return b

    def phase_norm1(l, hT, es_outer):
        with ExitStack() as es:
            xg = es.enter_context(nc.sbuf_tensor(U("xg"), [128, 16, 512], F32)).ap()
            nb = alloc_norm_bufs(es)
            xin = None
            if l == 0:
                xin = es.enter_context(nc.sbuf_tensor(U("xin"), [128, 2, 2048], F32)).ap()
            for gi, (g0, gsz) in enumerate(GROUPS):
                r = 1 if g0 >= L else 0
                if l == 0:
                    ntile = gsz // 128
                    for tt in range(ntile):
                        n0 = g0 + tt * 128
                        s = tt % 2
                        src = x_in[n0:n0 + 128, :] if n0 < L else ctx_in[n0 - L:n0 - L + 128, :]
                        P.dma("sp", xin[:, s, :], src, w=[f"xin{s}"])
                        for cb in range(4):
                            for cc in range(4):
                                c = cb * 4 + cc
                                P.op("pe", lambda e: e.transpose(bank(cb)[:, cc * 128:(cc + 1) * 128],
                                                                 xin[:, s, c * 128:(c + 1) * 128], identF),
                                     r=[f"xin{s}", "identF"], w=[bkey(cb)])
                            copy_op(evac_eng(), xg[:, cb * 4:(cb + 1) * 4, tt * 128:(tt + 1) * 128],
                                    bank(cb).rearrange("p (c n) -> p c n", c=4), r=[bkey(cb)], w=["xg"])
                    P.dma("sp", XTv[:, :, g0:g0 + gsz], xg[:, :, 0:gsz], r=["xg"], w=["XT"])
                else:
                    P.dma("sp", xg[:, :, 0:gsz], XTv[:, :, g0:g0 + gsz], r=["XT"], w=["xg"])

                def out_fn(c, tc, key, r=r, g0=g0, gsz=gsz):
                    P.op("act", lambda e: e.activation(hT[:, c, g0:g0 + gsz], tc, AF.Identity,
                                                       bias=ada_vec(l, 0, r)[:, c:c + 1], scale=1.0),
                         r=[key, "ada_sb"], w=["hT"])
                norm_group(nb, xg, gsz, scl[:, l, 0, :, r], None, out_fn)
            P.barrier()

    def phase_inproj(l, hT):
        with ExitStack() as es:
            ring = Ring(es)
            cs = es.enter_context(nc.sbuf_tensor(U("cs"), [128, 16, 4, 64], F32)).ap()
            zst = es.enter_context(nc.sbuf_tensor(U("zst"), [128, 3, 512], BF16)).ap()
            rt = es.enter_context(nc.sbuf_tensor(U("rt"), [128, 2, 2, 512], F32)).ap()
            P.dma("sp", cs, cs_tab, w=["cs"])
            zi = [0]
            bi = [0]
            tiles = [
                (0, 512, "T", ZT_RQ, "rope"), (512, 512, "T", ZT_RK, "ropek"),
                (1024, 512, "T", ZT_RV, "copy"), (1536, 512, "T", ZT_RG, "silu"),
                (2048, 512, "F", ZF_CB, "copy"), (2560, 512, "F", ZF_CC, "copy"), (3072, 512, "F", ZF_CH, "copy"),
                (3584, 512, "T", ZT_SQ, "ropeq_swa"), (4096, 256, "T", ZT_SK, "swakv"),
                (4352, 512, "F", ZF_NQ, "copy"), (4864, 512, "F", ZF_NK, "copy"),
                (5376, 512, "T", ZT_NV, "copy"),
            ]
            only = cfg.get("inproj_tiles")
            precast_prepare(l)
            pcc = [0]

            def pc_tick():
                pcc[0] += 1
                if pcc[0] % 3 == 0:
                    precast_step()
            for ti, (c0, ncol, orient, doff, kind) in enumerate(tiles):
                if only is not None and ti not in only:
                    continue
                wt, wk = ring.load(w_in[l, :, c0:c0 + ncol].rearrange("(c p) n -> p c n", p=128),
                                   lambda t: t[:, 0:16 * ncol].rearrange("p (c n) -> p c n", c=16))
                if orient == "T":
                    for tt in range(18):
                        b = bi[0] = (bi[0] + 1) % 6
                        for c in range(16):
                            P.op("pe", lambda e: e.matmul(bank(b)[:, 0:ncol], hT[:, c, tt * 128:(tt + 1) * 128], wt[:, c, :],
                                                          start=(c == 0), stop=(c == 15)),
                                 r=["hT", wk], w=[bkey(b)])
                        zs = zi[0] = (zi[0] + 1) % 3
                        zk = f"zst{zs}"
                        zo = zst[:, zs, 0:ncol]
                        ps = bank(b)[:, 0:ncol]
                        lat = tt < 16

                        def rope(ps_ap, out_ap, nh, tab, perm=False):
                            rs = tt % 2
                            x4 = ps_ap.rearrange("p (h t d) -> p h t d", t=2, d=32)
                            a = rt[:, rs, 0, 0:nh * 64]
                            bb = rt[:, rs, 1, 0:nh * 64]
                            a3 = a.rearrange("p (h d) -> p h d", d=64)
                            b4 = bb.rearrange("p (h t d) -> p h t d", t=2, d=32)
                            ccb = cs[:, tt, tab, :].unsqueeze(1).to_broadcast([128, nh, 64])
                            ssn = cs[:, tt, tab + 1, 0:32].unsqueeze(1).to_broadcast([128, nh, 32])
                            ssp = cs[:, tt, tab + 1, 32:64].unsqueeze(1).to_broadcast([128, nh, 32])
                            P.op("dve", lambda e: e.tensor_tensor(a3, ps_ap.rearrange("p (h d) -> p h d", d=64), ccb, ALU.mult),
                                 r=[bkey(b), "cs"], w=[f"rta{rs}"])
                            P.op("dve", lambda e: e.tensor_tensor(b4[:, :, 0, :], x4[:, :, 1, :], ssn, ALU.mult),
                                 r=[bkey(b), "cs"], w=[f"rtb{rs}"])
                            P.op("dve", lambda e: e.tensor_tensor(b4[:, :, 1, :], x4[:, :, 0, :], ssp, ALU.mult),
                                 r=[bkey(b), "cs"], w=[f"rtb{rs}"])
                            if perm:
                                o = out_ap.rearrange("p (i g d) -> p g i d", g=2, d=64)
                                P.op("dve", lambda e: e.tensor_tensor(o, a.rearrange("p (g i d) -> p g i d", g=2, d=64),
                                                                       bb.rearrange("p (g i d) -> p g i d", g=2, d=64), ALU.add),
                                     r=[f"rta{rs}", f"rtb{rs}"], w=[zk])
                            else:
                                P.op("dve", lambda e: e.tensor_tensor(out_ap, a, bb, ALU.add),
                                     r=[f"rta{rs}", f"rtb{rs}"], w=[zk])

                        if kind == "copy":
                            copy_op(evac_eng(), zo, ps, r=[bkey(b)], w=[zk])
                        elif kind == "silu":
                            P.op("act", lambda e: e.activation(zo, ps, AF.Silu), r=[bkey(b)], w=[zk])
                        elif kind == "rope":
                            if lat:
                                rope(ps, zo, 8, 0)
                            else:
                                copy_op("act", zo, ps, r=[bkey(b)], w=[zk])
                        elif kind == "ropek":
                            if lat:
                                rope(ps, zo, 8, 2)
                            else:
                                copy_op("act", zo, ps, r=[bkey(b)], w=[zk], scale=0.125)
                        elif kind == "ropeq_swa":
                            if lat:
                                rope(ps, zo, 8, 0, perm=True)
                            else:
                                P.op("act", lambda e: e.activation(zo.rearrange("p (i g d) -> p g i d", g=2, d=64),
                                                                   ps.rearrange("p (g i d) -> p g i d", g=2, d=64), AF.Copy),
                                     r=[bkey(b)], w=[zk])
                        elif kind == "swakv":
                            if lat:
                                rope(ps[:, 0:128], zo[:, 0:128], 2, 0)
                            else:
                                copy_op("act", zo[:, 0:128], ps[:, 0:128], r=[bkey(b)], w=[zk])
                            copy_op("dve", zo[:, 128:256], ps[:, 128:256], r=[bkey(b)], w=[zk])
                        P.dma("sp", ZT[tt * 128:(tt + 1) * 128, doff:doff + ncol], zo, r=[zk], w=["ZT"])
                        pc_tick()
                else:
                    for m in range(ncol // 128):
                        for (g0, gsz) in GROUPS:
                            b = bi[0] = (bi[0] + 1) % 6
                            for c in range(16):
                                P.op("pe", lambda e: e.matmul(bank(b)[:, 0:gsz], wt[:, c, m * 128:(m + 1) * 128], hT[:, c, g0:g0 + gsz],
                                                              start=(c == 0), stop=(c == 15)),
                                     r=["hT", wk], w=[bkey(b)])
                            zs = zi[0] = (zi[0] + 1) % 3
                            zk = f"zst{zs}"
                            copy_op(evac_eng(), zst[:, zs, 0:gsz], bank(b)[:, 0:gsz], r=[bkey(b)], w=[zk])
                            P.dma("sp", ZF[doff + m * 128:doff + (m + 1) * 128, g0:g0 + gsz], zst[:, zs, 0:gsz], r=[zk], w=["ZF"])
                            pc_tick()
            while pc_jobs:
                precast_step()
            P.barrier()

    def phase_ret(l):
        need_ctx = l < DEPTH - 1
        with ExitStack() as es:
            def sb(name, shape, dt=F32):
                return es.enter_context(nc.sbuf_tensor(U(name), shape, dt)).ap()
            lgA = sb("lgA", [128, 16]); lgP = sb("lgP", [128, 8]); tA = sb("tA", [128, 16]); tP = sb("tP", [128, 8])
            retc = sb("retc", [128, 4, 128]); posv = sb("posv", [128, 4]); qrow = sb("qrow", [128, 2, 128])
            zeta = sb("zeta", [128, 2, 8]); cdP = sb("cdP", [128, 8]); xiT = sb("xiT", [128, 2, 4, 128])
            dm = sb("dm", [128, 8, 128]); dtmp = sb("dtmp", [128, 2, 128])
            epsb = sb("epsb", [128, 1])
            SfAll = sb("SfAll", [128, 18, 512], BF16)
            stf = sb("stf", [128, 512]); stb = sb("stb", [128, 512]); stb_bf = sb("stb_bf", [128, 512], BF16)
            kvin = sb("kvin", [128, 2, 1024], BF16); zin = sb("zin", [128, 2, 2048], BF16)
            kz = sb("kz", [128, 2, 512], BF16)
            qTe = sb("qTe", [128, 2, 512], BF16); qTo = sb("qTo", [128, 2, 512], BF16); kT = sb("kT", [128, 2, 512], BF16)
            bmask = sb("bmask", [128, 512]); tkv = sb("tkv", [128, 512])
            qxf = sb("qxf", [128, 2, 512], BF16); qxb = sb("qxb", [128, 2, 512], BF16)
            inn = sb("inn", [128, 2, 1024], BF16)
            sqv = sb("sqv", [128, 512]); t1 = sb("t1", [128, 512]); t2 = sb("t2", [128, 512])
            st8 = sb("st8", [128, 8, 8])
            yr = sb("yr", [128, 2, 512], BF16); yst = sb("yst", [128, 2, 4, 512], BF16)
            P.op("dve", lambda e: e.memset(epsb, EPS), w=["epsb"])
            P.dma("sp", retc, retc_in, w=["retc"]); P.dma("sp", posv, posv_in, w=["posv"]); P.dma("sp", qrow, qrow_in, w=["qrow"])
            P.dma("sp", tA, decA[l:l + 1, :].partition_broadcast(128), w=["tA"])
            P.dma("sp", tP[0:64, :], decB[l, 0:1, :].partition_broadcast(64), w=["tP"])
            P.dma("sp", tP[64:128, :], decB[l, 1:2, :].partition_broadcast(64), w=["tP"])
            for (src, dst, k1, k2) in ((tA, lgA, "tA", "lgA"), (tP, lgP, "tP", "lgP")):
                P.op("act", lambda e: e.activation(src, src, AF.Exp, scale=-1.0), r=[k1], w=[k1])
                P.op("dve", lambda e: e.tensor_scalar(src, src, 1.0, None, ALU.add), r=[k1], w=[k1])
                P.op("act", lambda e: e.activation(src, src, AF.Ln), r=[k1], w=[k1])
                P.op("dve", lambda e: e.tensor_scalar(dst, src, -1.0, None, ALU.mult), r=[k1], w=[k2])
            P.op("act", lambda e: e.activation(zeta[:, 0, :], lgA[:, 0:8], AF.Exp, scale=posv[:, 1:2]), r=["lgA", "posv"], w=["zeta"])
            P.op("act", lambda e: e.activation(zeta[:, 1, :], lgA[:, 8:16], AF.Exp, scale=posv[:, 3:4]), r=["lgA", "posv"], w=["zeta"])
            P.op("act", lambda e: e.activation(cdP, lgP, AF.Exp, scale=128.0), r=["lgP"], w=["cdP"])
            for dr in range(2):
                for j in range(4):
                    P.op("act", lambda e: e.activation(xiT[:, dr, j, :], qrow[:, dr, :], AF.Exp, scale=lgP[:, dr * 4 + j:dr * 4 + j + 1]),
                         r=["lgP", "qrow"], w=["xiT"])
            for h in range(8):
                P.op("act", lambda e: e.activation(dtmp[:, 0, :], retc[:, 0, :], AF.Exp, scale=lgA[:, h:h + 1]), r=["lgA", "retc"], w=["dtmp0"])
                P.op("act", lambda e: e.activation(dtmp[:, 1, :], retc[:, 2, :], AF.Exp, scale=lgA[:, 8 + h:9 + h]), r=["lgA", "retc"], w=["dtmp1"])
                P.op("dve", lambda e: e.tensor_tensor(dtmp[:, 0, :], dtmp[:, 0, :], retc[:, 1, :], ALU.mult), r=["dtmp0", "retc"], w=["dtmp0"])
                P.op("dve", lambda e: e.tensor_tensor(dtmp[:, 1, :], dtmp[:, 1, :], retc[:, 3, :], ALU.mult), r=["dtmp1", "retc"], w=["dtmp1"])
                P.op("dve", lambda e: e.tensor_tensor(dm[:, h, :], dtmp[:, 0, :], dtmp[:, 1, :], ALU.add), r=["dtmp0", "dtmp1"], w=["dm"])
            P.op("pool", lambda e: e.memset(qTe, 0.0), w=["qTe0", "qTe1"])
            P.op("pool", lambda e: e.memset(qTo, 0.0), w=["qTo0", "qTo1"])
            bm4 = bmask.rearrange("p (j t d) -> p j t d", t=2, d=64)
            P.op("pool", lambda e: e.memset(bmask, 0.0), w=["bmask"])
            P.op("pool", lambda e: e.memset(bm4[0:64, :, 0, :], 1.0), r=["bmask"], w=["bmask"])
            P.op("pool", lambda e: e.memset(bm4[64:128, :, 1, :], 1.0), r=["bmask"], w=["bmask"])
            P.op("dve", lambda e: e.memset(stf, 0.0), w=["stf"])
            P.op("dve", lambda e: e.memset(stb, 0.0), w=["stb"])
            P.op("dve", lambda e: e.memset(stb_bf, 0.0), w=["stb_bf"])

            def kv_update(i, dr, kin, kk, st, stk, s):
                P.op("dve", lambda e: e.tensor_tensor(kz[:, s, :].rearrange("p (h d) -> p h d", d=64),
                                                      kin[:, 0:512].rearrange("p (h d) -> p h d", d=64),
                                                      zeta[:, dr, :].unsqueeze(2).to_broadcast([128, 8, 64]), ALU.mult),
                     r=[kk, "zeta"], w=[f"kz{s}"])
                for j in range(4):
                    P.op("pe", lambda e: e.matmul(bank(4)[:, j * 128:(j + 1) * 128], kz[:, s, j * 128:(j + 1) * 128],
                                                  kin[:, 512 + j * 128:512 + (j + 1) * 128], start=True, stop=True),
                         r=[f"kz{s}", kk], w=[bkey(4)])
                P.op("dve", lambda e: e.tensor_tensor(st.rearrange("p (j v) -> p j v", v=128), st.rearrange("p (j v) -> p j v", v=128),
                                                      cdP[:, dr * 4:dr * 4 + 4].unsqueeze(2).to_broadcast([128, 4, 128]), ALU.mult),
                     r=[stk, "cdP"], w=[stk])
                P.op("dve", lambda e: e.tensor_tensor(tkv, bank(4), bmask, ALU.mult), r=[bkey(4), "bmask"], w=["tkv"])
                P.op("dve", lambda e: e.tensor_tensor(st, st, tkv, ALU.add), r=[stk, "tkv"], w=[stk])

            cut = cfg.get("ret_cut", 99)
            fwd_order = [16, 17] + list(range(16)) if cut >= 1 else []
            bwd_order = [17, 16] + list(range(15, -1, -1)) if cut >= 2 else []
            for n, i in enumerate(fwd_order):
                s = n % 2
                P.dma("sp", kvin[:, s, :], ZT[i * 128:(i + 1) * 128, ZT_RK:ZT_RK + 1024], r=["ZT"], w=[f"kvin{s}"])
                P.op("act", lambda e: e.activation(SfAll[:, i, :], stf, AF.Copy), r=["stf"], w=["SfAll"])
                if n < len(fwd_order) - 1:
                    kv_update(i, 0, kvin[:, s, :], f"kvin{s}", stf, "stf", s)
            for n, i in enumerate(bwd_order):
                s = n % 2
                zk = f"zin{s}"
                P.dma("sp", zin[:, s, :], ZT[i * 128:(i + 1) * 128, 0:2048], r=["ZT"], w=[zk])
                need_out = (i < 16) or need_ctx
                if cut < 2.05:
                    continue
                if need_out:
                    yb = 3 if s == 0 else 7
                    TPq, TPk = bank(0), bank(6)
                    for j in range(4):
                        P.op("pe", lambda e: e.matmul(TPq[:, j * 128:(j + 1) * 128], zin[:, s, j * 128:(j + 1) * 128], identB, start=True, stop=True),
                             r=[zk, "identB"], w=[bkey(0)])
                        P.op("pe", lambda e: e.matmul(TPk[:, j * 128:(j + 1) * 128], zin[:, s, 512 + j * 128:512 + (j + 1) * 128], identB, start=True, stop=True),
                             r=[zk, "identB"], w=[bkey(6)])
                    if cut < 2.15:
                        continue
                    P.op("act", lambda e: e.activation(qTe[0:64, s, :], TPq[0:64, :], AF.Copy), r=[bkey(0)], w=[f"qTe{s}"])
                    P.op("act", lambda e: e.activation(qTo[64:128, s, :], TPq[64:128, :], AF.Copy), r=[bkey(0)], w=[f"qTo{s}"])
                    P.op("act", lambda e: e.activation(kT[:, s, :], TPk, AF.Copy), r=[bkey(6)], w=[f"kT{s}"])
                    if cut < 2.25:
                        continue
                    rv = cfg.get("ret_var", 0)
                    if rv == 1:
                        P.op("dve", lambda e: e.tensor_tensor(qxf[:, s, :], TPq, bmask, ALU.mult),
                             r=[bkey(0), "bmask"], w=[f"qxf{s}"])
                    elif rv == 2:
                        P.op("dve", lambda e: e.tensor_copy(qxf[:, s, :], TPq), r=[bkey(0)], w=[f"qxf{s}"])
                    elif rv == 3:
                        P.op("dve", lambda e: e.tensor_tensor(qxf[:, s, :], qTe[:, s, :], xiT[:, 0, :, :].rearrange("p j q -> p (j q)"), ALU.mult),
                             r=[f"qTe{s}", "xiT"], w=[f"qxf{s}"])
                    else:
                        P.op("dve", lambda e: e.tensor_tensor(qxf[:, s, :], TPq, xiT[:, 0, :, :].rearrange("p j q -> p (j q)"), ALU.mult),
                             r=[bkey(0), "xiT"], w=[f"qxf{s}"])
                        P.op("dve", lambda e: e.tensor_tensor(qxb[:, s, :], TPq, xiT[:, 1, :, :].rearrange("p j q -> p (j q)"), ALU.mult),
                             r=[bkey(0), "xiT"], w=[f"qxb{s}"])
                    if cut < 3:
                        continue
                    for h in range(8):
                        j, hf = h // 2, h % 2
                        rows = slice(hf * 64, hf * 64 + 64)
                        sb_ = 1 + h // 4
                        qz = qTe if hf == 0 else qTo
                        P.op("pe", lambda e: e.matmul(bank(sb_)[:, (h % 4) * 128:(h % 4 + 1) * 128], kT[:, s, j * 128:(j + 1) * 128],
                                                      qz[:, s, j * 128:(j + 1) * 128], start=True, stop=True),
                             r=[f"kT{s}", f"qTe{s}", f"qTo{s}"], w=[bkey(sb_)])
                    for hb in range(2):
                        P.op("dve", lambda e: e.tensor_tensor(inn[:, s, hb * 512:(hb + 1) * 512], bank(1 + hb),
                                                              dm[:, hb * 4:(hb + 1) * 4, :].rearrange("p h q -> p (h q)"), ALU.mult),
                             r=[bkey(1 + hb), "dm"], w=[f"inn{s}"])
                    if cut < 4:
                        continue
                    for j in range(4):
                        pc = slice(j * 128, (j + 1) * 128)
                        P.op("pe", lambda e: e.matmul(bank(yb)[:, pc], qxf[:, s, pc], SfAll[:, i, pc], start=True, stop=False),
                             r=[f"qxf{s}", "SfAll"], w=[bkey(yb)])
                        P.op("pe", lambda e: e.matmul(bank(yb)[:, pc], qxb[:, s, pc], stb_bf[:, pc], start=False, stop=False),
                             r=[f"qxb{s}", "stb_bf"], w=[bkey(yb)])
                        for hf in range(2):
                            h = 2 * j + hf
                            P.op("pe", lambda e: e.matmul(bank(yb)[:, h * 64:(h + 1) * 64], inn[:, s, h * 128:(h + 1) * 128],
                                                          zin[:, s, 1024 + h * 64:1024 + (h + 1) * 64], start=False, stop=(hf == 1)),
                                 r=[f"inn{s}", zk], w=[bkey(yb)])
                    if cut < 5:
                        continue
                    Yv = bank(yb).rearrange("p (h d) -> p h d", d=64)
                    s1, s2, mean, msq, var, sd, rstd = [st8[:, k, :] for k in range(7)]
                    P.op("dve", lambda e: e.tensor_reduce(s1, Yv, AX.X, ALU.add), r=[bkey(yb)], w=["st_s1"])
                    P.op("act", lambda e: e.activation(sqv, bank(yb), AF.Square), r=[bkey(yb)], w=["sqv"])
                    P.op("dve", lambda e: e.tensor_reduce(s2, sqv.rearrange("p (h d) -> p h d", d=64), AX.X, ALU.add), r=["sqv"], w=["st_s2"])
                    P.op("dve", lambda e: e.tensor_scalar(mean, s1, 1.0 / 64, None, ALU.mult), r=["st_s1"], w=["st_mean"])
                    P.op("dve", lambda e: e.tensor_tensor(msq, mean, mean, ALU.mult), r=["st_mean"], w=["st_msq"])
                    P.op("dve", lambda e: e.scalar_tensor_tensor(var, s2, 1.0 / 64, msq, ALU.mult, ALU.subtract), r=["st_s2", "st_msq"], w=["st_var"])
                    P.op("act", lambda e: e.activation(sd, var, AF.Sqrt, bias=epsb[:, 0:1], scale=1.0), r=["st_var", "epsb"], w=["st_sd"])
                    P.op("dve", lambda e: e.reciprocal(rstd, sd), r=["st_sd"], w=["st_rstd"])
                    t1v = t1.rearrange("p (h d) -> p h d", d=64)
                    t2v = t2.rearrange("p (h d) -> p h d", d=64)
                    P.op("dve", lambda e: e.tensor_tensor(t1v, Yv, mean.unsqueeze(2).to_broadcast([128, 8, 64]), ALU.subtract),
                         r=[bkey(yb), "st_mean"], w=["t1"])
                    P.op("dve", lambda e: e.tensor_tensor(t2v, t1v, rstd.unsqueeze(2).to_broadcast([128, 8, 64]), ALU.mult),
                         r=["t1", "st_rstd"], w=["t2"])
                    P.op("pool", lambda e: e.tensor_tensor(yr[:, s, :], t2, zin[:, s, 1536:2048], ALU.mult), r=["t2", zk], w=[f"yr{s}"])
                    if cut < 6:
                        continue
                    TY = bank(5)
                    for j in range(4):
                        P.op("pe", lambda e: e.matmul(TY[:, j * 128:(j + 1) * 128], yr[:, s, j * 128:(j + 1) * 128], identB, start=True, stop=True),
                             r=[f"yr{s}", "identB"], w=[bkey(5)])
                    if i < 16:
                        grp, pos, gw = i // 4, i % 4, 512
                        g0 = grp * 512
                    else:
                        grp, pos, gw = 4, i - 16, 256
                        g0 = 2048
                    ys = grp % 2
                    P.op("act", lambda e: e.activation(yst[:, ys, :, pos * 128:(pos + 1) * 128], TY[:, 0:512].rearrange("p (j n) -> p j n", j=4), AF.Copy),
                         r=[bkey(5)], w=[f"yst{ys}"])
                    if pos == 0:
                        P.dma("sp", YTv[:, 0:4, g0:g0 + gw], yst[:, ys, :, 0:gw], r=[f"yst{ys}"], w=["YT"])
                if n < len(bwd_order) - 1:
                    kv_update(i, 1, zin[:, s, 512:1536], zk, stb, "stb", s)
                    P.op("act", lambda e: e.activation(stb_bf, stb, AF.Copy), r=["stb"], w=["stb_bf"])
            P.barrier()

    def phase_conv(l):
        need_ctx = l < DEPTH - 1
        with ExitStack() as es:
            def sb(name, shape, dt=F32):
                return es.enter_context(nc.sbuf_tensor(U(name), shape, dt)).ap()
            cw = sb("cw", [128, 4, 3])
            bch = sb("bch", [128, 2, 3, NT], BF16)
            u = sb("u", [128, 2, NT]); yv = sb("yv", [128, 2, NT]); ob = sb("ob", [128, 2, NT], BF16)
            P.dma("sp", cw, convw_t[l], w=["cw"])
            nend = NT if need_ctx else L
            seqs = [(0, L)] + ([(L, NT)] if need_ctx else [])
            for cc in range(4):
                s = cc % 2
                for k3, off in enumerate((ZF_CB, ZF_CC, ZF_CH)):
                    P.dma("sp", bch[:, s, k3, 0:nend], ZF[off + cc * 128:off + (cc + 1) * 128, 0:nend], r=["ZF"], w=[f"bch{s}"])
                P.op("dve", lambda e: e.tensor_tensor(u[:, s, 0:nend], bch[:, s, 1, 0:nend], bch[:, s, 2, 0:nend], ALU.mult), r=[f"bch{s}"], w=[f"u{s}"])
                P.op("act", lambda e: e.activation(yv[:, s, 0:nend], u[:, s, 0:nend], AF.Copy, scale=cw[:, cc, 1:2]), r=[f"u{s}", "cw"], w=[f"yv{s}"])
                for (s0, s1) in seqs:
                    P.op("dve", lambda e: e.scalar_tensor_tensor(yv[:, s, s0 + 1:s1], u[:, s, s0:s1 - 1], cw[:, cc, 0:1], yv[:, s, s0 + 1:s1], ALU.mult, ALU.add),
                         r=[f"u{s}", "cw", f"yv{s}"], w=[f"yv{s}"])
                    P.op("dve", lambda e: e.scalar_tensor_tensor(yv[:, s, s0:s1 - 1], u[:, s, s0 + 1:s1], cw[:, cc, 2:3], yv[:, s, s0:s1 - 1], ALU.mult, ALU.add),
                         r=[f"u{s}", "cw", f"yv{s}"], w=[f"yv{s}"])
                P.op("pool", lambda e: e.tensor_tensor(ob[:, s, 0:nend], yv[:, s, 0:nend], bch[:, s, 0, 0:nend], ALU.mult), r=[f"yv{s}", f"bch{s}"], w=[f"ob{s}"])
                P.dma("sp", YT[512 + cc * 128:512 + (cc + 1) * 128, 0:nend], ob[:, s, 0:nend], r=[f"ob{s}"], w=["YT"])
            P.barrier()

    def phase_swa(l):
        need_ctx = l < DEPTH - 1
        with ExitStack() as es:
            def sb(name, shape, dt=F32):
                return es.enter_context(nc.sbuf_tensor(U(name), shape, dt)).ap()
            esink = sb("esink", [128, 8]); mkf = sb("mkf", [128, 2, 128]); mk = sb("mk", [128, 2, 128], BF16)
            QT0 = sb("QT0", [128, 4, NT], BF16); QT1 = sb("QT1", [128, 4, NT], BF16)
            KT = sb("KT", [128, NT], BF16); Vp = sb("Vp", [128, 18, 2, 65], BF16)
            P.op("pool", lambda e: e.memset(QT0[64:128], 0.0), w=["QT"])
            P.op("pool", lambda e: e.memset(QT1[0:64], 0.0), w=["QT"])
            zin = sb("zin", [128, 2, 768], BF16)
            PT = sb("PT", [128, 2, 5, 512], BF16)
            den = sb("den", [128, 2, 4]); rec = sb("rec", [128, 2, 4])
            ysw = sb("ysw", [128, 2, 512], BF16); yst = sb("yst", [128, 2, 4, 512], BF16)
            P.dma("sp", esink, sink_in[l:l + 1, :].partition_broadcast(128), w=["esink"])
            P.op("act", lambda e: e.activation(esink, esink, AF.Exp), r=["esink"], w=["esink"])
            P.dma("sp", mkf, swamask_in, w=["mkf"])
            P.op("dve", lambda e: e.tensor_copy(mk, mkf), r=["mkf"], w=["mk"])
            P.op("pool", lambda e: e.memset(Vp, 1.0), w=["Vp"])
            for tt in range(18):
                s = tt % 2
                zk = f"zin{s}"
                P.dma("sp", zin[:, s, :], ZT[tt * 128:(tt + 1) * 128, ZT_SQ:ZT_SQ + 768], r=["ZT"], w=[zk])
                tb = s
                TP = bank(tb)
                TPk = bank(2 + s)
                for i4 in range(4):
                    P.op("pe", lambda e: e.matmul(TP[:, i4 * 128:(i4 + 1) * 128], zin[:, s, i4 * 128:(i4 + 1) * 128], identB, start=True, stop=True),
                         r=[zk, "identB"], w=[bkey(tb)])
                P.op("pe", lambda e: e.matmul(TPk[:, 0:128], zin[:, s, 512:640], identB, start=True, stop=True), r=[zk, "identB"], w=[bkey(2 + s)])
                P.op("act", lambda e: e.activation(QT0[0:64, :, tt * 128:(tt + 1) * 128], TP[0:64, 0:512].rearrange("p (i n) -> p i n", i=4), AF.Copy),
                     r=[bkey(tb), "QT"], w=["QT"])
                P.op("act", lambda e: e.activation(QT1[64:128, :, tt * 128:(tt + 1) * 128], TP[64:128, 0:512].rearrange("p (i n) -> p i n", i=4), AF.Copy),
                     r=[bkey(tb), "QT"], w=["QT"])
                P.op("dve", lambda e: e.tensor_copy(KT[:, tt * 128:(tt + 1) * 128], TPk[:, 0:128]), r=[bkey(2 + s)], w=["KT"])
                P.op("pool", lambda e: e.tensor_copy(Vp[:, tt, :, 0:64], zin[:, s, 640:768].rearrange("p (k d) -> p k d", d=64)), r=[zk, "Vp"], w=["Vp"])
            order = ([17, 16] if need_ctx else []) + list(range(15, -1, -1))
            its = [(blk, kv) for blk in order for kv in range(2)]

            def tiles_of(blk):
                if blk < 16:
                    tiles = [(16, None), (17, None)]
                    if blk > 0:
                        tiles.append((blk - 1, 0))
                    tiles.append((blk, None))
                    if blk < 15:
                        tiles.append((blk + 1, 1))
                    return tiles
                return [(16, None), (17, None)]

            def sw_scores(i):
                blk, kv = its[i]
                ps = i % 2
                QZ = QT0 if kv == 0 else QT1
                for jj, (kt, mi) in enumerate(tiles_of(blk)):
                    b = 2 + (jj % 3)
                    P.op("pe", lambda e: e.matmul(bank(b), KT[:, kt * 128:(kt + 1) * 128], QZ[:, :, blk * 128:(blk + 1) * 128],
                                                  start=True, stop=True), r=["KT", "QT"], w=[bkey(b)])
                    P.op("act", lambda e: e.activation(PT[:, ps, jj, :], bank(b), AF.Exp, scale=0.125), r=[bkey(b)], w=[f"PT{ps}_{jj}"])
                    if mi is not None:
                        P.op("pool", lambda e: e.tensor_tensor(PT[:, ps, jj, :].rearrange("p (i q) -> p i q", i=4),
                                                               PT[:, ps, jj, :].rearrange("p (i q) -> p i q", i=4),
                                                               mk[:, mi, :].unsqueeze(1).to_broadcast([128, 4, 128]), ALU.mult),
                             r=[f"PT{ps}_{jj}", "mk"], w=[f"PT{ps}_{jj}"])

            def sw_pv(i):
                blk, kv = its[i]
                ps = i % 2
                tiles = tiles_of(blk)
                ob_ = 5 + ps
                for i4 in range(4):
                    for jj, (kt, mi) in enumerate(tiles):
                        P.op("pe", lambda e: e.matmul(bank(ob_)[:, i4 * 65:(i4 + 1) * 65], PT[:, ps, jj, i4 * 128:(i4 + 1) * 128], Vp[:, kt, kv, :],
                                                      start=(jj == 0), stop=(jj == len(tiles) - 1)),
                             r=[f"PT{ps}_{jj}", "Vp"], w=[bkey(ob_)])

            def sw_norm(i):
                blk, kv = its[i]
                ps = i % 2
                ob_ = 5 + ps
                ysb = (i // 2) % 2
                Ov = bank(ob_)[:, 0:260].rearrange("p (i d) -> p i d", d=65)
                P.op("dve", lambda e: e.tensor_tensor(den[:, ps, :], Ov[:, :, 64], esink[:, kv * 4:(kv + 1) * 4], ALU.add),
                     r=[bkey(ob_), "esink"], w=[f"den{ps}"])
                P.op("dve", lambda e: e.reciprocal(rec[:, ps, :], den[:, ps, :]), r=[f"den{ps}"], w=[f"rec{ps}"])
                P.op("dve", lambda e: e.tensor_tensor(ysw[:, ysb, kv * 256:(kv + 1) * 256].rearrange("p (i d) -> p i d", d=64), Ov[:, :, 0:64],
                                                      rec[:, ps, :].unsqueeze(2).to_broadcast([128, 4, 64]), ALU.mult),
                     r=[bkey(ob_), f"rec{ps}"], w=[f"ysw{ysb}"])
                if kv == 1:
                    TY = bank(7)
                    for j in range(4):
                        P.op("pe", lambda e: e.matmul(TY[:, j * 128:(j + 1) * 128], ysw[:, ysb, j * 128:(j + 1) * 128], identB, start=True, stop=True),
                             r=[f"ysw{ysb}", "identB"], w=[bkey(7)])
                    if blk < 16:
                        grp, pos, gw, g0 = blk // 4, blk % 4, 512, (blk // 4) * 512
                    else:
                        grp, pos, gw, g0 = 4, blk - 16, 256, 2048
                    ys = grp % 2
                    P.op("act", lambda e: e.activation(yst[:, ys, :, pos * 128:(pos + 1) * 128], TY[:, 0:512].rearrange("p (j n) -> p j n", j=4), AF.Copy),
                         r=[bkey(7)], w=[f"yst{ys}"])
                    if pos == 0:
                        P.dma("sp", YTv[:, 8:12, g0:g0 + gw], yst[:, ys, :, 0:gw], r=[f"yst{ys}"], w=["YT"])

            n_it = len(its)
            sw_scores(0)
            for i in range(n_it):
                if i + 1 < n_it:
                    sw_scores(i + 1)
                if i >= 1:
                    sw_norm(i - 1)
                sw_pv(i)
            sw_norm(n_it - 1)
            P.barrier()

    def phase_na(l):
        need_ctx = l < DEPTH - 1
        with ExitStack() as es:
            def sb(name, shape, dt=F32):
                return es.enter_context(nc.sbuf_tensor(U(name), shape, dt)).ap()
            QTe = sb("QTne", [128, 4, NT], BF16); QTo = sb("QTno", [128, 4, NT], BF16); KT = sb("KTn", [128, 4, NT], BF16)
            Vp = sb("Vpn", [128, 18, 8, 65], BF16); vtmp = sb("vtmp", [128, 2, 512], BF16)
            bias = sb("biasn", [128, 2, 25, 128])
            PT = sb("PTn", [128, 2, 7, 128], BF16); tmp = sb("tmpn", [128, 2, 640])
            rec = sb("recn", [128, 2, 1]); yna = sb("yna", [128, 18, 512], BF16)
            yst = sb("ystn", [128, 2, 4, 512], BF16)
            P.op("pool", lambda e: e.memset(QTe[64:128], 0.0), w=["QTe"])
            P.op("pool", lambda e: e.memset(QTo[0:64], 0.0), w=["QTo"])
            zq = ZF[ZF_NQ:ZF_NQ + 512, :].rearrange("(j p) n -> p j n", p=128)
            P.dma("sp", QTe[0:64], zq[0:64], r=["ZF", "QTe"], w=["QTe"])
            P.dma("sp", QTo[64:128], zq[64:128], r=["ZF", "QTo"], w=["QTo"])
            P.dma("sp", KT, ZF[ZF_NK:ZF_NK + 512, :].rearrange("(j p) n -> p j n", p=128), r=["ZF"], w=["KT"])
            P.op("pool", lambda e: e.memset(Vp, 1.0), w=["Vp"])
            for tt in range(18):
                s = tt % 2
                P.dma("sp", vtmp[:, s, :], ZT[tt * 128:(tt + 1) * 128, ZT_NV:ZT_NV + 512], r=["ZT"], w=[f"vtmp{s}"])
                P.op("pool", lambda e: e.tensor_copy(Vp[:, tt, :, 0:64], vtmp[:, s, :].rearrange("p (h d) -> p h d", d=64)),
                     r=[f"vtmp{s}", "Vp"], w=["Vp"])
            nq = 18 if need_ctx else 16
            bgring = None
            if ada_pending:
                bgring = Ring(es, nslots=3)
            iters = [(h, m) for h in range(8) for m in range(nq)]

            def info(k):
                h, m = iters[k]
                lat = na_tiles(m) if m < 16 else []
                return h, m, k % 2, lat, [16, 17] + lat

            def st_scores(k):
                h, m, ps, lat, tiles = info(k)
                j, hf = h // 2, h % 2
                bs = h % 2
                if m == 0:
                    P.dma("sp", bias[:, bs, :, :].rearrange("p a q -> p (a q)"), na_bias[l, h], w=[f"bias{bs}"])
                bA, bB = (0, 1) if ps == 0 else (2, 3)
                QZ = QTe if hf == 0 else QTo
                for jj, kt in enumerate(tiles):
                    b = bA if jj < 4 else bB
                    P.op("pe", lambda e: e.matmul(bank(b)[:, (jj % 4) * 128:(jj % 4 + 1) * 128], KT[:, j, kt * 128:(kt + 1) * 128],
                                                  QZ[:, j, m * 128:(m + 1) * 128], start=True, stop=True),
                         r=["KT", "QTe", "QTo"], w=[bkey(b)])

            def st_soft(k):
                h, m, ps, lat, tiles = info(k)
                bs = h % 2
                nl = len(lat)
                bA, bB = (0, 1) if ps == 0 else (2, 3)
                P.op("act", lambda e: e.activation(PT[:, ps, 0:2, :].rearrange("p a q -> p (a q)"), bank(bA)[:, 0:256], AF.Exp, scale=0.125),
                     r=[bkey(bA)], w=[f"PTa{ps}"])
                if nl:
                    cl = na_cls(m)
                    P.op("dve", lambda e: e.scalar_tensor_tensor(tmp[:, ps, 0:256], bank(bA)[:, 256:512], 0.125,
                                                                 bias[:, bs, cl * 5:cl * 5 + 2, :].rearrange("p a q -> p (a q)"), ALU.mult, ALU.add),
                         r=[bkey(bA), f"bias{bs}"], w=[f"tmp{ps}"])
                    P.op("dve", lambda e: e.scalar_tensor_tensor(tmp[:, ps, 256:nl * 128], bank(bB)[:, 0:(nl - 2) * 128], 0.125,
                                                                 bias[:, bs, cl * 5 + 2:cl * 5 + nl, :].rearrange("p a q -> p (a q)"), ALU.mult, ALU.add),
                         r=[bkey(bB), f"bias{bs}"], w=[f"tmp{ps}"])
                    P.op("act", lambda e: e.activation(PT[:, ps, 2:2 + nl, :].rearrange("p a q -> p (a q)"), tmp[:, ps, 0:nl * 128], AF.Exp),
                         r=[f"tmp{ps}"], w=[f"PTb{ps}"])

            def st_pv(k):
                h, m, ps, lat, tiles = info(k)
                ob_ = 4 + ps
                for jj, kt in enumerate(tiles):
                    P.op("pe", lambda e: e.matmul(bank(ob_)[:, 0:65], PT[:, ps, jj, :], Vp[:, kt, h, :],
                                                  start=(jj == 0), stop=(jj == len(tiles) - 1)),
                         r=[f"PTa{ps}", f"PTb{ps}", "Vp"], w=[bkey(ob_)])

            def st_norm(k):
                h, m, ps, lat, tiles = info(k)
                ob_ = 4 + ps
                P.op("dve", lambda e: e.reciprocal(rec[:, ps, :], bank(ob_)[:, 64:65]), r=[bkey(ob_)], w=[f"rec{ps}"])
                P.op("dve", lambda e: e.tensor_scalar(yna[:, m, h * 64:(h + 1) * 64], bank(ob_)[:, 0:64], rec[:, ps, 0:1], None, ALU.mult),
                     r=[bkey(ob_), f"rec{ps}"], w=["yna"])

            nit = len(iters)
            loaded = []
            had_bg = bool(ada_pending)

            def bg_prefetch():
                if ada_pending and len(loaded) < 2:
                    al, afg = ada_pending.pop(0)
                    loaded.append((al, afg, ada_load(al, afg, bgring)))

            def bg_compute():
                if loaded:
                    al, afg, ld = loaded.pop(0)
                    ada_tile(al, afg, bgring, (6, 7), loaded=ld)
            bg_prefetch()
            bg_prefetch()
            st_scores(0)
            for k in range(nit):
                if k + 1 < nit:
                    st_scores(k + 1)
                st_soft(k)
                if k >= 1:
                    st_norm(k - 1)
                st_pv(k)
                if had_bg and k % 3 == 2:
                    bg_compute()
                    bg_prefetch()
            st_norm(nit - 1)
            while loaded or ada_pending:
                bg_prefetch()
                bg_compute()
            if had_bg:
                ada_finish()
            for m in range(nq):
                tb = 6 + (m % 2)
                TY = bank(tb)
                for jx in range(4):
                    P.op("pe", lambda e: e.matmul(TY[:, jx * 128:(jx + 1) * 128], yna[:, m, jx * 128:(jx + 1) * 128], identB, start=True, stop=True),
                         r=["yna", "identB"], w=[bkey(tb)])
                if m < 16:
                    grp, pos, gw, g0 = m // 4, m % 4, 512, (m // 4) * 512
                    last = pos == 3
                else:
                    grp, pos, gw, g0 = 4, m - 16, 256, 2048
                    last = pos == 1
                ys = grp % 2
                P.op("act", lambda e: e.activation(yst[:, ys, :, pos * 128:(pos + 1) * 128], TY[:, 0:512].rearrange("p (j n) -> p j n", j=4), AF.Copy),
                     r=[bkey(tb)], w=[f"yst{ys}"])
                if last:
                    P.dma("sp", YTv[:, 12:16, g0:g0 + gw], yst[:, ys, :, 0:gw], r=[f"yst{ys}"], w=["YT"])
            P.barrier()

    def phase_outproj(l):
        need_ctx = l < DEPTH - 1
        with ExitStack() as es:
            ring = Ring(es)
            ytsb = es.enter_context(nc.sbuf_tensor(U("ytsb"), [128, 16, NT], BF16)).ap()
            xt = es.enter_context(nc.sbuf_tensor(U("xt"), [128, 3, 512], F32)).ap()
            nend = NT if need_ctx else L
            for c4 in range(4):
                P.dma("sp", ytsb[:, c4 * 4:(c4 + 1) * 4, 0:nend], YTv[:, c4 * 4:(c4 + 1) * 4, 0:nend], r=["YT"], w=["ytsb"])
            groups = GROUPS if need_ctx else GROUPS[:4]
            items = [(mg, mm, g0, gsz) for mg in range(4) for mm in range(4) for (g0, gsz) in groups]

            def xload(i):
                mg, mm, g0, gsz = items[i]
                m = mg * 4 + mm
                xs = i % 3
                P.dma("sp", xt[:, xs, 0:gsz], XT[m * 128:(m + 1) * 128, g0:g0 + gsz], r=["XT"], w=[f"xt{xs}"])
            wt = wk = None
            xload(0)
            for i, (mg, mm, g0, gsz) in enumerate(items):
                if mm == 0 and g0 == 0:
                    wt, wk = ring.load(w_out[l, :, mg * 512:(mg + 1) * 512].rearrange("(c p) n -> p c n", p=128),
                                       lambda t: t.rearrange("p (c n) -> p c n", c=16))
                if i + 1 < len(items):
                    xload(i + 1)
                m = mg * 4 + mm
                r = 1 if g0 >= L else 0
                xs = i % 3
                b = i % 6
                for c in range(16):
                    P.op("pe", lambda e: e.matmul(bank(b)[:, 0:gsz], wt[:, c, mm * 128:(mm + 1) * 128], ytsb[:, c, g0:g0 + gsz],
                                                  start=(c == 0), stop=(c == 15)), r=[wk, "ytsb"], w=[bkey(b)])
                P.op("dve", lambda e: e.scalar_tensor_tensor(xt[:, xs, 0:gsz], bank(b)[:, 0:gsz], ada_vec(l, 2, r)[:, m:m + 1],
                                                             xt[:, xs, 0:gsz], ALU.mult, ALU.add),
                     r=[bkey(b), f"xt{xs}", "ada_sb"], w=[f"xt{xs}"])
                P.dma("sp", XT[m * 128:(m + 1) * 128, g0:g0 + gsz], xt[:, xs, 0:gsz], r=[f"xt{xs}"], w=["XT"])
            P.barrier()

    def phase_ffn(l):
        need_ctx = l < DEPTH - 1
        grps = GROUPS if need_ctx else GROUPS[:4]
        nsl = NSL if need_ctx else CAP
        with ExitStack() as esf:
            def sbf(name, shape, dt=F32):
                return esf.enter_context(nc.sbuf_tensor(U(name), shape, dt)).ap()
            idxf = sbf("idxf", [16, CAP]); vals = sbf("vals", [16, CAP]); idxu = sbf("idxu", [16, CAP], U32)
            idxfc = sbf("idxfc", [16, CAPC]); valsc = sbf("valsc", [16, CAPC]); idxuc = sbf("idxuc", [16, CAPC], U32)
            idxT = sbf("idxT", [128, 16, 2]); gateT = sbf("gateT", [128, 16, 2])
            gateTc = sbf("gateTc", [32, 16]); idxTcu = sbf("idxTcu", [32, 16]); idxTcP = sbf("idxTcP", [128, 4])
            idxTu = sbf("idxTu", [128, 16, 2], U32); idxTcU = sbf("idxTcU", [32, 16], U32)
            with ExitStack() as esh:
                with ExitStack() as es:
                    def sb(name, shape, dt=F32):
                        return es.enter_context(nc.sbuf_tensor(U(name), shape, dt)).ap()
                    xg = sb("xg", [128, 16, 512]); nb = alloc_norm_bufs(es)
                    h32 = sb("h32", [128, 3, 512]); hg = sb("hg", [128, 16, 512], BF16)
                    wr = sb("wr", [128, 16, 16]); E_sb = sb("E_sb", [16, NT]); aff = sb("aff", [16, NT]); rs = sb("rs", [16, 512])
                    h2st = sb("h2st", [128, 2, 2048], BF16)
                    P.dma("sp", wr, w_router[l].rearrange("(c p) e -> p c e", p=128), w=["wr"])
                    zt = sb("zt", [128, 2048])
                    P.op("pool", lambda e: e.memset(zt, 0.0), w=["zt"])
                    for tz in range(18 if need_ctx else 16):
                        P.dma("sp", MOE[tz * 128:(tz + 1) * 128, :], zt, r=["zt"], w=["MOE"])
                    bi = 0
                    for (g0, gsz) in grps:
                        r = 1 if g0 >= L else 0
                        P.dma("sp", xg[:, :, 0:gsz], XTv[:, :, g0:g0 + gsz], r=["XT"], w=["xg"])

                        def out_fn(c, tc, key, r=r, gsz=gsz):
                            s = c % 3
                            P.op("act", lambda e: e.activation(h32[:, s, 0:gsz], tc, AF.Identity, bias=ada_vec(l, 3, r)[:, c:c + 1], scale=1.0),
                                 r=[key, "ada_sb"], w=[f"h32_{s}"])
                            P.op("pe", lambda e: e.matmul(bank(6)[0:16, 0:gsz], wr[:, c, :], h32[:, s, 0:gsz], start=(c == 0), stop=(c == 15)),
                                 r=["wr", f"h32_{s}"], w=[bkey(6)])
                            P.op("pool", lambda e: e.tensor_copy(hg[:, c, 0:gsz], h32[:, s, 0:gsz]), r=[f"h32_{s}"], w=["hg"])
                        norm_group(nb, xg, gsz, scl[:, l, 1, :, r], None, out_fn)
                        P.op("act", lambda e: e.activation(E_sb[:, g0:g0 + gsz], bank(6)[0:16, 0:gsz], AF.Exp), r=[bkey(6)], w=["E_sb"])
                        for tt in range(gsz // 128):
                            tile = (g0 // 128) + tt
                            for cb in range(4):
                                b = bi % 6
                                bi += 1
                                for cc in range(4):
                                    c = cb * 4 + cc
                                    P.op("pe", lambda e: e.matmul(bank(b)[:, cc * 128:(cc + 1) * 128], hg[:, c, tt * 128:(tt + 1) * 128], identB,
                                                                  start=True, stop=True), r=["hg", "identB"], w=[bkey(b)])
                                copy_op(evac_eng(), h2st[:, tile % 2, cb * 512:(cb + 1) * 512], bank(b), r=[bkey(b)], w=[f"h2st{tile % 2}"])
                            P.dma("sp", H2D[tile * 128:(tile + 1) * 128, :], h2st[:, tile % 2, :], r=[f"h2st{tile % 2}"], w=["H2D"])
                    for (g0, gsz) in grps:
                        P.op("pe", lambda e: e.matmul(bank(7)[0:16, 0:gsz], onesF[0:16, 0:16], E_sb[:, g0:g0 + gsz], start=True, stop=True),
                             r=["onesF", "E_sb"], w=[bkey(7)])
                        P.op("dve", lambda e: e.reciprocal(rs[:, 0:gsz], bank(7)[0:16, 0:gsz]), r=[bkey(7)], w=["rs"])
                        P.op("dve", lambda e: e.tensor_tensor(aff[:, g0:g0 + gsz], E_sb[:, g0:g0 + gsz], rs[:, 0:gsz], ALU.mult), r=["E_sb", "rs"], w=["aff"])
                    for (a0, n, k, vv, iu, ifl, kk) in ((0, L, CAP, vals, idxu, idxf, "l"),) + (((L, T, CAPC, valsc, idxuc, idxfc, "c"),) if need_ctx else ()):
                        aw = aff[:, a0:a0 + n]
                        for it in range(k // 8):
                            v8 = vv[:, it * 8:(it + 1) * 8]
                            P.op("dve", lambda e: e.max(v8, aw), r=["aff"], w=["v8" + kk])
                            P.op("dve", lambda e: e.max_index(iu[:, it * 8:(it + 1) * 8], v8, aw), r=["aff", "v8" + kk], w=["iu" + kk])
                            P.op("dve", lambda e: e.match_replace(aw, v8, aw, -1.0), r=["v8" + kk, "aff"], w=["aff"])
                        P.op("dve", lambda e: e.tensor_copy(ifl, iu), r=["iu" + kk], w=["idxf" + kk])
                    for t2 in range(2):
                        P.op("pe", lambda e: e.transpose(bank(0)[:, t2 * 16:(t2 + 1) * 16], idxf[0:16, t2 * 128:(t2 + 1) * 128], identF[0:16, 0:16]),
                             r=["idxfl", "identF"], w=[bkey(0)])
                        P.op("pe", lambda e: e.transpose(bank(1)[:, t2 * 16:(t2 + 1) * 16], vals[0:16, t2 * 128:(t2 + 1) * 128], identF[0:16, 0:16]),
                             r=["v8l", "identF"], w=[bkey(1)])
                    P.op("dve", lambda e: e.tensor_copy(idxT.rearrange("p e t -> p t e"), bank(0)[:, 0:32].rearrange("p (t e) -> p t e", t=2)), r=[bkey(0)], w=["idxT"])
                    P.op("dve", lambda e: e.tensor_copy(gateT.rearrange("p e t -> p t e"), bank(1)[:, 0:32].rearrange("p (t e) -> p t e", t=2)), r=[bkey(1)], w=["gateT"])
                    P.op("dve", lambda e: e.tensor_copy(idxTu, idxT), r=["idxT"], w=["idxTu"])
                    if need_ctx:
                        P.op("pe", lambda e: e.transpose(bank(2)[0:32, 0:16], idxfc[0:16, 0:32], identF[0:16, 0:16]), r=["idxfc", "identF"], w=[bkey(2)])
                        P.op("pe", lambda e: e.transpose(bank(3)[0:32, 0:16], valsc[0:16, 0:32], identF[0:16, 0:16]), r=["v8c", "identF"], w=[bkey(3)])
                        P.op("dve", lambda e: e.tensor_copy(idxTcu, bank(2)[0:32, 0:16]), r=[bkey(2)], w=["idxTcu"])
                        P.op("dve", lambda e: e.tensor_copy(gateTc, bank(3)[0:32, 0:16]), r=[bkey(3)], w=["gateTc"])
                        P.op("dve", lambda e: e.tensor_copy(idxTcU, idxTcu), r=["idxTcu"], w=["idxTcU"])
                        P.dma("sp", IDXC, idxTcu, r=["idxTcu"], w=["IDXC"])
                        for j4 in range(4):
                            P.dma("sp", idxTcP[j4 * 32:(j4 + 1) * 32, :], IDXC.rearrange("s (t j) -> s j t", j=4)[:, j4, :], r=["IDXC"], w=["idxTcP"],
                                  allow_slow_non_contiguous=True)
                    if dbg:
                        P.dma("sp", DBG1[:, 0:CAP], idxf, r=["idxfl"], w=["DBG1"])
                        P.dma("sp", DBG1[:, CAP:2 * CAP], vals, r=["v8l"], w=["DBG1"])
                    P.barrier()
                with ExitStack() as es:
                    def sb(name, shape, dt=F32):
                        return es.enter_context(nc.sbuf_tensor(U(name), shape, dt)).ap()
                    ring = Ring(es)
                    xs = sb("xs", [128, 2, 3, 2048], BF16)
                    xsT = sb("xsT", [128, 16, NSL], BF16); actT = sb("actT", [128, 8, NSL], BF16); sA = sb("sA", [128, 2, NSL])
                    yest = sb("yest", [128, 6, 2048], BF16)
                    gi = 0
                    yi = 0
                    ne = cfg.get("n_experts", NE)

                    def issue_scatter(ex):
                        for (s0, ssz, st) in stiles_all:
                            ys = (ex % 2) * 3 + st
                            prev = [f"MOEx{ex - 1}_{k}" for k in range(3)]
                            if st < 2:
                                P.dma_scatter_add(MOE, idxTu[:, ex, st:st + 1], yest[:, ys, :], r=[f"yest{ys}", "idxTu"] + prev, w=[f"MOEx{ex}_{st}"])
                            else:
                                P.dma_scatter_add(MOE, idxTcU[0:32, ex:ex + 1], yest[0:32, ys, :], element_offset=L * D,
                                                  r=[f"yest{ys}", "idxTcU"] + prev, w=[f"MOEx{ex}_{st}"])
                    stiles_all = [(0, 128, 0), (128, 128, 1)] + ([(256, 32, 2)] if need_ctx else [])
                    gtiles = [(0, 128, 0), (128, 128, 1)] + ([(256, 32, 2)] if need_ctx else [])

                    def issue_gather(ex):
                        xb = ex % 2
                        for (s0, ssz, st) in gtiles:
                            if st < 2:
                                P.dma_gather(xs[:, xb, st, :], H2D, idxTu[:, ex, st:st + 1], r=["H2D", "idxTu"], w=[f"xs{xb}_{st}"])
                            else:
                                P.dma_gather(xs[0:32, xb, st, :], H2D, idxTcU[0:32, ex:ex + 1], element_offset=L * D,
                                             r=["H2D", "idxTcU"], w=[f"xs{xb}_{st}"])
                    if ne > 0:
                        issue_gather(0)
                    for ex in range(ne):
                        xb = ex % 2
                        if ex + 1 < ne:
                            issue_gather(ex + 1)
                        for (s0, ssz, st) in gtiles:
                            for cb in range(4):
                                b = 1 + gi % 4
                                gi += 1
                                for cc in range(4):
                                    c = cb * 4 + cc
                                    P.op("pe", lambda e: e.matmul(bank(b)[:, cc * 128:cc * 128 + ssz], xs[0:ssz, xb, st, c * 128:(c + 1) * 128],
                                                                  identB[0:ssz, 0:ssz], start=True, stop=True),
                                         r=[f"xs{xb}_{st}", "identB"], w=[bkey(b)])
                                copy_op(evac_eng(), xsT[:, cb * 4:(cb + 1) * 4, s0:s0 + ssz],
                                        bank(b).rearrange("p (c n) -> p c n", c=4)[:, :, 0:ssz], r=[bkey(b)], w=["xsT"])
                        for hf in range(2):
                            gsrc = WBG[ex] if ex < NPC else w_gate[l, ex]
                            usrc = WBU[ex] if ex < NPC else w_up[l, ex]
                            G, gk = ring.load(gsrc[:, hf * 512:(hf + 1) * 512].rearrange("(c p) n -> p c n", p=128),
                                              lambda t: t.rearrange("p (c n) -> p c n", c=16))
                            Uw, uk = ring.load(usrc[:, hf * 512:(hf + 1) * 512].rearrange("(c p) n -> p c n", p=128),
                                               lambda t: t.rearrange("p (c n) -> p c n", c=16))
                            for fcl in range(4):
                                fc = hf * 4 + fcl
                                bA, bU = (1, 2) if fcl % 2 == 0 else (3, 4)
                                for c in range(16):
                                    P.op("pe", lambda e: e.matmul(bank(bA)[:, 0:nsl], G[:, c, fcl * 128:(fcl + 1) * 128], xsT[:, c, 0:nsl], start=(c == 0), stop=(c == 15)),
                                         r=[gk, "xsT"], w=[bkey(bA)])
                                for c in range(16):
                                    P.op("pe", lambda e: e.matmul(bank(bU)[:, 0:nsl], Uw[:, c, fcl * 128:(fcl + 1) * 128], xsT[:, c, 0:nsl], start=(c == 0), stop=(c == 15)),
                                         r=[uk, "xsT"], w=[bkey(bU)])
                                ss = fcl % 2
                                P.op("act", lambda e: e.activation(sA[:, ss, 0:nsl], bank(bA)[:, 0:nsl], AF.Silu), r=[bkey(bA)], w=[f"sA{ss}"])
                                P.op("dve", lambda e: e.tensor_tensor(actT[:, fc, 0:nsl], sA[:, ss, 0:nsl], bank(bU)[:, 0:nsl], ALU.mult),
                                     r=[f"sA{ss}", bkey(bU)], w=["actT"])
                        if ex > 0:
                            issue_scatter(ex - 1)
                        Dt = []
                        for hf in range(2):
                            dsrc = WBD[ex] if ex < NPC else w_down[l, ex]
                            Dw, dk = ring.load(dsrc[hf * 512:(hf + 1) * 512, :].rearrange("(c p) n -> p c n", p=128),
                                               lambda t: t.rearrange("p (c n) -> p c n", c=4))
                            Dt.append((Dw, dk))
                        stiles = [(0, 128, 0), (128, 128, 1)] + ([(256, 32, 2)] if need_ctx else [])
                        for (s0, ssz, st) in stiles:
                            ys = (ex % 2) * 3 + st
                            for dg in range(4):
                                b = 5 + gi % 3
                                gi += 1
                                for fc in range(8):
                                    Dw, dk = Dt[fc // 4]
                                    P.op("pe", lambda e: e.matmul(bank(b)[0:ssz, :], actT[:, fc, s0:s0 + ssz], Dw[:, fc % 4, dg * 512:(dg + 1) * 512],
                                                                  start=(fc == 0), stop=(fc == 7)), r=["actT", dk], w=[bkey(b)])
                                gsc = gateT[:, ex, st:st + 1] if st < 2 else gateTc[:, ex:ex + 1]
                                eng = evac_eng()
                                if eng == "act":
                                    P.op("act", lambda e: e.activation(yest[0:ssz, ys, dg * 512:(dg + 1) * 512], bank(b)[0:ssz, :], AF.Copy, scale=gsc[0:ssz]),
                                         r=[bkey(b), "gateT", "gateTc"], w=[f"yest{ys}"])
                                else:
                                    P.op("dve", lambda e: e.tensor_scalar(yest[0:ssz, ys, dg * 512:(dg + 1) * 512], bank(b)[0:ssz, :], gsc[0:ssz], None, ALU.mult),
                                         r=[bkey(b), "gateT", "gateTc"], w=[f"yest{ys}"])
                    if ne > 0:
                        issue_scatter(ne - 1)
                    P.barrier()
            with ExitStack() as es:
                def sb(name, shape, dt=F32):
                    return es.enter_context(nc.sbuf_tensor(U(name), shape, dt)).ap()
                mt = sb("mt", [128, 4, 2048]); xg = sb("xg3", [128, 16, 512])
                bi = 0
                for (g0, gsz) in grps:
                    r = 1 if g0 >= L else 0
                    ntl = gsz // 128
                    P.dma("sp", xg[:, :, 0:gsz], XTv[:, :, g0:g0 + gsz], r=["XT"], w=["xg"])
                    for tt in range(ntl):
                        P.dma("sp", mt[:, tt, :], MOE[g0 + tt * 128:g0 + (tt + 1) * 128, :], r=["MOE"], w=[f"mt{tt}"])
                    for c in range(16):
                        b = bi % 6
                        bi += 1
                        for tt in range(ntl):
                            P.op("pe", lambda e: e.transpose(bank(b)[:, tt * 128:(tt + 1) * 128], mt[:, tt, c * 128:(c + 1) * 128], identF),
                                 r=[f"mt{tt}", "identF"], w=[bkey(b)])
                        P.op("dve", lambda e: e.scalar_tensor_tensor(xg[:, c, 0:gsz], bank(b)[:, 0:gsz], ada_vec(l, 5, r)[:, c:c + 1],
                                                                     xg[:, c, 0:gsz], ALU.mult, ALU.add),
                             r=[bkey(b), "xg", "ada_sb"], w=["xg"])
                    P.dma("sp", XTv[:, :, g0:g0 + gsz], xg[:, :, 0:gsz], r=["xg"], w=["XT"])
                P.barrier()

    def phase_final():
        with ExitStack() as es:
            def sb(name, shape, dt=F32):
                return es.enter_context(nc.sbuf_tensor(U(name), shape, dt)).ap()
            xg = sb("xg", [128, 16, 512]); xn = sb("xn", [128, 16, 512]); nb = alloc_norm_bufs(es)
            ost = sb("ost", [128, 2, 2048])
            oi = 0
            bi = 0
            for (g0, gsz) in GROUPS[:4]:
                P.dma("sp", xg, XTv[:, :, g0:g0 + gsz], r=["XT"], w=["xg"])

                def out_fn(c, tc, key):
                    copy_op("act" if c % 2 else "pool", xn[:, c, :], tc, r=[key], w=["xn"])
                norm_group(nb, xg, gsz, nfin_sb, None, out_fn)
                for tt in range(4):
                    os_ = oi % 2
                    oi += 1
                    for cb in range(4):
                        b = bi % 6
                        bi += 1
                        for cc in range(4):
                            c = cb * 4 + cc
                            P.op("pe", lambda e: e.transpose(bank(b)[:, cc * 128:(cc + 1) * 128], xn[:, c, tt * 128:(tt + 1) * 128], identF),
                                 r=["xn", "identF"], w=[bkey(b)])
                        copy_op(evac_eng(), ost[:, os_, cb * 512:(cb + 1) * 512], bank(b), r=[bkey(b)], w=[f"ost{os_}"])
                    P.dma("sp", y_out[g0 + tt * 128:g0 + (tt + 1) * 128, :], ost[:, os_, :], r=[f"ost{os_}"], w=["y"])
            P.barrier()

    if not cfg.get("skip_ada"):
        phase_ada()
    only_mix = cfg.get("only_mix")
    for l in range(nlayers):
        if not cfg.get("skip_inproj"):
            with ExitStack() as esl:
                hT = esl.enter_context(nc.sbuf_tensor(U("hT"), [128, 16, NT], BF16)).ap()
                phase_norm1(l, hT, esl)
                if stop_after == "norm1":
                    break
                phase_inproj(l, hT)
        if stop_after == "inproj":
            break
        if only_mix is None or "ret" in only_mix:
            phase_ret(l)
        if only_mix is None or "conv" in only_mix:
            phase_conv(l)
        if only_mix is None or "swa" in only_mix:
            phase_swa(l)
        if only_mix is None or "na" in only_mix:
            phase_na(l)
        if stop_after == "mix":
            break
        if not cfg.get("skip_outproj"):
            phase_outproj(l)
        if stop_after == "outproj":
            break
        phase_ffn(l)
        if stop_after == "ffn":
            break
    else:
        phase_final()

    P.barrier()
    print(f"[build] ops={P.nops} waits={P.nwaits}")
    return nc


def make_in_maps(inputs):
    hc = _host_consts()
    f = lambda a: np.ascontiguousarray(np.asarray(a, dtype=np.float32))
    x = f(inputs["x"]); c = f(inputs["c"]); ctx = f(inputs["ctx"]); c_ctx = f(inputs["c_ctx"])
    shared = {
        "w_ada": f(inputs["w_ada"]),
        "bada_t": f(np.asarray(inputs["b_ada"]).reshape(DEPTH, 96, 128).transpose(0, 2, 1)),
        "nmix_t": f(np.asarray(inputs["norm_mix"]).reshape(DEPTH, 16, 128).transpose(0, 2, 1)),
        "nffn_t": f(np.asarray(inputs["norm_ffn"]).reshape(DEPTH, 16, 128).transpose(0, 2, 1)),
        "nfin_t": f(np.asarray(inputs["norm_final"]).reshape(16, 128).T),
        "w_in": f(inputs["w_in"]), "w_out": f(inputs["w_out"]),
        "decA": f(np.concatenate([inputs["ret_decay_fwd"], inputs["ret_decay_bwd"]], axis=1)),
        "decB": f(np.stack([np.concatenate([np.asarray(inputs["ret_decay_fwd"])[:, hf::2],
                                            np.asarray(inputs["ret_decay_bwd"])[:, hf::2]], axis=1) for hf in range(2)], axis=1)),
        "convw_t": f(np.asarray(inputs["conv_w"]).reshape(DEPTH, 3, 4, 128).transpose(0, 3, 2, 1)),
        "sink": f(inputs["swa_sink"]),
        "na_bias": _na_bias_layout(np.asarray(inputs["na_rpb"], dtype=np.float32)),
        "w_router": f(inputs["w_router"]),
        "w_gate": f(inputs["w_gate"]), "w_up": f(inputs["w_up"]), "w_down": f(inputs["w_down"]),
    }
    shared.update(hc)
    maps = []
    for b in range(8):
        m = dict(shared)
        m["x"] = x[b]
        m["ctx"] = ctx[b]
        m["c_t"] = f(np.stack([c[b].reshape(16, 128).T, c_ctx.reshape(16, 128).T], axis=-1))
        maps.append(m)
    return maps


def kernel(**inputs):
    nc = build_program()
    maps = make_in_maps(inputs)
    res = run_bass_kernel_spmd(nc, maps, core_ids=list(range(8)))
    return np.stack([np.asarray(r["y"], dtype=np.float32) for r in res.results], axis=0)
```

```python
from contextlib import ExitStack
import numpy as np
import concourse.bass as bass
import concourse.mybir as mybir
from concourse.bass_utils import run_bass_kernel_spmd

F32 = mybir.dt.float32
BF16 = mybir.dt.bfloat16
U32 = mybir.dt.uint32
ALU = mybir.AluOpType
AF = mybir.ActivationFunctionType
AX = mybir.AxisListType

D = 2048
L = 2048
T = 256
NT = L + T
DEPTH = 2
NE = 16
FF = 1024
CAP = 256
CAPC = 32
NSL = CAP + CAPC
IN_COLS = 5888
EPS = 1e-6
GROUPS = [(0, 512), (512, 512), (1024, 512), (1536, 512), (2048, 256)]
NEG = -30000.0

ZT_RQ, ZT_RK, ZT_RV, ZT_RG, ZT_SQ, ZT_SK, ZT_SV, ZT_NV = 0, 512, 1024, 1536, 2048, 2560, 2688, 2816
ZT_COLS = 3328
ZF_CB, ZF_CC, ZF_CH, ZF_NQ, ZF_NK = 0, 512, 1024, 1536, 2048
ZF_ROWS = 2560

N_DMA_SEMS = 24
N_SW_SEMS = 8


class Prog:
    def __init__(self, nc):
        self.nc = nc
        self.eng = {"pe": nc.tensor, "act": nc.scalar, "dve": nc.vector,
                    "pool": nc.gpsimd, "sp": nc.sync}
        self.sem = {k: nc.alloc_semaphore(name=f"s_{k}") for k in self.eng}
        self.cnt = {k: 0 for k in self.eng}
        self.dma_sems = [nc.alloc_semaphore(name=f"s_dma{i}") for i in range(N_DMA_SEMS)]
        self.dma_cnt = [0] * N_DMA_SEMS
        self.dma_rr = 0
        self.known = {k: {} for k in self.eng}
        self.res = {}
        self.nwaits = 0
        self.nops = 0
        self.sw_sems = [nc.alloc_semaphore(name=f"s_sw{i}") for i in range(N_SW_SEMS)]
        self.sw_cnt = [0] * N_SW_SEMS
        self.sw_rr = 0

    def _semh(self, key):
        if key[0] == "e":
            return self.sem[key[1]]
        if key[0] == "s":
            return self.sw_sems[key[1]]
        return self.dma_sems[key[1]]

    def dma_gather(self, out, in_, idx_ap, element_offset=0, r=(), w=()):
        q = "pool"
        for k, v in self._deps(q, r, w).items():
            self._wait(q, (k, v))
        i = self.sw_rr
        self.sw_rr = (self.sw_rr + 1) % N_SW_SEMS
        if self.sw_cnt[i] > 0:
            self._wait(q, (("s", i), 16 * self.sw_cnt[i]))
        ins = self.eng[q].indirect_dma_start(out, None, in_, bass.IndirectOffsetOnAxis(idx_ap, 0),
                                             element_offset=element_offset)
        self.sw_cnt[i] += 1
        ins.then_inc(self.sw_sems[i], 16)
        tok = (("s", i), 16 * self.sw_cnt[i])
        self._record(tok, r, w)
        self.nops += 1
        return tok

    def dma_scatter_add(self, out, idx_ap, in_, element_offset=0, r=(), w=()):
        q = "pool"
        for k, v in self._deps(q, r, w).items():
            self._wait(q, (k, v))
        i = self.sw_rr
        self.sw_rr = (self.sw_rr + 1) % N_SW_SEMS
        if self.sw_cnt[i] > 0:
            self._wait(q, (("s", i), 16 * self.sw_cnt[i]))
        ins = self.eng[q].indirect_dma_start(out, bass.IndirectOffsetOnAxis(idx_ap, 0), in_, None,
                                             element_offset=element_offset, compute_op=ALU.add)
        self.sw_cnt[i] += 1
        ins.then_inc(self.sw_sems[i], 16)
        tok = (("s", i), 16 * self.sw_cnt[i])
        self._record(tok, r, w)
        self.nops += 1
        return tok

    def dma_sw(self, slot, out, in_, r=(), w=(), **kw):
        q = "pool"
        for k, v in self._deps(q, r, w).items():
            self._wait(q, (k, v))
        i = self.sw_rr
        self.sw_rr = (self.sw_rr + 1) % N_SW_SEMS
        if self.sw_cnt[i] > 0:
            self._wait(q, (("s", i), 16 * self.sw_cnt[i]))
        ins = self.eng[q].dma_start(out=out, in_=in_, **kw)
        self.sw_cnt[i] += 1
        ins.then_inc(self.sw_sems[i], 16)
        tok = (("s", i), 16 * self.sw_cnt[i])
        self._record(tok, r, w)
        self.nops += 1
        return tok

    def _wait(self, e, tok):
        key, val = tok
        if self.known[e].get(key, 0) >= val:
            return
        self.eng[e].wait_ge(self._semh(key), val)
        self.known[e][key] = val
        self.nwaits += 1

    def _deps(self, e, r, w):
        deps = {}

        def add(tok):
            if tok is None:
                return
            k, v = tok
            if k == ("e", "pe") and e == "pe":
                return
            if deps.get(k, 0) < v:
                deps[k] = v
        for k in r:
            st = self.res.get(k)
            if st:
                add(st[0])
        for k in w:
            st = self.res.get(k)
            if st:
                add(st[0])
                for t in st[1]:
                    add(t)
        return deps

    def _record(self, tok, r, w):
        for k in w:
            self.res[k] = [tok, []]
        for k in r:
            st = self.res.setdefault(k, [None, []])
            lst = st[1]
            for i, (kk, vv) in enumerate(lst):
                if kk == tok[0]:
                    if vv < tok[1]:
                        lst[i] = tok
                    break
            else:
                lst.append(tok)

    def op(self, e, fn, r=(), w=()):
        psr = [k for k in r if k.startswith("ps")]
        if psr:
            r = [k for k in r if not k.startswith("ps")]
            w = list(w) + psr
        for k, v in self._deps(e, r, w).items():
            self._wait(e, (k, v))
        ins = fn(self.eng[e])
        self.cnt[e] += 1
        ins.then_inc(self.sem[e], 1)
        tok = (("e", e), self.cnt[e])
        self._record(tok, r, w)
        self.nops += 1
        return tok

    def dma(self, q, out, in_, r=(), w=(), **kw):
        for k, v in self._deps(q, r, w).items():
            self._wait(q, (k, v))
        i = self.dma_rr
        self.dma_rr = (self.dma_rr + 1) % N_DMA_SEMS
        if self.dma_cnt[i] > 0:
            self._wait(q, (("d", i), 16 * self.dma_cnt[i]))
        ins = self.eng[q].dma_start(out=out, in_=in_, **kw)
        self.dma_cnt[i] += 1
        ins.then_inc(self.dma_sems[i], 16)
        tok = (("d", i), 16 * self.dma_cnt[i])
        self._record(tok, r, w)
        self.nops += 1
        return tok

    def _bump(self, q):
        if self.cnt[q] > 0:
            self._wait(q, (("e", q), self.cnt[q]))
        ins = self.eng[q].nop()
        self.cnt[q] += 1
        ins.then_inc(self.sem[q], 1)

    def _all_wait_all(self):
        for e in self.eng:
            for e2 in self.eng:
                if self.cnt[e2] > 0:
                    self._wait(e, (("e", e2), self.cnt[e2]))

    def barrier(self):
        for i in range(N_DMA_SEMS):
            if self.dma_cnt[i] > 0:
                self._wait("sp", (("d", i), 16 * self.dma_cnt[i]))
        for i in range(N_SW_SEMS):
            if self.sw_cnt[i] > 0:
                self._wait("pool", (("s", i), 16 * self.sw_cnt[i]))
        self._bump("sp")
        self._bump("pool")
        self._all_wait_all()
        self.res = {}


def _host_consts():
    c = {}
    t = np.arange(L)
    row = (t // 64).astype(np.float32)
    col = (t % 64).astype(np.float32)
    inv = (10000.0 ** (-np.arange(16, dtype=np.float32) / 16)).astype(np.float32)
    ang = np.concatenate([row[:, None] * inv, col[:, None] * inv], axis=-1).astype(np.float32)
    cos, sin = np.cos(ang).astype(np.float32), np.sin(ang).astype(np.float32)
    CC = np.concatenate([cos, cos], -1)
    SS = np.concatenate([-sin, sin], -1)
    tab = np.stack([CC, SS, 0.125 * CC, 0.125 * SS], 1)
    c["cs_tab"] = np.ascontiguousarray(tab.reshape(16, 128, 4, 64).transpose(1, 0, 2, 3)).astype(np.float32)
    k = np.arange(128)[:, None].astype(np.float32)
    q = np.arange(128)[None, :].astype(np.float32)
    retc = np.stack([np.maximum(q - k, 0), (q >= k).astype(np.float32),
                     np.maximum(k - q, 0), (k > q).astype(np.float32)], 1)
    c["retc"] = np.ascontiguousarray(retc).astype(np.float32)
    p = np.arange(128, dtype=np.float32)
    c["posv"] = np.stack([p + 1, 127 - p, 128 - p, p], 1).astype(np.float32)
    qr = np.stack([np.arange(128) + 1.0, 128.0 - np.arange(128)], 0)
    c["qrow"] = np.ascontiguousarray(np.broadcast_to(qr[None], (128, 2, 128))).astype(np.float32)
    c["swamask"] = np.ascontiguousarray(np.stack([(k >= q), (k <= q)], 1)).astype(np.float32)
    return c


NA_CLS_TILES = {0: [0, 1, 2, 3], 1: [0, 1, 2, 3], 2: None, 3: [12, 13, 14, 15], 4: [12, 13, 14, 15]}
NA_CLS_REP = {0: 0, 1: 1, 2: 2, 3: 14, 4: 15}


def na_cls(m):
    if m <= 1:
        return m
    if m >= 14:
        return m - 11
    return 2


def na_tiles(m):
    cl = na_cls(m)
    if cl == 2:
        return [m - 2, m - 1, m, m + 1, m + 2]
    return NA_CLS_TILES[cl]


def _na_bias_layout(rpb):
    out = np.full((DEPTH, 8, 128, 5, 5, 128), NEG, np.float32)
    a = np.arange(128) // 64
    cc = np.arange(128) % 64
    for cl in range(5):
        m = NA_CLS_REP[cl]
        tiles = na_tiles(m)
        qr = 2 * m + a
        qc = cc
        bs = np.clip(qr - 4, 0, 24)
        cs = np.clip(qc - 8, 0, 48)
        for j, kt in enumerate(tiles):
            kr = 2 * kt + a
            kc = cc
            inband = (kr[:, None] >= bs[None, :]) & (kr[:, None] < bs[None, :] + 8)
            colok = (kc[:, None] >= cs[None, :]) & (kc[:, None] < cs[None, :] + 16)
            dr = np.clip(kr[:, None] - qr[None, :] + 7, 0, 14)
            dc = np.clip(kc[:, None] - qc[None, :], -15, 15) + 15
            g = rpb[:, :, dr, dc]
            out[:, :, :, cl, j, :] = np.where((inband & colok)[None, None], g, np.float32(NEG))
    return out.reshape(DEPTH, 8, 128, 25 * 128)


def build_program(cfg=None):
    cfg = cfg or {}
    _uid = [0]

    def U(n):
        _uid[0] += 1
        return f"{n}_{_uid[0]}"
    dbg = cfg.get("debug", False)
    stop_after = cfg.get("stop_after", None)
    nlayers = cfg.get("nlayers", DEPTH)
    nc = bass.Bass("TRN2", target_bir_lowering=False)
    P = Prog(nc)

    def din(name, shape, dt=F32):
        if name in cfg.get("shrink", ()):
            shape = [1] * len(shape)
        return nc.dram_tensor(name, list(shape), dt, kind="ExternalInput").ap()

    def dscr(name, shape, dt, out=False):
        return nc.dram_tensor(name, list(shape), dt,
                              kind="ExternalOutput" if (out and dbg) else "Internal").ap()

    x_in = din("x", [L, D]); ctx_in = din("ctx", [T, D]); c_t = din("c_t", [128, 16, 2])
    w_ada = din("w_ada", [DEPTH, D, 6 * D]); bada_t = din("bada_t", [DEPTH, 128, 96])
    nmix_t = din("nmix_t", [DEPTH, 128, 16]); nffn_t = din("nffn_t", [DEPTH, 128, 16]); nfin_t = din("nfin_t", [128, 16])
    w_in = din("w_in", [DEPTH, D, IN_COLS]); w_out = din("w_out", [DEPTH, D, D])
    decA = din("decA", [DEPTH, 16]); decB = din("decB", [DEPTH, 2, 8])
    convw_t = din("convw_t", [DEPTH, 128, 4, 3]); sink_in = din("sink", [DEPTH, 8])
    na_bias = din("na_bias", [DEPTH, 8, 128, 3200])
    w_router = din("w_router", [DEPTH, D, NE])
    w_gate = din("w_gate", [DEPTH, NE, D, FF]); w_up = din("w_up", [DEPTH, NE, D, FF]); w_down = din("w_down", [DEPTH, NE, FF, D])
    cs_tab = din("cs_tab", [128, 16, 4, 64]); retc_in = din("retc", [128, 4, 128]); posv_in = din("posv", [128, 4])
    qrow_in = din("qrow", [128, 2, 128]); swamask_in = din("swamask", [128, 2, 128])
    y_out = nc.dram_tensor("y", [L, D], F32, kind="ExternalOutput").ap()

    XT = dscr("XT", [D, NT], F32, out=True)
    ZT = dscr("ZT", [NT, ZT_COLS], BF16, out=True)
    ZF = dscr("ZF", [ZF_ROWS, NT], BF16, out=True)
    YT = dscr("YT", [D, NT], BF16, out=True)
    YE = dscr("YE", [NE, NSL, D], BF16, out=True)
    NPC = cfg.get("npc", 5)
    WBG = dscr("WBG", [max(NPC, 1), D, FF], BF16)
    WBU = dscr("WBU", [max(NPC, 1), D, FF], BF16)
    WBD = dscr("WBD", [max(NPC, 1), FF, D], BF16)
    pc_jobs = []
    pc_n = [0]

    def precast_prepare(l):
        del pc_jobs[:]
        for ex in range(NPC):
            for q in range(4):
                pc_jobs.append((WBG[ex, q * 512:(q + 1) * 512, :], w_gate[l, ex, q * 512:(q + 1) * 512, :]))
                pc_jobs.append((WBU[ex, q * 512:(q + 1) * 512, :], w_up[l, ex, q * 512:(q + 1) * 512, :]))
                pc_jobs.append((WBD[ex, q * 256:(q + 1) * 256, :], w_down[l, ex, q * 256:(q + 1) * 256, :]))

    def precast_step():
        if pc_jobs:
            dst, src = pc_jobs.pop(0)
            pc_n[0] += 1
            P.dma_sw(0, dst, src, w=[f"WB{pc_n[0]}"])

    H2D = dscr("H2D", [NT, D], BF16)
    MOE = dscr("MOE", [NT, D], F32)
    ADAD = dscr("ADAD", [DEPTH, 128, 96, 2], F32, out=True)
    IDXC = dscr("IDXC", [CAPC, NE], F32)
    DBG1 = dscr("DBG1", [NE, 2 * CAP], F32, out=True)
    XTv = XT.rearrange("(c p) n -> p c n", p=128)
    YTv = YT.rearrange("(c p) n -> p c n", p=128)

    PS = nc.alloc_psum_tensor("ps", [128, 8, 512], F32).ap()

    def bank(i):
        return PS[:, i, :]

    def bkey(i):
        return f"ps{i}"

    identF = nc.alloc_sbuf_tensor("identF", [128, 128], F32).ap()
    identB = nc.alloc_sbuf_tensor("identB", [128, 128], BF16).ap()
    onesF = nc.alloc_sbuf_tensor("onesF", [128, 128], F32).ap()
    iotaF = nc.alloc_sbuf_tensor("iotaF", [128, 2048], F32).ap()
    piota = nc.alloc_sbuf_tensor("piota", [128, 16], F32).ap()
    ada_sb = nc.alloc_sbuf_tensor("ada_sb", [128, DEPTH, 96, 2], F32).ap()
    scl = nc.alloc_sbuf_tensor("scl", [128, DEPTH, 2, 16, 2], F32).ap()
    nrm_sb = nc.alloc_sbuf_tensor("nrm_sb", [128, DEPTH, 2, 16], F32).ap()
    nfin_sb = nc.alloc_sbuf_tensor("nfin_sb", [128, 16], F32).ap()
    tmpi = nc.alloc_sbuf_tensor("tmpi", [128, 128], F32).ap()

    P.op("pool", lambda e: e.iota(tmpi, pattern=[[1, 128]], base=0, channel_multiplier=-1,
                                  allow_small_or_imprecise_dtypes=True), w=["tmpi"])
    P.op("dve", lambda e: e.tensor_scalar(identF, tmpi, 0.0, None, ALU.is_equal), r=["tmpi"], w=["identF"])
    P.op("dve", lambda e: e.tensor_copy(identB, identF), r=["identF"], w=["identB"])
    P.op("dve", lambda e: e.memset(onesF, 1.0), w=["onesF"])
    P.op("pool", lambda e: e.iota(iotaF, pattern=[[1, 2048]], base=0, channel_multiplier=0,
                                  allow_small_or_imprecise_dtypes=True), w=["iotaF"])
    P.op("pool", lambda e: e.iota(piota, pattern=[[128, 16]], base=0, channel_multiplier=1,
                                  allow_small_or_imprecise_dtypes=True), w=["piota"])
    P.dma("sp", nrm_sb[:, :, 0, :], nmix_t.rearrange("l p c -> p l c"), w=["nrm_sb"])
    P.dma("sp", nrm_sb[:, :, 1, :], nffn_t.rearrange("l p c -> p l c"), w=["nrm_sb"])
    P.dma("sp", nfin_sb, nfin_t, w=["nfin_sb"])

    NW = 4
    evac_rr = [0]

    def evac_eng():
        evac_rr[0] ^= 1
        return "act" if evac_rr[0] else "dve"

    def copy_op(eng, out, in_, r, w, scale=None):
        if eng == "act":
            if scale is None:
                P.op("act", lambda e: e.activation(out, in_, AF.Copy), r=r, w=w)
            else:
                P.op("act", lambda e: e.activation(out, in_, AF.Copy, scale=scale), r=r, w=w)
        else:
            if scale is None:
                P.op(eng, lambda e: e.tensor_copy(out, in_), r=r, w=w)
            else:
                P.op(eng, lambda e: e.tensor_scalar(out, in_, scale, None, ALU.mult), r=r, w=w)

    class Ring:
        def __init__(self, es, nslots=NW):
            self.n = nslots
            self.t = es.enter_context(nc.sbuf_tensor(U("wring"), [128, nslots, 8192], BF16)).ap()
            self.i = 0
            self.tag = U("w")

        def load(self, src_ap, view):
            s = self.i
            self.i = (self.i + 1) % self.n
            dst = view(self.t[:, s, :])
            key = f"{self.tag}_{s}"
            P.dma_sw(s, dst, src_ap, w=[key])
            return dst, key

    sc_t = nc.alloc_sbuf_tensor("sc_t", [128, 16, 2], BF16).ap()
    cin_t = nc.alloc_sbuf_tensor("cin_t", [128, 16, 2], F32).ap()
    bada_sb = nc.alloc_sbuf_tensor("bada_sb", [128, DEPTH, 96], F32).ap()
    ada_bi = [0]

    def ada_setup():
        P.dma("sp", cin_t, c_t, w=["cin"])
        P.dma("sp", bada_sb, bada_t.rearrange("l p c -> p l c"), w=["bada"])
        P.op("act", lambda e: e.activation(sc_t, cin_t, AF.Silu), r=["cin"], w=["sc"])

    def ada_load(l, fg, ring):
        return ring.load(w_ada[l, :, fg * 512:(fg + 1) * 512].rearrange("(c p) n -> p c n", p=128),
                         lambda t: t.rearrange("p (c n) -> p c n", c=16))

    def ada_tile(l, fg, ring, banks, loaded=None):
        wt, wk = loaded if loaded is not None else ada_load(l, fg, ring)
        bi = banks[ada_bi[0] % len(banks)]
        ada_bi[0] += 1
        for m in range(4):
            for c in range(16):
                P.op("pe", lambda e: e.matmul(bank(bi)[:, m * 2:m * 2 + 2], wt[:, c, m * 128:(m + 1) * 128],
                                              sc_t[:, c, :], start=(c == 0), stop=(c == 15)),
                     r=[wk, "sc"], w=[bkey(bi)])
        P.op("dve", lambda e: e.tensor_tensor(
            ada_sb[:, l, fg * 4:(fg + 1) * 4, :],
            bank(bi)[:, 0:8].rearrange("p (m r) -> p m r", r=2),
            bada_sb[:, l, fg * 4:(fg + 1) * 4].unsqueeze(2).to_broadcast([128, 4, 2]), ALU.add),
            r=[bkey(bi), "bada"], w=["ada_sb"])

    def ada_scl(l, wi):
        c0 = 16 if wi == 0 else 64
        P.op("dve", lambda e: e.scalar_tensor_tensor(
            scl[:, l, wi, :, :], ada_sb[:, l, c0:c0 + 16, :], 1.0,
            nrm_sb[:, l, wi, :].unsqueeze(2).to_broadcast([128, 16, 2]), ALU.add, ALU.mult),
            r=["ada_sb", "nrm_sb"], w=["scl"])

    ada_pending = []

    def phase_ada():
        with ExitStack() as es:
            ring = Ring(es)
            ada_setup()
            bg = cfg.get("ada_bg", True)
            for l in range(nlayers):
                for fg in range(24):
                    if bg and not (l == 0 and fg < 8):
                        ada_pending.append((l, fg))
                    else:
                        ada_tile(l, fg, ring, (0, 1))
                if not bg:
                    ada_scl(l, 0); ada_scl(l, 1)
            if bg:
                ada_scl(0, 0)
            P.barrier()

    def ada_finish():
        for l in range(nlayers):
            if l > 0:
                ada_scl(l, 0)
            ada_scl(l, 1)
        if dbg:
            P.dma("sp", ADAD.rearrange("l p c r -> p l c r")[:, 0:nlayers], ada_sb[:, 0:nlayers], r=["ada_sb"], w=["ADAD"])

    def ada_vec(l, which, r):
        return ada_sb[:, l, which * 16:(which + 1) * 16, r]

    def norm_group(bufs, xg, gsz, scale_ap, shift_ap, out_fn, ssb=7):
        sq, rstd, tcs = bufs["sq"], bufs["rstd"], bufs["tc"]
        for c in range(16):
            s = c % 3
            P.op("act", lambda e: e.activation(sq[:, s, 0:gsz], xg[:, c, 0:gsz], AF.Square), r=["xg"], w=[f"sq{s}"])
            P.op("pe", lambda e: e.matmul(bank(ssb)[:, 0:gsz], onesF, sq[:, s, 0:gsz], start=(c == 0), stop=(c == 15)),
                 r=[f"sq{s}", "onesF"], w=[bkey(ssb)])
        P.op("act", lambda e: e.activation(rstd[:, 0, 0:gsz], bank(ssb)[:, 0:gsz], AF.Sqrt, bias=bufs["eps"][:, 0:1], scale=1.0 / D),
             r=[bkey(ssb), "eps"], w=["rstd0"])
        P.op("dve", lambda e: e.reciprocal(rstd[:, 1, 0:gsz], rstd[:, 0, 0:gsz]), r=["rstd0"], w=["rstd1"])
        for c in range(16):
            s = c % 3
            P.op("dve", lambda e: e.scalar_tensor_tensor(tcs[:, s, 0:gsz], xg[:, c, 0:gsz], scale_ap[:, c:c + 1],
                                                         rstd[:, 1, 0:gsz], ALU.mult, ALU.mult),
                 r=["xg", "rstd1", "scl", "ada_sb", "nfin_sb"], w=[f"tc{s}"])
            out_fn(c, tcs[:, s, 0:gsz], f"tc{s}")

    def alloc_norm_bufs(es):
        b = {}
        b["sq"] = es.enter_context(nc.sbuf_tensor(U("sq"), [128, 3, 512], F32)).ap()
        b["rstd"] = es.enter_context(nc.sbuf_tensor(U("rstd"), [128, 2, 512], F32)).ap()
        b["tc"] = es.enter_context(nc.sbuf_tensor(U("tcs"), [128, 3, 512], F32)).ap()
        b["eps"] = es.enter_context(nc.sbuf_tensor(U("epsb"), [128, 1], F32)).ap()
        P.op("dve", lambda e: e.memset(b["eps"], EPS), w=["eps"])
        return b

    def phase_norm1(l, hT, es_outer):
        with ExitStack() as es:
            xg = es.enter_context(nc.sbuf_tensor(U("xg"), [128, 16, 512], F32)).ap()
            nb = alloc_norm_bufs(es)
            xin = None
            if l == 0:
                xin = es.enter_context(nc.sbuf_tensor(U("xin"), [128, 2, 2048], F32)).ap()
            for gi, (g0, gsz) in enumerate(GROUPS):
                r = 1 if g0 >= L else 0
                if l == 0:
                    ntile = gsz // 128
                    for tt in range(ntile):
                        n0 = g0 + tt * 128
                        s = tt % 2
                        src = x_in[n0:n0 + 128, :] if n0 < L else ctx_in[n0 - L:n0 - L + 128, :]
                        P.dma("sp", xin[:, s, :], src, w=[f"xin{s}"])
                        for cb in range(4):
                            for cc in range(4):
                                c = cb * 4 + cc
                                P.op("pe", lambda e: e.transpose(bank(cb)[:, cc * 128:(cc + 1) * 128],
                                                                 xin[:, s, c * 128:(c + 1) * 128], identF),
                                     r=[f"xin{s}", "identF"], w=[bkey(cb)])
                            copy_op(evac_eng(), xg[:, cb * 4:(cb + 1) * 4, tt * 128:(tt + 1) * 128],
                                    bank(cb).rearrange("p (c n) -> p c n", c=4), r=[bkey(cb)], w=["xg"])
                    P.dma("sp", XTv[:, :, g0:g0 + gsz], xg[:, :, 0:gsz], r=["xg"], w=["XT"])
                else:
                    P.dma("sp", xg[:, :, 0:gsz], XTv[:, :, g0:g0 + gsz], r=["XT"], w=["xg"])

                def out_fn(c, tc, key, r=r, g0=g0, gsz=gsz):
                    P.op("act", lambda e: e.activation(hT[:, c, g0:g0 + gsz], tc, AF.Identity,
                                                       bias=ada_vec(l, 0, r)[:, c:c + 1], scale=1.0),
                         r=[key, "ada_sb"], w=["hT"])
                norm_group(nb, xg, gsz, scl[:, l, 0, :, r], None, out_fn)
            P.barrier()

    def phase_inproj(l, hT):
        with ExitStack() as es:
            ring = Ring(es)
            cs = es.enter_context(nc.sbuf_tensor(U("cs"), [128, 16, 4, 64], F32)).ap()
            zst = es.enter_context(nc.sbuf_tensor(U("zst"), [128, 3, 512], BF16)).ap()
            rt = es.enter_context(nc.sbuf_tensor(U("rt"), [128, 2, 2, 512], F32)).ap()
            P.dma("sp", cs, cs_tab, w=["cs"])
            zi = [0]
            bi = [0]
            tiles = [
                (0, 512, "T", ZT_RQ, "rope"), (512, 512, "T", ZT_RK, "ropek"),
                (1024, 512, "T", ZT_RV, "copy"), (1536, 512, "T", ZT_RG, "silu"),
                (2048, 512, "F", ZF_CB, "copy"), (2560, 512, "F", ZF_CC, "copy"), (3072, 512, "F", ZF_CH, "copy"),
                (3584, 512, "T", ZT_SQ, "ropeq_swa"), (4096, 256, "T", ZT_SK, "swakv"),
                (4352, 512, "F", ZF_NQ, "copy"), (4864, 512, "F", ZF_NK, "copy"),
                (5376, 512, "T", ZT_NV, "copy"),
            ]
            only = cfg.get("inproj_tiles")
            precast_prepare(l)
            pcc = [0]

            def pc_tick():
                pcc[0] += 1
                if pcc[0] % 3 == 0:
                    precast_step()
            for ti, (c0, ncol, orient, doff, kind) in enumerate(tiles):
                if only is not None and ti not in only:
                    continue
                wt, wk = ring.load(w_in[l, :, c0:c0 + ncol].rearrange("(c p) n -> p c n", p=128),
                                   lambda t: t[:, 0:16 * ncol].rearrange("p (c n) -> p c n", c=16))
                if orient == "T":
                    for tt in range(18):
                        b = bi[0] = (bi[0] + 1) % 6
                        for c in range(16):
                            P.op("pe", lambda e: e.matmul(bank(b)[:, 0:ncol], hT[:, c, tt * 128:(tt + 1) * 128], wt[:, c, :],
                                                          start=(c == 0), stop=(c == 15)),
                                 r=["hT", wk], w=[bkey(b)])
                        zs = zi[0] = (zi[0] + 1) % 3
                        zk = f"zst{zs}"
                        zo = zst[:, zs, 0:ncol]
                        ps = bank(b)[:, 0:ncol]
                        lat = tt < 16

                        def rope(ps_ap, out_ap, nh, tab, perm=False):
                            rs = tt % 2
                            x4 = ps_ap.rearrange("p (h t d) -> p h t d", t=2, d=32)
                            a = rt[:, rs, 0, 0:nh * 64]
                            bb = rt[:, rs, 1, 0:nh * 64]
                            a3 = a.rearrange("p (h d) -> p h d", d=64)
                            b4 = bb.rearrange("p (h t d) -> p h t d", t=2, d=32)
                            ccb = cs[:, tt, tab, :].unsqueeze(1).to_broadcast([128, nh, 64])
                            ssn = cs[:, tt, tab + 1, 0:32].unsqueeze(1).to_broadcast([128, nh, 32])
                            ssp = cs[:, tt, tab + 1, 32:64].unsqueeze(1).to_broadcast([128, nh, 32])
                            P.op("dve", lambda e: e.tensor_tensor(a3, ps_ap.rearrange("p (h d) -> p h d", d=64), ccb, ALU.mult),
                                 r=[bkey(b), "cs"], w=[f"rta{rs}"])
                            P.op("dve", lambda e: e.tensor_tensor(b4[:, :, 0, :], x4[:, :, 1, :], ssn, ALU.mult),
                                 r=[bkey(b), "cs"], w=[f"rtb{rs}"])
                            P.op("dve", lambda e: e.tensor_tensor(b4[:, :, 1, :], x4[:, :, 0, :], ssp, ALU.mult),
                                 r=[bkey(b), "cs"], w=[f"rtb{rs}"])
                            if perm:
                                o = out_ap.rearrange("p (i g d) -> p g i d", g=2, d=64)
                                P.op("dve", lambda e: e.tensor_tensor(o, a.rearrange("p (g i d) -> p g i d", g=2, d=64),
                                                                       bb.rearrange("p (g i d) -> p g i d", g=2, d=64), ALU.add),
                                     r=[f"rta{rs}", f"rtb{rs}"], w=[zk])
                            else:
                                P.op("dve", lambda e: e.tensor_tensor(out_ap, a, bb, ALU.add),
                                     r=[f"rta{rs}", f"rtb{rs}"], w=[zk])

                        if kind == "copy":
                            copy_op(evac_eng(), zo, ps, r=[bkey(b)], w=[zk])
                        elif kind == "silu":
                            P.op("act", lambda e: e.activation(zo, ps, AF.Silu), r=[bkey(b)], w=[zk])
                        elif kind == "rope":
                            if lat:
                                rope(ps, zo, 8, 0)
                            else:
                                copy_op("act", zo, ps, r=[bkey(b)], w=[zk])
                        elif kind == "ropek":
                            if lat:
                                rope(ps, zo, 8, 2)
                            else:
                                copy_op("act", zo, ps, r=[bkey(b)], w=[zk], scale=0.125)
                        elif kind == "ropeq_swa":
                            if lat:
                                rope(ps, zo, 8, 0, perm=True)
                            else:
                                P.op("act", lambda e: e.activation(zo.rearrange("p (i g d) -> p g i d", g=2, d=64),
                                                                   ps.rearrange("p (g i d) -> p g i d", g=2, d=64), AF.Copy),
                                     r=[bkey(b)], w=[zk])
                        elif kind == "swakv":
                            if lat:
                                rope(ps[:, 0:128], zo[:, 0:128], 2, 0)
                            else:
                                copy_op("act", zo[:, 0:128], ps[:, 0:128], r=[bkey(b)], w=[zk])
                            copy_op("dve", zo[:, 128:256], ps[:, 128:256], r=[bkey(b)], w=[zk])
                        P.dma("sp", ZT[tt * 128:(tt + 1) * 128, doff:doff + ncol], zo, r=[zk], w=["ZT"])
                        pc_tick()
                else:
                    for m in range(ncol // 128):
                        for (g0, gsz) in GROUPS:
                            b = bi[0] = (bi[0] + 1) % 6
                            for c in range(16):
                                P.op("pe", lambda e: e.matmul(bank(b)[:, 0:gsz], wt[:, c, m * 128:(m + 1) * 128], hT[:, c, g0:g0 + gsz],
                                                              start=(c == 0), stop=(c == 15)),
                                     r=["hT", wk], w=[bkey(b)])
                            zs = zi[0] = (zi[0] + 1) % 3
                            zk = f"zst{zs}"
                            copy_op(evac_eng(), zst[:, zs, 0:gsz], bank(b)[:, 0:gsz], r=[bkey(b)], w=[zk])
                            P.dma("sp", ZF[doff + m * 128:doff + (m + 1) * 128, g0:g0 + gsz], zst[:, zs, 0:gsz], r=[zk], w=["ZF"])
                            pc_tick()
            while pc_jobs:
                precast_step()
            P.barrier()

    def phase_ret(l):
        need_ctx = l < DEPTH - 1
        with ExitStack() as es:
            def sb(name, shape, dt=F32):
                return es.enter_context(nc.sbuf_tensor(U(name), shape, dt)).ap()
            lgA = sb("lgA", [128, 16]); lgP = sb("lgP", [128, 8]); tA = sb("tA", [128, 16]); tP = sb("tP", [128, 8])
            retc = sb("retc", [128, 4, 128]); posv = sb("posv", [128, 4]); qrow = sb("qrow", [128, 2, 128])
            zeta = sb("zeta", [128, 2, 8]); cdP = sb("cdP", [128, 8]); xiT = sb("xiT", [128, 2, 4, 128])
            dm = sb("dm", [128, 8, 128]); dtmp = sb("dtmp", [128, 2, 128])
            epsb = sb("epsb", [128, 1])
            SfAll = sb("SfAll", [128, 18, 512], BF16)
            stf = sb("stf", [128, 512]); stb = sb("stb", [128, 512]); stb_bf = sb("stb_bf", [128, 512], BF16)
            kvin = sb("kvin", [128, 2, 1024], BF16); zin = sb("zin", [128, 2, 2048], BF16)
            kz = sb("kz", [128, 2, 512], BF16)
            qTe = sb("qTe", [128, 2, 512], BF16); qTo = sb("qTo", [128, 2, 512], BF16); kT = sb("kT", [128, 2, 512], BF16)
            bmask = sb("bmask", [128, 512]); tkv = sb("tkv", [128, 512])
            qxf = sb("qxf", [128, 2, 512], BF16); qxb = sb("qxb", [128, 2, 512], BF16)
            inn = sb("inn", [128, 2, 1024], BF16)
            sqv = sb("sqv", [128, 512]); t1 = sb("t1", [128, 512]); t2 = sb("t2", [128, 512])
            st8 = sb("st8", [128, 8, 8])
            yr = sb("yr", [128, 2, 512], BF16); yst = sb("yst", [128, 2, 4, 512], BF16)
            P.op("dve", lambda e: e.memset(epsb, EPS), w=["epsb"])
            P.dma("sp", retc, retc_in, w=["retc"]); P.dma("sp", posv, posv_in, w=["posv"]); P.dma("sp", qrow, qrow_in, w=["qrow"])
            P.dma("sp", tA, decA[l:l + 1, :].partition_broadcast(128), w=["tA"])
            P.dma("sp", tP[0:64, :], decB[l, 0:1, :].partition_broadcast(64), w=["tP"])
            P.dma("sp", tP[64:128, :], decB[l, 1:2, :].partition_broadcast(64), w=["tP"])
            for (src, dst, k1, k2) in ((tA, lgA, "tA", "lgA"), (tP, lgP, "tP", "lgP")):
                P.op("act", lambda e: e.activation(src, src, AF.Exp, scale=-1.0), r=[k1], w=[k1])
                P.op("dve", lambda e: e.tensor_scalar(src, src, 1.0, None, ALU.add), r=[k1], w=[k1])
                P.op("act", lambda e: e.activation(src, src, AF.Ln), r=[k1], w=[k1])
                P.op("dve", lambda e: e.tensor_scalar(dst, src, -1.0, None, ALU.mult), r=[k1], w=[k2])
            P.op("act", lambda e: e.activation(zeta[:, 0, :], lgA[:, 0:8], AF.Exp, scale=posv[:, 1:2]), r=["lgA", "posv"], w=["zeta"])
            P.op("act", lambda e: e.activation(zeta[:, 1, :], lgA[:, 8:16], AF.Exp, scale=posv[:, 3:4]), r=["lgA", "posv"], w=["zeta"])
            P.op("act", lambda e: e.activation(cdP, lgP, AF.Exp, scale=128.0), r=["lgP"], w=["cdP"])
            for dr in range(2):
                for j in range(4):
                    P.op("act", lambda e: e.activation(xiT[:, dr, j, :], qrow[:, dr, :], AF.Exp, scale=lgP[:, dr * 4 + j:dr * 4 + j + 1]),
                         r=["lgP", "qrow"], w=["xiT"])
            for h in range(8):
                P.op("act", lambda e: e.activation(dtmp[:, 0, :], retc[:, 0, :], AF.Exp, scale=lgA[:, h:h + 1]), r=["lgA", "retc"], w=["dtmp0"])
                P.op("act", lambda e: e.activation(dtmp[:, 1, :], retc[:, 2, :], AF.Exp, scale=lgA[:, 8 + h:9 + h]), r=["lgA", "retc"], w=["dtmp1"])
                P.op("dve", lambda e: e.tensor_tensor(dtmp[:, 0, :], dtmp[:, 0, :], retc[:, 1, :], ALU.mult), r=["dtmp0", "retc"], w=["dtmp0"])
                P.op("dve", lambda e: e.tensor_tensor(dtmp[:, 1, :], dtmp[:, 1, :], retc[:, 3, :], ALU.mult), r=["dtmp1", "retc"], w=["dtmp1"])
                P.op("dve", lambda e: e.tensor_tensor(dm[:, h, :], dtmp[:, 0, :], dtmp[:, 1, :], ALU.add), r=["dtmp0", "dtmp1"], w=["dm"])
            P.op("pool", lambda e: e.memset(qTe, 0.0), w=["qTe0", "qTe1"])
            P.op("pool", lambda e: e.memset(qTo, 0.0), w=["qTo0", "qTo1"])
            bm4 = bmask.rearrange("p (j t d) -> p j t d", t=2, d=64)
            P.op("pool", lambda e: e.memset(bmask, 0.0), w=["bmask"])
            P.op("pool", lambda e: e.memset(bm4[0:64, :, 0, :], 1.0), r=["bmask"], w=["bmask"])
            P.op("pool", lambda e: e.memset(bm4[64:128, :, 1, :], 1.0), r=["bmask"], w=["bmask"])
            P.op("dve", lambda e: e.memset(stf, 0.0), w=["stf"])
            P.op("dve", lambda e: e.memset(stb, 0.0), w=["stb"])
            P.op("dve", lambda e: e.memset(stb_bf, 0.0), w=["stb_bf"])

            def kv_update(i, dr, kin, kk, st, stk, s):
                P.op("dve", lambda e: e.tensor_tensor(kz[:, s, :].rearrange("p (h d) -> p h d", d=64),
                                                      kin[:, 0:512].rearrange("p (h d) -> p h d", d=64),
                                                      zeta[:, dr, :].unsqueeze(2).to_broadcast([128, 8, 64]), ALU.mult),
                     r=[kk, "zeta"], w=[f"kz{s}"])
                for j in range(4):
                    P.op("pe", lambda e: e.matmul(bank(4)[:, j * 128:(j + 1) * 128], kz[:, s, j * 128:(j + 1) * 128],
                                                  kin[:, 512 + j * 128:512 + (j + 1) * 128], start=True, stop=True),
                         r=[f"kz{s}", kk], w=[bkey(4)])
                P.op("dve", lambda e: e.tensor_tensor(st.rearrange("p (j v) -> p j v", v=128), st.rearrange("p (j v) -> p j v", v=128),
                                                      cdP[:, dr * 4:dr * 4 + 4].unsqueeze(2).to_broadcast([128, 4, 128]), ALU.mult),
                     r=[stk, "cdP"], w=[stk])
                P.op("dve", lambda e: e.tensor_tensor(tkv, bank(4), bmask, ALU.mult), r=[bkey(4), "bmask"], w=["tkv"])
                P.op("dve", lambda e: e.tensor_tensor(st, st, tkv, ALU.add), r=[stk, "tkv"], w=[stk])

            cut = cfg.get("ret_cut", 99)
            fwd_order = [16, 17] + list(range(16)) if cut >= 1 else []
            bwd_order = [17, 16] + list(range(15, -1, -1)) if cut >= 2 else []
            for n, i in enumerate(fwd_order):
                s = n % 2
                P.dma("sp", kvin[:, s, :], ZT[i * 128:(i + 1) * 128, ZT_RK:ZT_RK + 1024], r=["ZT"], w=[f"kvin{s}"])
                P.op("act", lambda e: e.activation(SfAll[:, i, :], stf, AF.Copy), r=["stf"], w=["SfAll"])
                if n < len(fwd_order) - 1:
                    kv_update(i, 0, kvin[:, s, :], f"kvin{s}", stf, "stf", s)
            for n, i in enumerate(bwd_order):
                s = n % 2
                zk = f"zin{s}"
                P.dma("sp", zin[:, s, :], ZT[i * 128:(i + 1) * 128, 0:2048], r=["ZT"], w=[zk])
                need_out = (i < 16) or need_ctx
                if cut < 2.05:
                    continue
                if need_out:
                    yb = 3 if s == 0 else 7
                    TPq, TPk = bank(0), bank(6)
                    for j in range(4):
                        P.op("pe", lambda e: e.matmul(TPq[:, j * 128:(j + 1) * 128], zin[:, s, j * 128:(j + 1) * 128], identB, start=True, stop=True),
                             r=[zk, "identB"], w=[bkey(0)])
                        P.op("pe", lambda e: e.matmul(TPk[:, j * 128:(j + 1) * 128], zin[:, s, 512 + j * 128:512 + (j + 1) * 128], identB, start=True, stop=True),
                             r=[zk, "identB"], w=[bkey(6)])
                    if cut < 2.15:
                        continue
                    P.op("act", lambda e: e.activation(qTe[0:64, s, :], TPq[0:64, :], AF.Copy), r=[bkey(0)], w=[f"qTe{s}"])
                    P.op("act", lambda e: e.activation(qTo[64:128, s, :], TPq[64:128, :], AF.Copy), r=[bkey(0)], w=[f"qTo{s}"])
                    P.op("act", lambda e: e.activation(kT[:, s, :], TPk, AF.Copy), r=[bkey(6)], w=[f"kT{s}"])
                    if cut < 2.25:
                        continue
                    rv = cfg.get("ret_var", 0)
                    if rv == 1:
                        P.op("dve", lambda e: e.tensor_tensor(qxf[:, s, :], TPq, bmask, ALU.mult),
                             r=[bkey(0), "bmask"], w=[f"qxf{s}"])
                    elif rv == 2:
                        P.op("dve", lambda e: e.tensor_copy(qxf[:, s, :], TPq), r=[bkey(0)], w=[f"qxf{s}"])
                    elif rv == 3:
                        P.op("dve", lambda e: e.tensor_tensor(qxf[:, s, :], qTe[:, s, :], xiT[:, 0, :, :].rearrange("p j q -> p (j q)"), ALU.mult),
                             r=[f"qTe{s}", "xiT"], w=[f"qxf{s}"])
                    else:
                        P.op("dve", lambda e: e.tensor_tensor(qxf[:, s, :], TPq, xiT[:, 0, :, :].rearrange("p j q -> p (j q)"), ALU.mult),
                             r=[bkey(0), "xiT"], w=[f"qxf{s}"])
                        P.op("dve", lambda e: e.tensor_tensor(qxb[:, s, :], TPq, xiT[:, 1, :, :].rearrange("p j q -> p (j q)"), ALU.mult),
                             r=[bkey(0), "xiT"], w=[f"qxb{s}"])
                    if cut < 3:
                        continue
                    for h in range(8):
                        j, hf = h // 2, h % 2
                        rows = slice(hf * 64, hf * 64 + 64)
                        sb_ = 1 + h // 4
                        qz = qTe if hf == 0 else qTo
                        P.op("pe", lambda e: e.matmul(bank(sb_)[:, (h % 4) * 128:(h % 4 + 1) * 128], kT[:, s, j * 128:(j + 1) * 128],
                                                      qz[:, s, j * 128:(j + 1) * 128], start=True, stop=True),
                             r=[f"kT{s}", f"qTe{s}", f"qTo{s}"], w=[bkey(sb_)])
                    for hb in range(2):
                        P.op("dve", lambda e: e.tensor_tensor(inn[:, s, hb * 512:(hb + 1) * 512], bank(1 + hb),
                                                              dm[:, hb * 4:(hb + 1) * 4, :].rearrange("p h q -> p (h q)"), ALU.mult),
                             r=[bkey(1 + hb), "dm"], w=[f"inn{s}"])
                    if cut < 4:
                        continue
                    for j in range(4):
                        pc = slice(j * 128, (j + 1) * 128)
                        P.op("pe", lambda e: e.matmul(bank(yb)[:, pc], qxf[:, s, pc], SfAll[:, i, pc], start=True, stop=False),
                             r=[f"qxf{s}", "SfAll"], w=[bkey(yb)])
                        P.op("pe", lambda e: e.matmul(bank(yb)[:, pc], qxb[:, s, pc], stb_bf[:, pc], start=False, stop=False),
                             r=[f"qxb{s}", "stb_bf"], w=[bkey(yb)])
                        for hf in range(2):
                            h = 2 * j + hf
                            P.op("pe", lambda e: e.matmul(bank(yb)[:, h * 64:(h + 1) * 64], inn[:, s, h * 128:(h + 1) * 128],
                                                          zin[:, s, 1024 + h * 64:1024 + (h + 1) * 64], start=False, stop=(hf == 1)),
                                 r=[f"inn{s}", zk], w=[bkey(yb)])
                    if cut < 5:
                        continue
                    Yv = bank(yb).rearrange("p (h d) -> p h d", d=64)
                    s1, s2, mean, msq, var, sd, rstd = [st8[:, k, :] for k in range(7)]
                    P.op("dve", lambda e: e.tensor_reduce(s1, Yv, AX.X, ALU.add), r=[bkey(yb)], w=["st_s1"])
                    P.op("act", lambda e: e.activation(sqv, bank(yb), AF.Square), r=[bkey(yb)], w=["sqv"])
                    P.op("dve", lambda e: e.tensor_reduce(s2, sqv.rearrange("p (h d) -> p h d", d=64), AX.X, ALU.add), r=["sqv"], w=["st_s2"])
                    P.op("dve", lambda e: e.tensor_scalar(mean, s1, 1.0 / 64, None, ALU.mult), r=["st_s1"], w=["st_mean"])
                    P.op("dve", lambda e: e.tensor_tensor(msq, mean, mean, ALU.mult), r=["st_mean"], w=["st_msq"])
                    P.op("dve", lambda e: e.scalar_tensor_tensor(var, s2, 1.0 / 64, msq, ALU.mult, ALU.subtract), r=["st_s2", "st_msq"], w=["st_var"])
                    P.op("act", lambda e: e.activation(sd, var, AF.Sqrt, bias=epsb[:, 0:1], scale=1.0), r=["st_var", "epsb"], w=["st_sd"])
                    P.op("dve", lambda e: e.reciprocal(rstd, sd), r=["st_sd"], w=["st_rstd"])
                    t1v = t1.rearrange("p (h d) -> p h d", d=64)
                    t2v = t2.rearrange("p (h d) -> p h d", d=64)
                    P.op("dve", lambda e: e.tensor_tensor(t1v, Yv, mean.unsqueeze(2).to_broadcast([128, 8, 64]), ALU.subtract),
                         r=[bkey(yb), "st_mean"], w=["t1"])
                    P.op("dve", lambda e: e.tensor_tensor(t2v, t1v, rstd.unsqueeze(2).to_broadcast([128, 8, 64]), ALU.mult),
                         r=["t1", "st_rstd"], w=["t2"])
                    P.op("pool", lambda e: e.tensor_tensor(yr[:, s, :], t2, zin[:, s, 1536:2048], ALU.mult), r=["t2", zk], w=[f"yr{s}"])
                    if cut < 6:
                        continue
                    TY = bank(5)
                    for j in range(4):
                        P.op("pe", lambda e: e.matmul(TY[:, j * 128:(j + 1) * 128], yr[:, s, j * 128:(j + 1) * 128], identB, start=True, stop=True),
                             r=[f"yr{s}", "identB"], w=[bkey(5)])
                    if i < 16:
                        grp, pos, gw = i // 4, i % 4, 512
                        g0 = grp * 512
                    else:
                        grp, pos, gw = 4, i - 16, 256
                        g0 = 2048
                    ys = grp % 2
                    P.op("act", lambda e: e.activation(yst[:, ys, :, pos * 128:(pos + 1) * 128], TY[:, 0:512].rearrange("p (j n) -> p j n", j=4), AF.Copy),
                         r=[bkey(5)], w=[f"yst{ys}"])
                    if pos == 0:
                        P.dma("sp", YTv[:, 0:4, g0:g0 + gw], yst[:, ys, :, 0:gw], r=[f"yst{ys}"], w=["YT"])
                if n < len(bwd_order) - 1:
                    kv_update(i, 1, zin[:, s, 512:1536], zk, stb, "stb", s)
                    P.op("act", lambda e: e.activation(stb_bf, stb, AF.Copy), r=["stb"], w=["stb_bf"])
            P.barrier()

    def phase_conv(l):
        need_ctx = l < DEPTH - 1
        with ExitStack() as es:
            def sb(name, shape, dt=F32):
                return es.enter_context(nc.sbuf_tensor(U(name), shape, dt)).ap()
            cw = sb("cw", [128, 4, 3])
            bch = sb("bch", [128, 2, 3, NT], BF16)
            u = sb("u", [128, 2, NT]); yv = sb("yv", [128, 2, NT]); ob = sb("ob", [128, 2, NT], BF16)
            P.dma("sp", cw, convw_t[l], w=["cw"])
            nend = NT if need_ctx else L
            seqs = [(0, L)] + ([(L, NT)] if need_ctx else [])
            for cc in range(4):
                s = cc % 2
                for k3, off in enumerate((ZF_CB, ZF_CC, ZF_CH)):
                    P.dma("sp", bch[:, s, k3, 0:nend], ZF[off + cc * 128:off + (cc + 1) * 128, 0:nend], r=["ZF"], w=[f"bch{s}"])
                P.op("dve", lambda e: e.tensor_tensor(u[:, s, 0:nend], bch[:, s, 1, 0:nend], bch[:, s, 2, 0:nend], ALU.mult), r=[f"bch{s}"], w=[f"u{s}"])
                P.op("act", lambda e: e.activation(yv[:, s, 0:nend], u[:, s, 0:nend], AF.Copy, scale=cw[:, cc, 1:2]), r=[f"u{s}", "cw"], w=[f"yv{s}"])
                for (s0, s1) in seqs:
                    P.op("dve", lambda e: e.scalar_tensor_tensor(yv[:, s, s0 + 1:s1], u[:, s, s0:s1 - 1], cw[:, cc, 0:1], yv[:, s, s0 + 1:s1], ALU.mult, ALU.add),
                         r=[f"u{s}", "cw", f"yv{s}"], w=[f"yv{s}"])
                    P.op("dve", lambda e: e.scalar_tensor_tensor(yv[:, s, s0:s1 - 1], u[:, s, s0 + 1:s1], cw[:, cc, 2:3], yv[:, s, s0:s1 - 1], ALU.mult, ALU.add),
                         r=[f"u{s}", "cw", f"yv{s}"], w=[f"yv{s}"])
                P.op("pool", lambda e: e.tensor_tensor(ob[:, s, 0:nend], yv[:, s, 0:nend], bch[:, s, 0, 0:nend], ALU.mult), r=[f"yv{s}", f"bch{s}"], w=[f"ob{s}"])
                P.dma("sp", YT[512 + cc * 128:512 + (cc + 1) * 128, 0:nend], ob[:, s, 0:nend], r=[f"ob{s}"], w=["YT"])
            P.barrier()

    def phase_swa(l):
        need_ctx = l < DEPTH - 1
        with ExitStack() as es:
            def sb(name, shape, dt=F32):
                return es.enter_context(nc.sbuf_tensor(U(name), shape, dt)).ap()
            esink = sb("esink", [128, 8]); mkf = sb("mkf", [128, 2, 128]); mk = sb("mk", [128, 2, 128], BF16)
            QT0 = sb("QT0", [128, 4, NT], BF16); QT1 = sb("QT1", [128, 4, NT], BF16)
            KT = sb("KT", [128, NT], BF16); Vp = sb("Vp", [128, 18, 2, 65], BF16)
            P.op("pool", lambda e: e.memset(QT0[64:128], 0.0), w=["QT"])
            P.op("pool", lambda e: e.memset(QT1[0:64], 0.0), w=["QT"])
            zin = sb("zin", [128, 2, 768], BF16)
            PT = sb("PT", [128, 2, 5, 512], BF16)
            den = sb("den", [128, 2, 4]); rec = sb("rec", [128, 2, 4])
            ysw = sb("ysw", [128, 2, 512], BF16); yst = sb("yst", [128, 2, 4, 512], BF16)
            P.dma("sp", esink, sink_in[l:l + 1, :].partition_broadcast(128), w=["esink"])
            P.op("act", lambda e: e.activation(esink, esink, AF.Exp), r=["esink"], w=["esink"])
            P.dma("sp", mkf, swamask_in, w=["mkf"])
            P.op("dve", lambda e: e.tensor_copy(mk, mkf), r=["mkf"], w=["mk"])
            P.op("pool", lambda e: e.memset(Vp, 1.0), w=["Vp"])
            for tt in range(18):
                s = tt % 2
                zk = f"zin{s}"
                P.dma("sp", zin[:, s, :], ZT[tt * 128:(tt + 1) * 128, ZT_SQ:ZT_SQ + 768], r=["ZT"], w=[zk])
                tb = s
                TP = bank(tb)
                TPk = bank(2 + s)
                for i4 in range(4):
                    P.op("pe", lambda e: e.matmul(TP[:, i4 * 128:(i4 + 1) * 128], zin[:, s, i4 * 128:(i4 + 1) * 128], identB, start=True, stop=True),
                         r=[zk, "identB"], w=[bkey(tb)])
                P.op("pe", lambda e: e.matmul(TPk[:, 0:128], zin[:, s, 512:640], identB, start=True, stop=True), r=[zk, "identB"], w=[bkey(2 + s)])
                P.op("act", lambda e: e.activation(QT0[0:64, :, tt * 128:(tt + 1) * 128], TP[0:64, 0:512].rearrange("p (i n) -> p i n", i=4), AF.Copy),
                     r=[bkey(tb), "QT"], w=["QT"])
                P.op("act", lambda e: e.activation(QT1[64:128, :, tt * 128:(tt + 1) * 128], TP[64:128, 0:512].rearrange("p (i n) -> p i n", i=4), AF.Copy),
                     r=[bkey(tb), "QT"], w=["QT"])
                P.op("dve", lambda e: e.tensor_copy(KT[:, tt * 128:(tt + 1) * 128], TPk[:, 0:128]), r=[bkey(2 + s)], w=["KT"])
                P.op("pool", lambda e: e.tensor_copy(Vp[:, tt, :, 0:64], zin[:, s, 640:768].rearrange("p (k d) -> p k d", d=64)), r=[zk, "Vp"], w=["Vp"])
            order = ([17, 16] if need_ctx else []) + list(range(15, -1, -1))
            its = [(blk, kv) for blk in order for kv in range(2)]

            def tiles_of(blk):
                if blk < 16:
                    tiles = [(16, None), (17, None)]
                    if blk > 0:
                        tiles.append((blk - 1, 0))
                    tiles.append((blk, None))
                    if blk < 15:
                        tiles.append((blk + 1, 1))
                    return tiles
                return [(16, None), (17, None)]

            def sw_scores(i):
                blk, kv = its[i]
                ps = i % 2
                QZ = QT0 if kv == 0 else QT1
                for jj, (kt, mi) in enumerate(tiles_of(blk)):
                    b = 2 + (jj % 3)
                    P.op("pe", lambda e: e.matmul(bank(b), KT[:, kt * 128:(kt + 1) * 128], QZ[:, :, blk * 128:(blk + 1) * 128],
                                                  start=True, stop=True), r=["KT", "QT"], w=[bkey(b)])
                    P.op("act", lambda e: e.activation(PT[:, ps, jj, :], bank(b), AF.Exp, scale=0.125), r=[bkey(b)], w=[f"PT{ps}_{jj}"])
                    if mi is not None:
                        P.op("pool", lambda e: e.tensor_tensor(PT[:, ps, jj, :].rearrange("p (i q) -> p i q", i=4),
                                                               PT[:, ps, jj, :].rearrange("p (i q) -> p i q", i=4),
                                                               mk[:, mi, :].unsqueeze(1).to_broadcast([128, 4, 128]), ALU.mult),
                             r=[f"PT{ps}_{jj}", "mk"], w=[f"PT{ps}_{jj}"])

            def sw_pv(i):
                blk, kv = its[i]
                ps = i % 2
                tiles = tiles_of(blk)
                ob_ = 5 + ps
                for i4 in range(4):
                    for jj, (kt, mi) in enumerate(tiles):
                        P.op("pe", lambda e: e.matmul(bank(ob_)[:, i4 * 65:(i4 + 1) * 65], PT[:, ps, jj, i4 * 128:(i4 + 1) * 128], Vp[:, kt, kv, :],
                                                      start=(jj == 0), stop=(jj == len(tiles) - 1)),
                             r=[f"PT{ps}_{jj}", "Vp"], w=[bkey(ob_)])

            def sw_norm(i):
                blk, kv = its[i]
                ps = i % 2
                ob_ = 5 + ps
                ysb = (i // 2) % 2
                Ov = bank(ob_)[:, 0:260].rearrange("p (i d) -> p i d", d=65)
                P.op("dve", lambda e: e.tensor_tensor(den[:, ps, :], Ov[:, :, 64], esink[:, kv * 4:(kv + 1) * 4], ALU.add),
                     r=[bkey(ob_), "esink"], w=[f"den{ps}"])
                P.op("dve", lambda e: e.reciprocal(rec[:, ps, :], den[:, ps, :]), r=[f"den{ps}"], w=[f"rec{ps}"])
                P.op("dve", lambda e: e.tensor_tensor(ysw[:, ysb, kv * 256:(kv + 1) * 256].rearrange("p (i d) -> p i d", d=64), Ov[:, :, 0:64],
                                                      rec[:, ps, :].unsqueeze(2).to_broadcast([128, 4, 64]), ALU.mult),
                     r=[bkey(ob_), f"rec{ps}"], w=[f"ysw{ysb}"])
                if kv == 1:
                    TY = bank(7)
                    for j in range(4):
                        P.op("pe", lambda e: e.matmul(TY[:, j * 128:(j + 1) * 128], ysw[:, ysb, j * 128:(j + 1) * 128], identB, start=True, stop=True),
                             r=[f"ysw{ysb}", "identB"], w=[bkey(7)])
                    if blk < 16:
                        grp, pos, gw, g0 = blk // 4, blk % 4, 512, (blk // 4) * 512
                    else:
                        grp, pos, gw, g0 = 4, blk - 16, 256, 2048
                    ys = grp % 2
                    P.op("act", lambda e: e.activation(yst[:, ys, :, pos * 128:(pos + 1) * 128], TY[:, 0:512].rearrange("p (j n) -> p j n", j=4), AF.Copy),
                         r=[bkey(7)], w=[f"yst{ys}"])
                    if pos == 0:
                        P.dma("sp", YTv[:, 8:12, g0:g0 + gw], yst[:, ys, :, 0:gw], r=[f"yst{ys}"], w=["YT"])

            n_it = len(its)
            sw_scores(0)
            for i in range(n_it):
                if i + 1 < n_it:
                    sw_scores(i + 1)
                if i >= 1:
                    sw_norm(i - 1)
                sw_pv(i)
            sw_norm(n_it - 1)
            P.barrier()

    def phase_na(l):
        need_ctx = l < DEPTH - 1
        with ExitStack() as es:
            def sb(name, shape, dt=F32):
                return es.enter_context(nc.sbuf_tensor(U(name), shape, dt)).ap()
            QTe = sb("QTne", [128, 4, NT], BF16); QTo = sb("QTno", [128, 4, NT], BF16); KT = sb("KTn", [128, 4, NT], BF16)
            Vp = sb("Vpn", [128, 18, 8, 65], BF16); vtmp = sb("vtmp", [128, 2, 512], BF16)
            bias = sb("biasn", [128, 2, 25, 128])
            PT = sb("PTn", [128, 2, 7, 128], BF16); tmp = sb("tmpn", [128, 2, 640])
            rec = sb("recn", [128, 2, 1]); yna = sb("yna", [128, 18, 512], BF16)
            yst = sb("ystn", [128, 2, 4, 512], BF16)
            P.op("pool", lambda e: e.memset(QTe[64:128], 0.0), w=["QTe"])
            P.op("pool", lambda e: e.memset(QTo[0:64], 0.0), w=["QTo"])
            zq = ZF[ZF_NQ:ZF_NQ + 512, :].rearrange("(j p) n -> p j n", p=128)
            P.dma("sp", QTe[0:64], zq[0:64], r=["ZF", "QTe"], w=["QTe"])
            P.dma("sp", QTo[64:128], zq[64:128], r=["ZF", "QTo"], w=["QTo"])
            P.dma("sp", KT, ZF[ZF_NK:ZF_NK + 512, :].rearrange("(j p) n -> p j n", p=128), r=["ZF"], w=["KT"])
            P.op("pool", lambda e: e.memset(Vp, 1.0), w=["Vp"])
            for tt in range(18):
                s = tt % 2
                P.dma("sp", vtmp[:, s, :], ZT[tt * 128:(tt + 1) * 128, ZT_NV:ZT_NV + 512], r=["ZT"], w=[f"vtmp{s}"])
                P.op("pool", lambda e: e.tensor_copy(Vp[:, tt, :, 0:64], vtmp[:, s, :].rearrange("p (h d) -> p h d", d=64)),
                     r=[f"vtmp{s}", "Vp"], w=["Vp"])
            nq = 18 if need_ctx else 16
            bgring = None
            if ada_pending:
                bgring = Ring(es, nslots=3)
            iters = [(h, m) for h in range(8) for m in range(nq)]

            def info(k):
                h, m = iters[k]
                lat = na_tiles(m) if m < 16 else []
                return h, m, k % 2, lat, [16, 17] + lat

            def st_scores(k):
                h, m, ps, lat, tiles = info(k)
                j, hf = h // 2, h % 2
                bs = h % 2
                if m == 0:
                    P.dma("sp", bias[:, bs, :, :].rearrange("p a q -> p (a q)"), na_bias[l, h], w=[f"bias{bs}"])
                bA, bB = (0, 1) if ps == 0 else (2, 3)
                QZ = QTe if hf == 0 else QTo
                for jj, kt in enumerate(tiles):
                    b = bA if jj < 4 else bB
                    P.op("pe", lambda e: e.matmul(bank(b)[:, (jj % 4) * 128:(jj % 4 + 1) * 128], KT[:, j, kt * 128:(kt + 1) * 128],
                                                  QZ[:, j, m * 128:(m + 1) * 128], start=True, stop=True),
                         r=["KT", "QTe", "QTo"], w=[bkey(b)])

            def st_soft(k):
                h, m, ps, lat, tiles = info(k)
                bs = h % 2
                nl = len(lat)
                bA, bB = (0, 1) if ps == 0 else (2, 3)
                P.op("act", lambda e: e.activation(PT[:, ps, 0:2, :].rearrange("p a q -> p (a q)"), bank(bA)[:, 0:256], AF.Exp, scale=0.125),
                     r=[bkey(bA)], w=[f"PTa{ps}"])
                if nl:
                    cl = na_cls(m)
                    P.op("dve", lambda e: e.scalar_tensor_tensor(tmp[:, ps, 0:256], bank(bA)[:, 256:512], 0.125,
                                                                 bias[:, bs, cl * 5:cl * 5 + 2, :].rearrange("p a q -> p (a q)"), ALU.mult, ALU.add),
                         r=[bkey(bA), f"bias{bs}"], w=[f"tmp{ps}"])
                    P.op("dve", lambda e: e.scalar_tensor_tensor(tmp[:, ps, 256:nl * 128], bank(bB)[:, 0:(nl - 2) * 128], 0.125,
                                                                 bias[:, bs, cl * 5 + 2:cl * 5 + nl, :].rearrange("p a q -> p (a q)"), ALU.mult, ALU.add),
                         r=[bkey(bB), f"bias{bs}"], w=[f"tmp{ps}"])
                    P.op("act", lambda e: e.activation(PT[:, ps, 2:2 + nl, :].rearrange("p a q -> p (a q)"), tmp[:, ps, 0:nl * 128], AF.Exp),
                         r=[f"tmp{ps}"], w=[f"PTb{ps}"])

            def st_pv(k):
                h, m, ps, lat, tiles = info(k)
                ob_ = 4 + ps
                for jj, kt in enumerate(tiles):
                    P.op("pe", lambda e: e.matmul(bank(ob_)[:, 0:65], PT[:, ps, jj, :], Vp[:, kt, h, :],
                                                  start=(jj == 0), stop=(jj == len(tiles) - 1)),
                         r=[f"PTa{ps}", f"PTb{ps}", "Vp"], w=[bkey(ob_)])

            def st_norm(k):
                h, m, ps, lat, tiles = info(k)
                ob_ = 4 + ps
                P.op("dve", lambda e: e.reciprocal(rec[:, ps, :], bank(ob_)[:, 64:65]), r=[bkey(ob_)], w=[f"rec{ps}"])
                P.op("dve", lambda e: e.tensor_scalar(yna[:, m, h * 64:(h + 1) * 64], bank(ob_)[:, 0:64], rec[:, ps, 0:1], None, ALU.mult),
                     r=[bkey(ob_), f"rec{ps}"], w=["yna"])

            nit = len(iters)
            loaded = []
            had_bg = bool(ada_pending)

            def bg_prefetch():
                if ada_pending and len(loaded) < 2:
                    al, afg = ada_pending.pop(0)
                    loaded.append((al, afg, ada_load(al, afg, bgring)))

            def bg_compute():
                if loaded:
                    al, afg, ld = loaded.pop(0)
                    ada_tile(al, afg, bgring, (6, 7), loaded=ld)
            bg_prefetch()
            bg_prefetch()
            st_scores(0)
            for k in range(nit):
                if k + 1 < nit:
                    st_scores(k + 1)
                st_soft(k)
                if k >= 1:
                    st_norm(k - 1)
                st_pv(k)
                if had_bg and k % 3 == 2:
                    bg_compute()
                    bg_prefetch()
            st_norm(nit - 1)
            while loaded or ada_pending:
                bg_prefetch()
                bg_compute()
            if had_bg:
                ada_finish()
            for m in range(nq):
                tb = 6 + (m % 2)
                TY = bank(tb)
                for jx in range(4):
                    P.op("pe", lambda e: e.matmul(TY[:, jx * 128:(jx + 1) * 128], yna[:, m, jx * 128:(jx + 1) * 128], identB, start=True, stop=True),
                         r=["yna", "identB"], w=[bkey(tb)])
                if m < 16:
                    grp, pos, gw, g0 = m // 4, m % 4, 512, (m // 4) * 512
                    last = pos == 3
                else:
                    grp, pos, gw, g0 = 4, m - 16, 256, 2048
                    last = pos == 1
                ys = grp % 2
                P.op("act", lambda e: e.activation(yst[:, ys, :, pos * 128:(pos + 1) * 128], TY[:, 0:512].rearrange("p (j n) -> p j n", j=4), AF.Copy),
                     r=[bkey(tb)], w=[f"yst{ys}"])
                if last:
                    P.dma("sp", YTv[:, 12:16, g0:g0 + gw], yst[:, ys, :, 0:gw], r=[f"yst{ys}"], w=["YT"])
            P.barrier()

    def phase_outproj(l):
        need_ctx = l < DEPTH - 1
        with ExitStack() as es:
            ring = Ring(es)
            ytsb = es.enter_context(nc.sbuf_tensor(U("ytsb"), [128, 16, NT], BF16)).ap()
            xt = es.enter_context(nc.sbuf_tensor(U("xt"), [128, 3, 512], F32)).ap()
            nend = NT if need_ctx else L
            for c4 in range(4):
                P.dma("sp", ytsb[:, c4 * 4:(c4 + 1) * 4, 0:nend], YTv[:, c4 * 4:(c4 + 1) * 4, 0:nend], r=["YT"], w=["ytsb"])
            groups = GROUPS if need_ctx else GROUPS[:4]
            items = [(mg, mm, g0, gsz) for mg in range(4) for mm in range(4) for (g0, gsz) in groups]

            def xload(i):
                mg, mm, g0, gsz = items[i]
                m = mg * 4 + mm
                xs = i % 3
                P.dma("sp", xt[:, xs, 0:gsz], XT[m * 128:(m + 1) * 128, g0:g0 + gsz], r=["XT"], w=[f"xt{xs}"])
            wt = wk = None
            xload(0)
            for i, (mg, mm, g0, gsz) in enumerate(items):
                if mm == 0 and g0 == 0:
                    wt, wk = ring.load(w_out[l, :, mg * 512:(mg + 1) * 512].rearrange("(c p) n -> p c n", p=128),
                                       lambda t: t.rearrange("p (c n) -> p c n", c=16))
                if i + 1 < len(items):
                    xload(i + 1)
                m = mg * 4 + mm
                r = 1 if g0 >= L else 0
                xs = i % 3
                b = i % 6
                for c in range(16):
                    P.op("pe", lambda e: e.matmul(bank(b)[:, 0:gsz], wt[:, c, mm * 128:(mm + 1) * 128], ytsb[:, c, g0:g0 + gsz],
                                                  start=(c == 0), stop=(c == 15)), r=[wk, "ytsb"], w=[bkey(b)])
                P.op("dve", lambda e: e.scalar_tensor_tensor(xt[:, xs, 0:gsz], bank(b)[:, 0:gsz], ada_vec(l, 2, r)[:, m:m + 1],
                                                             xt[:, xs, 0:gsz], ALU.mult, ALU.add),
                     r=[bkey(b), f"xt{xs}", "ada_sb"], w=[f"xt{xs}"])
                P.dma("sp", XT[m * 128:(m + 1) * 128, g0:g0 + gsz], xt[:, xs, 0:gsz], r=[f"xt{xs}"], w=["XT"])
            P.barrier()

    def phase_ffn(l):
        need_ctx = l < DEPTH - 1
        grps = GROUPS if need_ctx else GROUPS[:4]
        nsl = NSL if need_ctx else CAP
        with ExitStack() as esf:
            def sbf(name, shape, dt=F32):
                return esf.enter_context(nc.sbuf_tensor(U(name), shape, dt)).ap()
            idxf = sbf("idxf", [16, CAP]); vals = sbf("vals", [16, CAP]); idxu = sbf("idxu", [16, CAP], U32)
            idxfc = sbf("idxfc", [16, CAPC]); valsc = sbf("valsc", [16, CAPC]); idxuc = sbf("idxuc", [16, CAPC], U32)
            idxT = sbf("idxT", [128, 16, 2]); gateT = sbf("gateT", [128, 16, 2])
            gateTc = sbf("gateTc", [32, 16]); idxTcu = sbf("idxTcu", [32, 16]); idxTcP = sbf("idxTcP", [128, 4])
            idxTu = sbf("idxTu", [128, 16, 2], U32); idxTcU = sbf("idxTcU", [32, 16], U32)
            with ExitStack() as esh:
                with ExitStack() as es:
                    def sb(name, shape, dt=F32):
                        return es.enter_context(nc.sbuf_tensor(U(name), shape, dt)).ap()
                    xg = sb("xg", [128, 16, 512]); nb = alloc_norm_bufs(es)
                    h32 = sb("h32", [128, 3, 512]); hg = sb("hg", [128, 16, 512], BF16)
                    wr = sb("wr", [128, 16, 16]); E_sb = sb("E_sb", [16, NT]); aff = sb("aff", [16, NT]); rs = sb("rs", [16, 512])
                    h2st = sb("h2st", [128, 2, 2048], BF16)
                    P.dma("sp", wr, w_router[l].rearrange("(c p) e -> p c e", p=128), w=["wr"])
                    zt = sb("zt", [128, 2048])
                    P.op("pool", lambda e: e.memset(zt, 0.0), w=["zt"])
                    for tz in range(18 if need_ctx else 16):
                        P.dma("sp", MOE[tz * 128:(tz + 1) * 128, :], zt, r=["zt"], w=["MOE"])
                    bi = 0
                    for (g0, gsz) in grps:
                        r = 1 if g0 >= L else 0
                        P.dma("sp", xg[:, :, 0:gsz], XTv[:, :, g0:g0 + gsz], r=["XT"], w=["xg"])

                        def out_fn(c, tc, key, r=r, gsz=gsz):
                            s = c % 3
                            P.op("act", lambda e: e.activation(h32[:, s, 0:gsz], tc, AF.Identity, bias=ada_vec(l, 3, r)[:, c:c + 1], scale=1.0),
                                 r=[key, "ada_sb"], w=[f"h32_{s}"])
                            P.op("pe", lambda e: e.matmul(bank(6)[0:16, 0:gsz], wr[:, c, :], h32[:, s, 0:gsz], start=(c == 0), stop=(c == 15)),
                                 r=["wr", f"h32_{s}"], w=[bkey(6)])
                            P.op("pool", lambda e: e.tensor_copy(hg[:, c, 0:gsz], h32[:, s, 0:gsz]), r=[f"h32_{s}"], w=["hg"])
                        norm_group(nb, xg, gsz, scl[:, l, 1, :, r], None, out_fn)
                        P.op("act", lambda e: e.activation(E_sb[:, g0:g0 + gsz], bank(6)[0:16, 0:gsz], AF.Exp), r=[bkey(6)], w=["E_sb"])
                        for tt in range(gsz // 128):
                            tile = (g0 // 128) + tt
                            for cb in range(4):
                                b = bi % 6
                                bi += 1
                                for cc in range(4):
                                    c = cb * 4 + cc
                                    P.op("pe", lambda e: e.matmul(bank(b)[:, cc * 128:(cc + 1) * 128], hg[:, c, tt * 128:(tt + 1) * 128], identB,
                                                                  start=True, stop=True), r=["hg", "identB"], w=[bkey(b)])
                                copy_op(evac_eng(), h2st[:, tile % 2, cb * 512:(cb + 1) * 512], bank(b), r=[bkey(b)], w=[f"h2st{tile % 2}"])
                            P.dma("sp", H2D[tile * 128:(tile + 1) * 128, :], h2st[:, tile % 2, :], r=[f"h2st{tile % 2}"], w=["H2D"])
                    for (g0, gsz) in grps:
                        P.op("pe", lambda e: e.matmul(bank(7)[0:16, 0:gsz], onesF[0:16, 0:16], E_sb[:, g0:g0 + gsz], start=True, stop=True),
                             r=["onesF", "E_sb"], w=[bkey(7)])
                        P.op("dve", lambda e: e.reciprocal(rs[:, 0:gsz], bank(7)[0:16, 0:gsz]), r=[bkey(7)], w=["rs"])
                        P.op("dve", lambda e: e.tensor_tensor(aff[:, g0:g0 + gsz], E_sb[:, g0:g0 + gsz], rs[:, 0:gsz], ALU.mult), r=["E_sb", "rs"], w=["aff"])
                    for (a0, n, k, vv, iu, ifl, kk) in ((0, L, CAP, vals, idxu, idxf, "l"),) + (((L, T, CAPC, valsc, idxuc, idxfc, "c"),) if need_ctx else ()):
                        aw = aff[:, a0:a0 + n]
                        for it in range(k // 8):
                            v8 = vv[:, it * 8:(it + 1) * 8]
                            P.op("dve", lambda e: e.max(v8, aw), r=["aff"], w=["v8" + kk])
                            P.op("dve", lambda e: e.max_index(iu[:, it * 8:(it + 1) * 8], v8, aw), r=["aff", "v8" + kk], w=["iu" + kk])
                            P.op("dve", lambda e: e.match_replace(aw, v8, aw, -1.0), r=["v8" + kk, "aff"], w=["aff"])
                        P.op("dve", lambda e: e.tensor_copy(ifl, iu), r=["iu" + kk], w=["idxf" + kk])
                    for t2 in range(2):
                        P.op("pe", lambda e: e.transpose(bank(0)[:, t2 * 16:(t2 + 1) * 16], idxf[0:16, t2 * 128:(t2 + 1) * 128], identF[0:16, 0:16]),
                             r=["idxfl", "identF"], w=[bkey(0)])
                        P.op("pe", lambda e: e.transpose(bank(1)[:, t2 * 16:(t2 + 1) * 16], vals[0:16, t2 * 128:(t2 + 1) * 128], identF[0:16, 0:16]),
                             r=["v8l", "identF"], w=[bkey(1)])
                    P.op("dve", lambda e: e.tensor_copy(idxT.rearrange("p e t -> p t e"), bank(0)[:, 0:32].rearrange("p (t e) -> p t e", t=2)), r=[bkey(0)], w=["idxT"])
                    P.op("dve", lambda e: e.tensor_copy(gateT.rearrange("p e t -> p t e"), bank(1)[:, 0:32].rearrange("p (t e) -> p t e", t=2)), r=[bkey(1)], w=["gateT"])
                    P.op("dve", lambda e: e.tensor_copy(idxTu, idxT), r=["idxT"], w=["idxTu"])
                    if need_ctx:
                        P.op("pe", lambda e: e.transpose(bank(2)[0:32, 0:16], idxfc[0:16, 0:32], identF[0:16, 0:16]), r=["idxfc", "identF"], w=[bkey(2)])
                        P.op("pe", lambda e: e.transpose(bank(3)[0:32, 0:16], valsc[0:16, 0:32], identF[0:16, 0:16]), r=["v8c", "identF"], w=[bkey(3)])
                        P.op("dve", lambda e: e.tensor_copy(idxTcu, bank(2)[0:32, 0:16]), r=[bkey(2)], w=["idxTcu"])
                        P.op("dve", lambda e: e.tensor_copy(gateTc, bank(3)[0:32, 0:16]), r=[bkey(3)], w=["gateTc"])
                        P.op("dve", lambda e: e.tensor_copy(idxTcU, idxTcu), r=["idxTcu"], w=["idxTcU"])
                        P.dma("sp", IDXC, idxTcu, r=["idxTcu"], w=["IDXC"])
                        for j4 in range(4):
                            P.dma("sp", idxTcP[j4 * 32:(j4 + 1) * 32, :], IDXC.rearrange("s (t j) -> s j t", j=4)[:, j4, :], r=["IDXC"], w=["idxTcP"],
                                  allow_slow_non_contiguous=True)
                    if dbg:
                        P.dma("sp", DBG1[:, 0:CAP], idxf, r=["idxfl"], w=["DBG1"])
                        P.dma("sp", DBG1[:, CAP:2 * CAP], vals, r=["v8l"], w=["DBG1"])
                    P.barrier()
                with ExitStack() as es:
                    def sb(name, shape, dt=F32):
                        return es.enter_context(nc.sbuf_tensor(U(name), shape, dt)).ap()
                    ring = Ring(es)
                    xs = sb("xs", [128, 2, 3, 2048], BF16)
                    xsT = sb("xsT", [128, 16, NSL], BF16); actT = sb("actT", [128, 8, NSL], BF16); sA = sb("sA", [128, 2, NSL])
                    yest = sb("yest", [128, 6, 2048], BF16)
                    gi = 0
                    yi = 0
                    ne = cfg.get("n_experts", NE)

                    _p = list(range(min(NPC, ne))); _o = list(range(min(NPC, ne), ne)); eorder = []
                    while _p or _o:
                        if _p:
                            eorder.append(_p.pop(0))
                        eorder.extend(_o[:2]); del _o[:2]

                    def issue_scatter(pos):
                        ex = eorder[pos]
                        for (s0, ssz, st) in stiles_all:
                            ys = (pos % 2) * 3 + st
                            prev = [f"MOEx{pos - 1}_{k}" for k in range(3)]
                            if st < 2:
                                P.dma_scatter_add(MOE, idxTu[:, ex, st:st + 1], yest[:, ys, :], r=[f"yest{ys}", "idxTu"] + prev, w=[f"MOEx{pos}_{st}"])
                            else:
                                P.dma_scatter_add(MOE, idxTcU[0:32, ex:ex + 1], yest[0:32, ys, :], element_offset=L * D,
                                                  r=[f"yest{ys}", "idxTcU"] + prev, w=[f"MOEx{pos}_{st}"])
                    stiles_all = [(0, 128, 0), (128, 128, 1)] + ([(256, 32, 2)] if need_ctx else [])
                    gtiles = [(0, 128, 0), (128, 128, 1)] + ([(256, 32, 2)] if need_ctx else [])

                    def issue_gather(pos):
                        ex = eorder[pos]
                        xb = pos % 2
                        for (s0, ssz, st) in gtiles:
                            if st < 2:
                                P.dma_gather(xs[:, xb, st, :], H2D, idxTu[:, ex, st:st + 1], r=["H2D", "idxTu"], w=[f"xs{xb}_{st}"])
                            else:
                                P.dma_gather(xs[0:32, xb, st, :], H2D, idxTcU[0:32, ex:ex + 1], element_offset=L * D,
                                             r=["H2D", "idxTcU"], w=[f"xs{xb}_{st}"])
                    if ne > 0:
                        issue_gather(0)
                    for pos in range(ne):
                        ex = eorder[pos]
                        xb = pos % 2
                        if pos + 1 < ne:
                            issue_gather(pos + 1)
                        for (s0, ssz, st) in gtiles:
                            for cb in range(4):
                                b = 1 + gi % 4
                                gi += 1
                                for cc in range(4):
                                    c = cb * 4 + cc
                                    P.op("pe", lambda e: e.matmul(bank(b)[:, cc * 128:cc * 128 + ssz], xs[0:ssz, xb, st, c * 128:(c + 1) * 128],
                                                                  identB[0:ssz, 0:ssz], start=True, stop=True),
                                         r=[f"xs{xb}_{st}", "identB"], w=[bkey(b)])
                                copy_op(evac_eng(), xsT[:, cb * 4:(cb + 1) * 4, s0:s0 + ssz],
                                        bank(b).rearrange("p (c n) -> p c n", c=4)[:, :, 0:ssz], r=[bkey(b)], w=["xsT"])
                        for hf in range(2):
                            gsrc = WBG[ex] if ex < NPC else w_gate[l, ex]
                            usrc = WBU[ex] if ex < NPC else w_up[l, ex]
                            G, gk = ring.load(gsrc[:, hf * 512:(hf + 1) * 512].rearrange("(c p) n -> p c n", p=128),
                                              lambda t: t.rearrange("p (c n) -> p c n", c=16))
                            Uw, uk = ring.load(usrc[:, hf * 512:(hf + 1) * 512].rearrange("(c p) n -> p c n", p=128),
                                               lambda t: t.rearrange("p (c n) -> p c n", c=16))
                            for fcl in range(4):
                                fc = hf * 4 + fcl
                                bA, bU = (1, 2) if fcl % 2 == 0 else (3, 4)
                                for c in range(16):
                                    P.op("pe", lambda e: e.matmul(bank(bA)[:, 0:nsl], G[:, c, fcl * 128:(fcl + 1) * 128], xsT[:, c, 0:nsl], start=(c == 0), stop=(c == 15)),
                                         r=[gk, "xsT"], w=[bkey(bA)])
                                for c in range(16):
                                    P.op("pe", lambda e: e.matmul(bank(bU)[:, 0:nsl], Uw[:, c, fcl * 128:(fcl + 1) * 128], xsT[:, c, 0:nsl], start=(c == 0), stop=(c == 15)),
                                         r=[uk, "xsT"], w=[bkey(bU)])
                                ss = fcl % 2
                                P.op("act", lambda e: e.activation(sA[:, ss, 0:nsl], bank(bA)[:, 0:nsl], AF.Silu), r=[bkey(bA)], w=[f"sA{ss}"])
                                P.op("dve", lambda e: e.tensor_tensor(actT[:, fc, 0:nsl], sA[:, ss, 0:nsl], bank(bU)[:, 0:nsl], ALU.mult),
                                     r=[f"sA{ss}", bkey(bU)], w=["actT"])
                        if pos > 0:
                            issue_scatter(pos - 1)
                        Dt = []
                        for hf in range(2):
                            dsrc = WBD[ex] if ex < NPC else w_down[l, ex]
                            Dw, dk = ring.load(dsrc[hf * 512:(hf + 1) * 512, :].rearrange("(c p) n -> p c n", p=128),
                                               lambda t: t.rearrange("p (c n) -> p c n", c=4))
                            Dt.append((Dw, dk))
                        stiles = [(0, 128, 0), (128, 128, 1)] + ([(256, 32, 2)] if need_ctx else [])
                        for (s0, ssz, st) in stiles:
                            ys = (pos % 2) * 3 + st
                            for dg in range(4):
                                b = 5 + gi % 3
                                gi += 1
                                for fc in range(8):
                                    Dw, dk = Dt[fc // 4]
                                    P.op("pe", lambda e: e.matmul(bank(b)[0:ssz, :], actT[:, fc, s0:s0 + ssz], Dw[:, fc % 4, dg * 512:(dg + 1) * 512],
                                                                  start=(fc == 0), stop=(fc == 7)), r=["actT", dk], w=[bkey(b)])
                                gsc = gateT[:, ex, st:st + 1] if st < 2 else gateTc[:, ex:ex + 1]
                                eng = evac_eng()
                                if eng == "act":
                                    P.op("act", lambda e: e.activation(yest[0:ssz, ys, dg * 512:(dg + 1) * 512], bank(b)[0:ssz, :], AF.Copy, scale=gsc[0:ssz]),
                                         r=[bkey(b), "gateT", "gateTc"], w=[f"yest{ys}"])
                                else:
                                    P.op("dve", lambda e: e.tensor_scalar(yest[0:ssz, ys, dg * 512:(dg + 1) * 512], bank(b)[0:ssz, :], gsc[0:ssz], None, ALU.mult),
                                         r=[bkey(b), "gateT", "gateTc"], w=[f"yest{ys}"])
                    if ne > 0:
                        issue_scatter(ne - 1)
                    P.barrier()
            with ExitStack() as es:
                def sb(name, shape, dt=F32):
                    return es.enter_context(nc.sbuf_tensor(U(name), shape, dt)).ap()
                mt = sb("mt", [128, 4, 2048]); xg = sb("xg3", [128, 16, 512])
                bi = 0
                for (g0, gsz) in grps:
                    r = 1 if g0 >= L else 0
                    ntl = gsz // 128
                    P.dma("sp", xg[:, :, 0:gsz], XTv[:, :, g0:g0 + gsz], r=["XT"], w=["xg"])
                    for tt in range(ntl):
                        P.dma("sp", mt[:, tt, :], MOE[g0 + tt * 128:g0 + (tt + 1) * 128, :], r=["MOE"], w=[f"mt{tt}"])
                    for c in range(16):
                        b = bi % 6
                        bi += 1
                        for tt in range(ntl):
                            P.op("pe", lambda e: e.transpose(bank(b)[:, tt * 128:(tt + 1) * 128], mt[:, tt, c * 128:(c + 1) * 128], identF),
                                 r=[f"mt{tt}", "identF"], w=[bkey(b)])
                        P.op("dve", lambda e: e.scalar_tensor_tensor(xg[:, c, 0:gsz], bank(b)[:, 0:gsz], ada_vec(l, 5, r)[:, c:c + 1],
                                                                     xg[:, c, 0:gsz], ALU.mult, ALU.add),
                             r=[bkey(b), "xg", "ada_sb"], w=["xg"])
                    P.dma("sp", XTv[:, :, g0:g0 + gsz], xg[:, :, 0:gsz], r=["xg"], w=["XT"])
                P.barrier()

    def phase_final():
        with ExitStack() as es:
            def sb(name, shape, dt=F32):
                return es.enter_context(nc.sbuf_tensor(U(name), shape, dt)).ap()
            xg = sb("xg", [128, 16, 512]); xn = sb("xn", [128, 16, 512]); nb = alloc_norm_bufs(es)
            ost = sb("ost", [128, 2, 2048])
            oi = 0
            bi = 0
            for (g0, gsz) in GROUPS[:4]:
                P.dma("sp", xg, XTv[:, :, g0:g0 + gsz], r=["XT"], w=["xg"])

                def out_fn(c, tc, key):
                    copy_op("act" if c % 2 else "pool", xn[:, c, :], tc, r=[key], w=["xn"])
                norm_group(nb, xg, gsz, nfin_sb, None, out_fn)
                for tt in range(4):
                    os_ = oi % 2
                    oi += 1
                    for cb in range(4):
                        b = bi % 6
                        bi += 1
                        for cc in range(4):
                            c = cb * 4 + cc
                            P.op("pe", lambda e: e.transpose(bank(b)[:, cc * 128:(cc + 1) * 128], xn[:, c, tt * 128:(tt + 1) * 128], identF),
                                 r=["xn", "identF"], w=[bkey(b)])
                        copy_op(evac_eng(), ost[:, os_, cb * 512:(cb + 1) * 512], bank(b), r=[bkey(b)], w=[f"ost{os_}"])
                    P.dma("sp", y_out[g0 + tt * 128:g0 + (tt + 1) * 128, :], ost[:, os_, :], r=[f"ost{os_}"], w=["y"])
            P.barrier()

    if not cfg.get("skip_ada"):
        phase_ada()
    only_mix = cfg.get("only_mix")
    for l in range(nlayers):
        if not cfg.get("skip_inproj"):
            with ExitStack() as esl:
                hT = esl.enter_context(nc.sbuf_tensor(U("hT"), [128, 16, NT], BF16)).ap()
                phase_norm1(l, hT, esl)
                if stop_after == "norm1":
                    break
                phase_inproj(l, hT)
        if stop_after == "inproj":
            break
        if only_mix is None or "ret" in only_mix:
            phase_ret(l)
        if only_mix is None or "conv" in only_mix:
            phase_conv(l)
        if only_mix is None or "swa" in only_mix:
            phase_swa(l)
        if only_mix is None or "na" in only_mix:
            phase_na(l)
        if stop_after == "mix":
            break
        if not cfg.get("skip_outproj"):
            phase_outproj(l)
        if stop_after == "outproj":
            break
        phase_ffn(l)
        if stop_after == "ffn":
            break
    else:
        phase_final()

    P.barrier()
    print(f"[build] ops={P.nops} waits={P.nwaits}")
    return nc


def make_in_maps(inputs):
    hc = _host_consts()
    f = lambda a: np.ascontiguousarray(np.asarray(a, dtype=np.float32))
    x = f(inputs["x"]); c = f(inputs["c"]); ctx = f(inputs["ctx"]); c_ctx = f(inputs["c_ctx"])
    shared = {
        "w_ada": f(inputs["w_ada"]),
        "bada_t": f(np.asarray(inputs["b_ada"]).reshape(DEPTH, 96, 128).transpose(0, 2, 1)),
        "nmix_t": f(np.asarray(inputs["norm_mix"]).reshape(DEPTH, 16, 128).transpose(0, 2, 1)),
        "nffn_t": f(np.asarray(inputs["norm_ffn"]).reshape(DEPTH, 16, 128).transpose(0, 2, 1)),
        "nfin_t": f(np.asarray(inputs["norm_final"]).reshape(16, 128).T),
        "w_in": f(inputs["w_in"]), "w_out": f(inputs["w_out"]),
        "decA": f(np.concatenate([inputs["ret_decay_fwd"], inputs["ret_decay_bwd"]], axis=1)),
        "decB": f(np.stack([np.concatenate([np.asarray(inputs["ret_decay_fwd"])[:, hf::2],
                                            np.asarray(inputs["ret_decay_bwd"])[:, hf::2]], axis=1) for hf in range(2)], axis=1)),
        "convw_t": f(np.asarray(inputs["conv_w"]).reshape(DEPTH, 3, 4, 128).transpose(0, 3, 2, 1)),
        "sink": f(inputs["swa_sink"]),
        "na_bias": _na_bias_layout(np.asarray(inputs["na_rpb"], dtype=np.float32)),
        "w_router": f(inputs["w_router"]),
        "w_gate": f(inputs["w_gate"]), "w_up": f(inputs["w_up"]), "w_down": f(inputs["w_down"]),
    }
    shared.update(hc)
    maps = []
    for b in range(8):
        m = dict(shared)
        m["x"] = x[b]
        m["ctx"] = ctx[b]
        m["c_t"] = f(np.stack([c[b].reshape(16, 128).T, c_ctx.reshape(16, 128).T], axis=-1))
        maps.append(m)
    return maps


def kernel(**inputs):
    nc = build_program()
    maps = make_in_maps(inputs)
    res = run_bass_kernel_spmd(nc, maps, core_ids=list(range(8)))
    return np.stack([np.asarray(r["y"], dtype=np.float32) for r in res.results], axis=0)
```
